# Optimizing a Trainium2 kernel written in Bass

```python
import math
import jax
import jax.numpy as jnp
from jax import lax
import numpy as np

D_MODEL = 1024
BATCH = 8
SEQ = 4096
DEPTH = 4

GRID_W = 64
CTX_LEN = 256

ATT_HEADS = 4
ATT_QK = 64
ATT_V = 2 * ATT_QK
ATT_WIDTH = ATT_HEADS * ATT_V
AXIS_DIM = ATT_QK // 2
ROPE_THETA = 10000.0
Q_BLOCK = 128
SUBLN_EPS = 1e-5

RWKV_HEADS = 4
RWKV_N = 64
RWKV_WIDTH = RWKV_HEADS * RWKV_N
DECAY_LORA = 32
ICLR_LORA = 32
GATE_LORA = 64
N_DIR = 2
GN_EPS = 64e-5

CONV_WIDTH = 256
CONV_K = 3

MIX_WIDTH = ATT_WIDTH + RWKV_WIDTH + CONV_WIDTH
IN_SIZES = (ATT_WIDTH, ATT_WIDTH, ATT_WIDTH,
            RWKV_WIDTH, RWKV_WIDTH, RWKV_WIDTH, N_DIR * DECAY_LORA, N_DIR * ICLR_LORA, GATE_LORA,
            CONV_WIDTH, CONV_WIDTH, CONV_WIDTH)
IN_WIDTH = sum(IN_SIZES)
IN_OFFSETS = tuple(int(o) for o in np.cumsum(IN_SIZES)[:-1])

D_FF = 2816
N_EXPERTS = 8
TOP_K = 2
D_FF_EXPERT = 1408
N_DENSE = (DEPTH + 1) // 2
N_MOE = DEPTH // 2

DN_ALPHA = (2 * DEPTH) ** 0.25
DN_BETA = (8 * DEPTH) ** -0.25
LN_EPS = 1e-5

kernel_name = 'hybrid_diffattn_rwkv7_shortconv_moe_dit'


def layer_norm(x, g, b, eps=LN_EPS):
    xf = x.astype(jnp.float32)
    mu = xf.mean(-1, keepdims=True)
    var = jnp.square(xf - mu).mean(-1, keepdims=True)
    return ((xf - mu) * lax.rsqrt(var + eps) * g + b).astype(x.dtype)


def rms_norm(x, g, eps):
    xf = x.astype(jnp.float32)
    return (xf * lax.rsqrt(jnp.mean(xf * xf, -1, keepdims=True) + eps) * g).astype(x.dtype)


def head_norm(y, g, b, eps):
    yf = y.astype(jnp.float32)
    mu = yf.mean(-1, keepdims=True)
    var = jnp.square(yf - mu).mean(-1, keepdims=True)
    yn = (yf - mu) * lax.rsqrt(var + eps)
    return yn.reshape(y.shape[:-2] + (-1,)) * g + b


def short_conv(u, w):
    return lax.conv_general_dilated(
        u, w[:, None, :].astype(u.dtype), window_strides=(1,),
        padding=((CONV_K // 2, CONV_K // 2),), dimension_numbers=('NWC', 'WIO', 'NWC'),
        feature_group_count=u.shape[-1])


def axial_rope(x, row, col):
    b, t, h, m, _ = x.shape
    inv = ROPE_THETA ** (-jnp.arange(0, AXIS_DIM, 2, dtype=jnp.float32) / AXIS_DIM)
    ang = jnp.stack([row, col], -1).astype(jnp.float32)[..., None] * inv
    cos = jnp.cos(ang)[None, :, None, None]
    sin = jnp.sin(ang)[None, :, None, None]
    xf = x.astype(jnp.float32).reshape(b, t, h, m, 2, 2, AXIS_DIM // 2)
    x1, x2 = xf[..., 0, :], xf[..., 1, :]
    out = jnp.stack([x1 * cos - x2 * sin, x2 * cos + x1 * sin], -2)
    return out.reshape(x.shape).astype(x.dtype)


def diff_attend(q, k, v, lam):
    s = jnp.einsum('bqhmd,bkhmd->bhmqk', q, k).astype(jnp.float32) * (ATT_QK ** -0.5)
    p = jax.nn.softmax(s, axis=-1)
    w = p[:, :, 0] - lam * p[:, :, 1]
    return jnp.einsum('bhqk,bkhd->bqhd', w.astype(v.dtype), v)


def blockwise_diff_attend(q, k, v, lam):
    b, t = q.shape[:2]
    nb = t // Q_BLOCK
    qb = jnp.moveaxis(q.reshape((b, nb, Q_BLOCK) + q.shape[2:]), 1, 0)
    ob = lax.map(lambda qi: diff_attend(qi, k, v, lam), qb)
    return jnp.moveaxis(ob, 0, 1).reshape(b, t, ATT_HEADS, ATT_V)


def rwkv_prepare(r, k, v, wd, ad, gd, conv_w, w0, w_up, a0, a_up, g_up, k_k, k_a):
    r, k, v = jnp.split(short_conv(jnp.concatenate([r, k, v], -1), conv_w), 3, axis=-1)
    b, t, _ = r.shape
    wl = w0[:, None, None, :] + jnp.einsum('btdr,drc->dbtc', jnp.tanh(wd.reshape(b, t, N_DIR, DECAY_LORA)), w_up)
    decay = jnp.exp(-jnp.exp(-jax.nn.softplus(-wl.astype(jnp.float32)) - 0.5))
    a = jax.nn.sigmoid(a0[:, None, None, :] + jnp.einsum('btdr,drc->dbtc', ad.reshape(b, t, N_DIR, ICLR_LORA), a_up))
    g = jax.nn.sigmoid(gd) @ g_up
    kk = (k * k_k).reshape(b, t, RWKV_HEADS, RWKV_N).astype(jnp.float32)
    kk = (kk * lax.rsqrt(jnp.maximum(jnp.sum(kk * kk, -1, keepdims=True), 1e-24))).reshape(b, t, RWKV_WIDTH)
    kd = k * (1.0 + (a - 1.0) * k_a)
    return r, v, kk, decay, a, kd, g


def rwkv_scan(state0, r, v, kk, decay, a, kd, emit):
    def to_scan(u):
        u = jnp.stack([u[0], jnp.flip(u[1], 1)]).astype(jnp.float32)
        d, b, t, _ = u.shape
        return jnp.transpose(u, (2, 0, 1, 3)).reshape(t, d, b, RWKV_HEADS, RWKV_N)

    def both(u):
        return jnp.stack([u, u])

    xs = (to_scan(decay), to_scan(kd), to_scan(both(v)), to_scan(both(kk)), to_scan(a))
    if emit:
        xs = xs + (to_scan(both(r)),)

    def step(s, inp):
        w_t, k_t, v_t, kk_t, a_t = inp[:5]
        sa = jnp.einsum('dbhvk,dbhk->dbhv', s, -kk_t)
        s = s * w_t[..., None, :] + sa[..., None] * (kk_t * a_t)[..., None, :] + v_t[..., None] * k_t[..., None, :]
        y = jnp.einsum('dbhvk,dbhk->dbhv', s, inp[5]) if emit else None
        return s, y

    s, y = lax.scan(step, state0, xs)
    if not emit:
        return s, None
    y = y[:, 0] + jnp.flip(y[:, 1], 0)
    return s, jnp.transpose(y, (1, 0, 2, 3))


def rwkv_output(y, r, kd, v, g, r_k, gn_g, gn_b):
    b, t, _ = r.shape
    hs = lambda u: u.reshape(u.shape[:-1] + (RWKV_HEADS, RWKV_N))
    yn = head_norm(y, gn_g, gn_b, GN_EPS)
    bonus = jnp.sum(hs(r) * hs(kd) * r_k, axis=(0, -1)).astype(jnp.float32)
    out = yn + (bonus[..., None] * hs(v)).reshape(b, t, RWKV_WIDTH)
    return (out * g).astype(r.dtype)


def token_mixer(hl, hc, row, col, lam, lam_init, with_ctx, w_in, subln_g, rkv_conv, decay_w0, decay_up,
                iclr_a0, iclr_up, gate_up, k_k, k_a, r_k, gn_g, gn_b, conv_w, w_out):
    (q_l, k_l, v_l, rr_l, rk_l, rv_l, wd_l, ad_l, gd_l, ch_l, cb_l, cc_l) = jnp.split(hl @ w_in, IN_OFFSETS, axis=-1)
    (q_c, k_c, v_c, rr_c, rk_c, rv_c, wd_c, ad_c, gd_c, ch_c, cb_c, cc_c) = jnp.split(hc @ w_in, IN_OFFSETS, axis=-1)
    qk_heads = lambda u: u.reshape(u.shape[:2] + (ATT_HEADS, 2, ATT_QK))
    v_heads = lambda u: u.reshape(u.shape[:2] + (ATT_HEADS, ATT_V))
    att_post = lambda o: (rms_norm(o, subln_g, SUBLN_EPS) * (1.0 - lam_init)).reshape(o.shape[:2] + (ATT_WIDTH,))

    kc, vc = qk_heads(k_c), v_heads(v_c)
    k_all = jnp.concatenate([kc, axial_rope(qk_heads(k_l), row, col)], axis=1)
    v_all = jnp.concatenate([vc, v_heads(v_l)], axis=1)
    att_l = att_post(blockwise_diff_attend(axial_rope(qk_heads(q_l), row, col), k_all, v_all, lam))

    rw_args = (rkv_conv, decay_w0, decay_up, iclr_a0, iclr_up, gate_up, k_k, k_a)
    r_c, vr_c, kk_c, dec_c, a_c, kd_c, g_c = rwkv_prepare(rr_c, rk_c, rv_c, wd_c, ad_c, gd_c, *rw_args)
    r_l, vr_l, kk_l, dec_l, a_l, kd_l, g_l = rwkv_prepare(rr_l, rk_l, rv_l, wd_l, ad_l, gd_l, *rw_args)
    state0 = jnp.zeros((N_DIR, hl.shape[0], RWKV_HEADS, RWKV_N, RWKV_N), jnp.float32)
    s_ctx, y_c = rwkv_scan(state0, r_c, vr_c, kk_c, dec_c, a_c, kd_c, with_ctx)
    _, y_l = rwkv_scan(s_ctx, r_l, vr_l, kk_l, dec_l, a_l, kd_l, True)
    rwkv_l = rwkv_output(y_l, r_l, kd_l, vr_l, g_l, r_k, gn_g, gn_b)

    conv_l = cb_l * short_conv(cc_l * ch_l, conv_w)

    out_l = jnp.concatenate([att_l, rwkv_l, conv_l], axis=-1) @ w_out
    if not with_ctx:
        return out_l, None
    att_c = att_post(diff_attend(qk_heads(q_c), kc, vc, lam))
    rwkv_c = rwkv_output(y_c, r_c, kd_c, vr_c, g_c, r_k, gn_g, gn_b)
    conv_c = cb_c * short_conv(cc_c * ch_c, conv_w)
    out_c = jnp.concatenate([att_c, rwkv_c, conv_c], axis=-1) @ w_out
    return out_l, out_c


def swiglu(h, w1, w3, w2):
    return (jax.nn.silu(h @ w1) * (h @ w3)) @ w2


def moe_swiglu(h, router_w, router_b, w1, w3, w2):
    logits = (h @ router_w).astype(jnp.float32) + router_b
    top_logit, top_idx = lax.top_k(logits, TOP_K)
    top_gate = jax.nn.softmax(top_logit, axis=-1)
    gates = jnp.sum(jax.nn.one_hot(top_idx, N_EXPERTS, dtype=jnp.float32) * top_gate[..., None], axis=-2)
    out = jnp.zeros_like(h)
    for e in range(N_EXPERTS):
        out = out + gates[..., e:e + 1].astype(h.dtype) * swiglu(h, w1[e], w3[e], w2[e])
    return out


def setup_inputs(seed: int = 0) -> dict:
    key = jax.random.key(seed)
    keys = iter(jax.random.split(key, 48))

    def nrm(shape, scale):
        return jax.random.normal(next(keys), shape, jnp.float32) * scale

    L, D = DEPTH, D_MODEL
    centre_tap = jnp.array([0.0, 1.0, 0.0], jnp.float32)[None, :, None]
    return {
        'x': nrm((BATCH, SEQ, D), 1.0),
        'c': nrm((BATCH, D), 1.0),
        'ctx': nrm((BATCH, CTX_LEN, D), 1.0),
        'c_ctx': nrm((D,), 1.0),
        'w_ada': nrm((L, D, 6 * D), 0.5 * D ** -0.5),
        'b_ada': nrm((L, 6 * D), 0.02),
        'w_in': nrm((L, D, IN_WIDTH), D ** -0.5),
        'w_out': nrm((L, MIX_WIDTH, D), DN_BETA * MIX_WIDTH ** -0.5),
        'ln1_g': 1.0 + nrm((L, D), 0.02),
        'ln1_b': nrm((L, D), 0.02),
        'ln2_g': 1.0 + nrm((L, D), 0.02),
        'ln2_b': nrm((L, D), 0.02),
        'lam_q1': nrm((L, ATT_QK), 0.1),
        'lam_k1': nrm((L, ATT_QK), 0.1),
        'lam_q2': nrm((L, ATT_QK), 0.1),
        'lam_k2': nrm((L, ATT_QK), 0.1),
        'subln_g': 1.0 + nrm((L, ATT_V), 0.02),
        'rkv_conv': nrm((L, CONV_K, 3 * RWKV_WIDTH), 0.2) + centre_tap,
        'decay_w0': nrm((L, N_DIR, RWKV_WIDTH), 1.0) - 2.0,
        'decay_up': nrm((L, N_DIR, DECAY_LORA, RWKV_WIDTH), 0.1),
        'iclr_a0': nrm((L, N_DIR, RWKV_WIDTH), 0.5),
        'iclr_up': nrm((L, N_DIR, ICLR_LORA, RWKV_WIDTH), 0.1),
        'gate_up': nrm((L, GATE_LORA, RWKV_WIDTH), GATE_LORA ** -0.5),
        'k_k': 0.85 + nrm((L, RWKV_WIDTH), 0.05),
        'k_a': 1.0 + nrm((L, RWKV_WIDTH), 0.05),
        'r_k': nrm((L, RWKV_HEADS, RWKV_N), 0.1),
        'gn_g': 1.0 + nrm((L, RWKV_WIDTH), 0.02),
        'gn_b': nrm((L, RWKV_WIDTH), 0.02),
        'conv_w': nrm((L, CONV_K, CONV_WIDTH), CONV_K ** -0.5),
        'ffn_w1': nrm((N_DENSE, D, D_FF), D ** -0.5),
        'ffn_w3': nrm((N_DENSE, D, D_FF), D ** -0.5),
        'ffn_w2': nrm((N_DENSE, D_FF, D), DN_BETA * D_FF ** -0.5),
        'router_w': nrm((N_MOE, D, N_EXPERTS), D ** -0.5),
        'router_b': nrm((N_MOE, N_EXPERTS), 0.01),
        'moe_w1': nrm((N_MOE, N_EXPERTS, D, D_FF_EXPERT), D ** -0.5),
        'moe_w3': nrm((N_MOE, N_EXPERTS, D, D_FF_EXPERT), D ** -0.5),
        'moe_w2': nrm((N_MOE, N_EXPERTS, D_FF_EXPERT, D), DN_BETA * D_FF_EXPERT ** -0.5),
    }


def reference(x, c, ctx, c_ctx, w_ada, b_ada, w_in, w_out, ln1_g, ln1_b, ln2_g, ln2_b,
              lam_q1, lam_k1, lam_q2, lam_k2, subln_g, rkv_conv, decay_w0, decay_up, iclr_a0, iclr_up,
              gate_up, k_k, k_a, r_k, gn_g, gn_b, conv_w, ffn_w1, ffn_w3, ffn_w2,
              router_w, router_b, moe_w1, moe_w3, moe_w2):
    t = x.shape[1]
    rows = t // GRID_W
    row = jnp.repeat(jnp.arange(rows), GRID_W)
    col = jnp.tile(jnp.arange(GRID_W), rows)
    xl, xc = x, ctx
    sc, scc = jax.nn.silu(c), jax.nn.silu(c_ctx)
    for l in range(DEPTH):
        with_ctx = l < DEPTH - 1
        mod_l = jnp.split((sc @ w_ada[l] + b_ada[l])[:, None, :], 6, axis=-1)
        mod_c = jnp.split(scc @ w_ada[l] + b_ada[l], 6, axis=-1)

        lam_init = 0.8 - 0.6 * math.exp(-0.3 * l)
        lam = (jnp.exp(jnp.sum(lam_q1[l].astype(jnp.float32) * lam_k1[l].astype(jnp.float32)))
               - jnp.exp(jnp.sum(lam_q2[l].astype(jnp.float32) * lam_k2[l].astype(jnp.float32))) + lam_init)
        hl = xl * (1.0 + mod_l[1]) + mod_l[0]
        hc = xc * (1.0 + mod_c[1]) + mod_c[0]
        mix_l, mix_c = token_mixer(hl, hc, row, col, lam, lam_init, with_ctx, w_in[l], subln_g[l], rkv_conv[l],
                                   decay_w0[l], decay_up[l], iclr_a0[l], iclr_up[l], gate_up[l], k_k[l], k_a[l],
                                   r_k[l], gn_g[l], gn_b[l], conv_w[l], w_out[l])
        xl = layer_norm(DN_ALPHA * xl + mod_l[2] * mix_l, ln1_g[l], ln1_b[l])
        if with_ctx:
            xc = layer_norm(DN_ALPHA * xc + mod_c[2] * mix_c, ln1_g[l], ln1_b[l])

        def channel_mixer(h):
            j = l // 2
            if l % 2 == 0:
                return swiglu(h, ffn_w1[j], ffn_w3[j], ffn_w2[j])
            return moe_swiglu(h, router_w[j], router_b[j], moe_w1[j], moe_w3[j], moe_w2[j])

        hl = xl * (1.0 + mod_l[4]) + mod_l[3]
        xl = layer_norm(DN_ALPHA * xl + mod_l[5] * channel_mixer(hl), ln2_g[l], ln2_b[l])
        if with_ctx:
            hc = xc * (1.0 + mod_c[4]) + mod_c[3]
            xc = layer_norm(DN_ALPHA * xc + mod_c[5] * channel_mixer(hc), ln2_g[l], ln2_b[l])
    return xl
```

```python
import math
import contextlib
import bisect
import numpy as np
import concourse.bass as bass
import concourse.mybir as mybir
from concourse.bass_utils import run_bass_kernel_spmd

F32 = mybir.dt.float32
BF16 = mybir.dt.bfloat16
F32R = mybir.dt.float32r
ALU = mybir.AluOpType
AF = mybir.ActivationFunctionType
AX = mybir.AxisListType

D = 1024
TC = 256
TL = 4096
T = TC + TL
NT = T // 128
DEPTH = 4
INW = 3264
DFF = 2816
DFE = 1408
NE = 8
DN_ALPHA = (2 * DEPTH) ** 0.25
LN_EPS = 1e-5
GN_EPS = 64e-5
SUBLN_EPS = 1e-5
CH = 64
NCH = T // CH
PR = T + 3
import os
SCAN_LIMIT = int(os.environ.get('SCAN_LIMIT', '0'))
STAGE = int(os.environ.get('STAGE', '99'))
SUB = int(os.environ.get('SUB', '0'))


def prow(tok):
    return 1 + tok if tok < TC else 2 + tok


class Buf:
    __slots__ = ("w", "r", "excl")

    def __init__(self, excl=False):
        self.w = None
        self.r = {}
        self.excl = excl


class KB:
    KD = 8

    def __init__(self, nc):
        self.nc = nc
        self.eng = {"pe": nc.tensor, "act": nc.scalar, "dve": nc.vector, "pool": nc.gpsimd, "sp": nc.sync}
        self.sem = {e: nc.alloc_semaphore("s_" + e) for e in self.eng}
        self.cnt = {e: 0 for e in self.eng}
        self.ins = {e: [] for e in self.eng}
        self.sigi = {e: [] for e in self.eng}
        self.seen = {e: {} for e in self.eng}
        self.dsem = {q: [nc.alloc_semaphore("d_%s%d" % (q, i)) for i in range(self.KD)] for q in ("sp", "pool", "act")}
        self.duse = {q: [0] * self.KD for q in self.dsem}
        self.di = {q: 0 for q in self.dsem}
        self.nsb = 0
        self.rec = None
        self.stacks = [contextlib.ExitStack()]

    def sb(self, shape, dt=F32, name=None):
        self.nsb += 1
        return self.stacks[-1].enter_context(self.nc.sbuf_tensor("t%d" % self.nsb, list(shape), dt))

    @contextlib.contextmanager
    def phase(self):
        self.stacks.append(contextlib.ExitStack())
        try:
            yield
        finally:
            self.barrier()
            self.stacks.pop().close()

    def wait(self, e, ev):
        if ev is None:
            return
        sem, val, key = ev
        if key == "pe" and e == "pe":
            return
        if sem is None:
            sl = self.sigi[key]
            j = bisect.bisect_left(sl, val)
            if j == len(sl):
                self.ins[key][val - 1].then_inc(self.sem[key], 1)
                sl.append(val)
            val = j + 1
            sem = self.sem[key]
        if self.seen[e].get(key, 0) >= val:
            return
        self.eng[e].wait_ge(sem, val)
        self.seen[e][key] = val

    def _deps(self, e, reads, writes):
        for b in reads:
            self.wait(e, b.w)
            if b.excl:
                for k_, ev in b.r.items():
                    if k_ != e:
                        self.wait(e, ev)
        for b in writes:
            self.wait(e, b.w)
            for ev in b.r.values():
                self.wait(e, ev)

    def _post(self, ev, reads, writes):
        for b in reads:
            b.r[ev[2]] = ev
        for b in writes:
            b.w = ev
            b.r = {}

    def interleave(self, it, k=2):
        items = list(it)
        for j in range(0, len(items), k):
            recs = []
            for x in items[j:j + k]:
                self.rec = []
                yield x
                recs.append(self.rec)
            self.rec = None
            for t in range(max(len(r) for r in recs)):
                for r in recs:
                    if t < len(r):
                        kind, a = r[t]
                        if kind == "op":
                            self.op(*a)
                        else:
                            self.dma(*a)

    def op(self, e, fn, reads=(), writes=()):
        if self.rec is not None:
            self.rec.append(("op", (e, fn, tuple(reads), tuple(writes))))
            return None
        self._deps(e, reads, writes)
        ins = fn(self.eng[e])
        self.cnt[e] += 1
        self.ins[e].append(ins)
        ev = (None, self.cnt[e], e)
        self._post(ev, reads, writes)
        return ev

    def dma(self, q, out, in_, reads=(), writes=()):
        if self.rec is not None:
            self.rec.append(("dma", (q, out, in_, tuple(reads), tuple(writes))))
            return None
        self._deps(q, reads, writes)
        k = self.di[q] % self.KD
        self.di[q] += 1
        key = "d_%s%d" % (q, k)
        sem = self.dsem[q][k]
        if self.duse[q][k] > 0:
            self.wait(q, (sem, 16 * self.duse[q][k], key))
        ins = self.eng[q].dma_start(out=out, in_=in_)
        self.duse[q][k] += 1
        ins.then_inc(sem, 16)
        ev = (sem, 16 * self.duse[q][k], key)
        self._post(ev, reads, writes)
        return ev

    def barrier(self):
        evs = [(None, self.cnt[e], e) for e in self.eng if self.cnt[e] > 0]
        for q in self.dsem:
            for k in range(self.KD):
                if self.duse[q][k] > 0:
                    evs.append((self.dsem[q][k], 16 * self.duse[q][k], "d_%s%d" % (q, k)))
        for e in self.eng:
            for ev in evs:
                self.wait(e, ev)


class Ring:
    def __init__(self, kb, n, shape, dt=F32):
        self.t = [kb.sb(shape, dt) for _ in range(n)]
        self.b = [Buf() for _ in range(n)]
        self.i = 0

    def next(self):
        j = self.i % len(self.t)
        self.i += 1
        return self.t[j], self.b[j]


def make_consts():
    c = np.zeros((128, 1024), np.float32)
    c[:, 0:128] = np.eye(128, dtype=np.float32)
    ii = np.arange(CH)
    for d in range(2):
        before = (ii[:, None] > ii[None, :]) if d == 1 else (ii[:, None] < ii[None, :])
        beq = before | np.eye(CH, dtype=bool)
        c[0:CH, 128 + d * 128:128 + d * 128 + 64] = before
        c[0:CH, 128 + d * 128 + 64:128 + d * 128 + 128] = beq
        c[0:CH, 384 + d * 64:384 + d * 64 + 64] = before.T
        c[0:CH, 512 + d * 64:512 + d * 64 + 64] = beq
    c[:, 640:768] = 1.0
    c[0, 768:896] = 1.0
    c[1, 896:1024] = 1.0
    return c


def make_rope():
    rows = TL // 64
    row = np.repeat(np.arange(rows), 64).astype(np.float64)
    col = np.tile(np.arange(64), rows).astype(np.float64)
    inv = 10000.0 ** (-np.arange(0, 32, 2, dtype=np.float64) / 32)
    ar = row[:, None] * inv
    ac = col[:, None] * inv
    cosT = np.concatenate([np.cos(ar), np.cos(ar), np.cos(ac), np.cos(ac)], 1)
    sinT = np.concatenate([-np.sin(ar), np.sin(ar), -np.sin(ac), np.sin(ac)], 1)
    return np.concatenate([cosT, sinT], 1).astype(np.float32)


PARAMS = [("w_ada", [DEPTH, D, 6 * D]), ("b_ada", [DEPTH, 6 * D]), ("w_in", [DEPTH, D, INW]), ("w_out", [DEPTH, D, D]),
          ("ln1_g", [DEPTH, D]), ("ln1_b", [DEPTH, D]), ("ln2_g", [DEPTH, D]), ("ln2_b", [DEPTH, D]),
          ("lam_q1", [DEPTH, 64]), ("lam_k1", [DEPTH, 64]), ("lam_q2", [DEPTH, 64]), ("lam_k2", [DEPTH, 64]),
          ("subln_g", [DEPTH, 128]), ("rkv_conv", [DEPTH, 3, 768]), ("decay_w0", [DEPTH, 2, 256]),
          ("decay_up", [DEPTH, 2, 32, 256]), ("iclr_a0", [DEPTH, 2, 256]), ("iclr_up", [DEPTH, 2, 32, 256]),
          ("gate_up", [DEPTH, 64, 256]), ("k_k", [DEPTH, 256]), ("k_a", [DEPTH, 256]), ("r_k", [DEPTH, 256]),
          ("gn_g", [DEPTH, 256]), ("gn_b", [DEPTH, 256]), ("conv_w", [DEPTH, 3, 256]),
          ("ffn_w1", [2, D, DFF]), ("ffn_w3", [2, D, DFF]), ("ffn_w2", [2, DFF, D]),
          ("router_w", [2, D, NE]), ("router_b", [2, NE]),
          ("moe_w1", [2, NE, D, DFE]), ("moe_w3", [2, NE, D, DFE]), ("moe_w2", [2, NE, DFE, D])]


def build(n_layers=DEPTH, dump=None):
    nc = bass.Bass("TRN2", target_bir_lowering=False)
    kb = KB(nc)
    I = {}
    I["x"] = nc.dram_tensor("x", [TL, D], F32, kind="ExternalInput").ap()
    I["ctx"] = nc.dram_tensor("ctx", [TC, D], F32, kind="ExternalInput").ap()
    I["cvec"] = nc.dram_tensor("cvec", [16, 128], F32, kind="ExternalInput").ap()
    I["consts"] = nc.dram_tensor("consts", [128, 1024], F32, kind="ExternalInput").ap()
    I["rope"] = nc.dram_tensor("rope", [TL, 128], F32, kind="ExternalInput").ap()
    for n, s in PARAMS:
        I[n] = nc.dram_tensor(n, s, F32, kind="ExternalInput").ap()
    OUT = nc.dram_tensor("out", [TL, D], F32, kind="ExternalOutput").ap()
    dumps = {}

    def scratch(name, shape, dt=F32):
        if dump and name in dump:
            dumps[name] = nc.dram_tensor("dump_" + name, shape, dt, kind="ExternalOutput").ap()
            return dumps[name]
        return nc.dram_tensor("scr_" + name, shape, dt).ap()

    X = scratch("X", [T, D])
    X1 = scratch("X1", [T, D])
    QT = scratch("QT", [4, 128, T], BF16)
    KT = scratch("KT", [4, 128, T], BF16)
    VV = scratch("VV", [T, 512], BF16)
    RKV = scratch("RKV", [PR, 768])
    CIN = scratch("CIN", [PR, 768])
    LORA = scratch("LORA", [T, 192])
    MIX = scratch("MIX", [T, D])
    PREP = scratch("PREP", [T, 2564])
    YD = scratch("YD", [2, T, 256])
    MODD = scratch("MODD", [2, 6 * D])
    dB = {n: [Buf() for _ in range(NT + 2)] for n in ("X", "X1", "QK", "VV", "RKV", "CIN", "LORA", "MIX", "PREP", "YD0", "YD1")}

    cst = kb.sb([128, 1024], F32, "cst")
    cstB = Buf()
    kb.dma("sp", cst[:], I["consts"][:, :], writes=[cstB])
    ident = cst[:, 0:128]
    zrow = kb.sb([1, 768], F32, "zrow")
    zB = Buf()
    kb.op("pool", lambda e: e.memset(zrow[:], 0.0), writes=[zB])
    for r_ in (0, TC + 1, PR - 1):
        kb.dma("sp", RKV[r_:r_ + 1, :], zrow[:], reads=[zB])
        kb.dma("sp", CIN[r_:r_ + 1, :], zrow[:], reads=[zB])
    PS = [nc.alloc_psum_tensor("ps%d" % i, [128, 512], F32) for i in range(8)]
    PB = [Buf(excl=True) for _ in range(8)]
    kb.barrier()

    def xsrc(l, i):
        if l == 0:
            return I["ctx"][i * 128:(i + 1) * 128, :] if i < 2 else I["x"][(i - 2) * 128:(i - 1) * 128, :]
        return X[i * 128:(i + 1) * 128, :]


    upto = dump[0] if dump else None
    stop = [False]

    for l in range(n_layers):
        last = (l == DEPTH - 1)
        lam_init = 0.8 - 0.6 * math.exp(-0.3 * l)
        lstack = contextlib.ExitStack()
        kb.stacks.append(lstack)
        modS = kb.sb([128, 2, 4, 8], F32)
        modSB = Buf()
        mrowDB = Buf()

        def load_gate_ln(which):
            gB_ = kb.sb([128, 2, D], F32)
            gBB_ = Buf()
            ln_ = kb.sb([128, 2, D], F32)
            lnB_ = Buf()
            v = 2 if which == 0 else 5
            for w_ in range(2):
                kb.dma("sp", gB_[:, w_, :], MODD[w_, v * D:(v + 1) * D].partition_broadcast(128), reads=[mrowDB], writes=[gBB_])
            for j, n in enumerate((("ln1_g", "ln1_b") if which == 0 else ("ln2_g", "ln2_b"))):
                kb.dma("sp", ln_[:, j, :], I[n][l, :].partition_broadcast(128), writes=[lnB_])
            return gB_, gBB_, ln_, lnB_

        with kb.phase():
            cv = kb.sb([16, 128], F32)
            cvB = Buf()
            kb.dma("sp", cv[:], I["cvec"][:, :], writes=[cvB])
            s2 = kb.sb([128, 8, 2], F32)
            s2B = Buf()
            kb.op("pe", lambda e: e.transpose(out=PS[0][:, 0:16], in_=cv[:], identity=cst[0:16, 0:16]), reads=[cvB, cstB], writes=[PB[0]])
            kb.op("act", lambda e: e.activation(out=s2[:].rearrange("p c w -> p w c"), in_=PS[0][:, 0:16].rearrange("p (w c) -> p w c", w=2), func=AF.Silu),
                  reads=[PB[0]], writes=[s2B])
            wr = Ring(kb, 2, [128, 8, 512], F32)
            ba = kb.sb([2, 6 * D], F32)
            baB = Buf()
            kb.dma("sp", ba[0:1, :], I["b_ada"][l:l + 1, :], writes=[baB])
            kb.dma("sp", ba[1:2, :], I["b_ada"][l:l + 1, :], writes=[baB])
            mrow = kb.sb([2, 6 * D], F32)
            mrowB = Buf()
            for n in range(12):
                wt, wb = wr.next()
                kb.dma("sp", wt[:], I["w_ada"][l, :, n * 512:(n + 1) * 512].rearrange("(c p) n -> p c n", p=128), writes=[wb])
                pb = 1 + (n % 2)
                for c in range(8):
                    kb.op("pe", lambda e, c=c, wt=wt, pb=pb: e.matmul(PS[pb][0:2, :], lhsT=s2[:, c, :], rhs=wt[:, c, :], start=(c == 0), stop=(c == 7)),
                          reads=[s2B, wb], writes=[PB[pb]])
                kb.op("dve", lambda e, n=n, pb=pb: e.tensor_tensor(out=mrow[:, n * 512:(n + 1) * 512], in0=PS[pb][0:2, :], in1=ba[:, n * 512:(n + 1) * 512], op=ALU.add),
                      reads=[PB[pb], baB], writes=[mrowB])
            for v in (1, 4):
                kb.op("dve", lambda e, v=v: e.tensor_scalar(out=mrow[:, v * D:(v + 1) * D], in0=mrow[:, v * D:(v + 1) * D], scalar1=1.0, scalar2=None, op0=ALU.add),
                      reads=[mrowB], writes=[mrowB])
            kb.dma("sp", MODD[:, :], mrow[:], reads=[mrowB], writes=[mrowDB])
            for j, v in enumerate((0, 1, 3, 4)):
                for c in range(8):
                    o = (j * 8 + c) * 2
                    kb.op("pe", lambda e, o=o, v=v, c=c: e.matmul(PS[3][:, o:o + 2], lhsT=mrow[0:2, v * D + c * 128:v * D + (c + 1) * 128],
                                                              rhs=cst[0:2, 0:2], start=True, stop=True), reads=[mrowB, cstB], writes=[PB[3]])
            kb.op("act", lambda e: e.activation(out=modS[:].rearrange("p w j c -> p j c w"), in_=PS[3][:, 0:64].rearrange("p (j c w) -> p j c w", j=4, c=8), func=AF.Identity),
                  reads=[PB[3]], writes=[modSB])
        if upto == "MODD":
            break

        with kb.phase():
            win = kb.sb([128, 8, INW], BF16)
            winB = Buf()
            for c in range(8):
                kb.dma("pool", win[:, c, :], I["w_in"][l, c * 128:(c + 1) * 128, :], writes=[winB])
            xr = Ring(kb, 2, [128, D], F32)
            hT = Ring(kb, 2, [128, 8, 128], BF16)
            qk = Ring(kb, 2, [128, 1024], F32)
            qkr = Ring(kb, 2, [128, 1024], F32)
            qkt = Ring(kb, 2, [128, 1024], F32)
            rp = Ring(kb, 2, [128, 128], F32)
            qkT = Ring(kb, 2, [128, 8, 128], BF16)
            vb = Ring(kb, 2, [128, 512], BF16)
            pj = Ring(kb, 2, [128, 1728], F32)
            chunks = [(0, 512), (512, 1024), (1024, 1536), (1536, 2048), (2048, 2496), (2496, 3008), (3008, 3264)]
            for i in range(NT):
                w = 1 if i < 2 else 0
                pr0 = prow(i * 128)
                xt, xb = xr.next()
                kb.dma("sp", xt[:], xsrc(l, i), reads=([dB["X"][i]] if l > 0 else []), writes=[xb])
                for c in range(8):
                    kb.op("pe", lambda e, c=c, xt=xt: e.transpose(out=PS[c // 4][:, (c % 4) * 128:(c % 4 + 1) * 128], in_=xt[:, c * 128:(c + 1) * 128], identity=ident),
                          reads=[xb, cstB], writes=[PB[c // 4]])
                ht, hb = hT.next()
                for c in range(8):
                    kb.op("act", lambda e, c=c, ht=ht, w=w: e.activation(out=ht[:, c, :], in_=PS[c // 4][:, (c % 4) * 128:(c % 4 + 1) * 128], func=AF.Identity,
                                                                      scale=modS[:, w, 1, c:c + 1], bias=modS[:, w, 0, c:c + 1]),
                          reads=[PB[c // 4], modSB], writes=[hb])
                qt, qb = qk.next()
                vt, vbb = vb.next()
                pt, pjb = pj.next()
                for n, (a, b) in enumerate(chunks):
                    pb = 2 + (n % 6)
                    for c in range(8):
                        kb.op("pe", lambda e, c=c, ht=ht, pb=pb, a=a, b=b: e.matmul(PS[pb][:, 0:b - a], lhsT=ht[:, c, :], rhs=win[:, c, a:b], start=(c == 0), stop=(c == 7)),
                              reads=[hb, winB], writes=[PB[pb]])
                    if n < 2:
                        kb.op("act", lambda e, n=n, pb=pb, qt=qt: e.activation(out=qt[:, n * 512:(n + 1) * 512], in_=PS[pb][:, :], func=AF.Identity), reads=[PB[pb]], writes=[qb])
                    elif n == 2:
                        kb.op("dve", lambda e, pb=pb, vt=vt: e.tensor_copy(out=vt[:], in_=PS[pb][:, :]), reads=[PB[pb]], writes=[vbb])
                    else:
                        eng = "dve" if n % 2 == 0 else "act"
                        if eng == "dve":
                            kb.op("dve", lambda e, pb=pb, pt=pt, a=a, b=b: e.tensor_copy(out=pt[:, a - 1536:b - 1536], in_=PS[pb][:, 0:b - a]), reads=[PB[pb]], writes=[pjb])
                        else:
                            kb.op("act", lambda e, pb=pb, pt=pt, a=a, b=b: e.activation(out=pt[:, a - 1536:b - 1536], in_=PS[pb][:, 0:b - a], func=AF.Identity), reads=[PB[pb]], writes=[pjb])
                kb.dma("sp", VV[i * 128:(i + 1) * 128, :], vt[:], reads=[vbb], writes=[dB["VV"][i]])
                kb.dma("sp", RKV[pr0:pr0 + 128, :], pt[:, 0:768], reads=[pjb], writes=[dB["RKV"][i]])
                kb.dma("sp", LORA[i * 128:(i + 1) * 128, :], pt[:, 768:960], reads=[pjb], writes=[dB["LORA"][i]])
                kb.dma("sp", CIN[pr0:pr0 + 128, :], pt[:, 960:1728], reads=[pjb], writes=[dB["CIN"][i]])
                src, srcb = qt, qb
                if i >= 2:
                    rt, rb = rp.next()
                    kb.dma("sp", rt[:], I["rope"][(i - 2) * 128:(i - 1) * 128, :], writes=[rb])
                    q1, q1b = qkr.next()
                    q2, q2b = qkt.next()
                    qv = qt[:].rearrange("p (g a h n) -> p g a h n", g=16, a=2, h=2)
                    kb.op("dve", lambda e, qt=qt, q1=q1, rt=rt: e.tensor_tensor(out=q1[:].rearrange("p (g n) -> p g n", g=16), in0=qt[:].rearrange("p (g n) -> p g n", g=16),
                                                                       in1=rt[:, 0:64].unsqueeze(1).to_broadcast([128, 16, 64]), op=ALU.mult), reads=[qb, rb], writes=[q1b])
                    q2v = q2[:].rearrange("p (g a h n) -> p g a h n", g=16, a=2, h=2)
                    sv = rt[:, 64:128].rearrange("p (a h n) -> p a h n", a=2, h=2)
                    for hh in range(2):
                        kb.op("pool", lambda e, hh=hh, qv=qv, q2v=q2v, sv=sv: e.tensor_tensor(out=q2v[:, :, :, hh, :], in0=qv[:, :, :, 1 - hh, :],
                                                                                     in1=sv[:, :, hh, :].unsqueeze(1).to_broadcast([128, 16, 2, 16]), op=ALU.mult),
                              reads=[qb, rb], writes=[q2b])
                    kb.op("dve", lambda e, q1=q1, q2=q2: e.tensor_tensor(out=q1[:], in0=q1[:], in1=q2[:], op=ALU.add), reads=[q1b, q2b], writes=[q1b])
                    src, srcb = q1, q1b
                for c in range(8):
                    kb.op("pe", lambda e, c=c, src=src: e.transpose(out=PS[c // 4][:, (c % 4) * 128:(c % 4 + 1) * 128], in_=src[:, c * 128:(c + 1) * 128], identity=ident),
                          reads=[srcb, cstB], writes=[PB[c // 4]])
                tt, tb = qkT.next()
                for hf in range(2):
                    kb.op("dve" if hf == 0 else "act",
                          (lambda e, tt=tt: e.tensor_copy(out=tt[:, 0:4, :], in_=PS[0][:, :].rearrange("p (c n) -> p c n", c=4))) if hf == 0 else
                          (lambda e, tt=tt: e.activation(out=tt[:, 4:8, :], in_=PS[1][:, :].rearrange("p (c n) -> p c n", c=4), func=AF.Identity)),
                          reads=[PB[hf]], writes=[tb])
                kb.dma("sp", QT[:, :, i * 128:(i + 1) * 128].rearrange("h p t -> p h t"), tt[:, 0:4, :], reads=[tb], writes=[dB["QK"][i]])
                kb.dma("sp", KT[:, :, i * 128:(i + 1) * 128].rearrange("h p t -> p h t"), tt[:, 4:8, :], reads=[tb], writes=[dB["QK"][i]])
        if upto in ("QT", "RKV", "VV"):
            break

        with kb.phase():
            lq = kb.sb([128, 4, 64], F32)
            lqB = Buf()
            for j, n in enumerate(("lam_q1", "lam_k1", "lam_q2", "lam_k2")):
                kb.dma("sp", lq[:, j, :], I[n][l, :].partition_broadcast(128), writes=[lqB])
            lt = kb.sb([128, 2, 64], F32)
            lv = kb.sb([128, 4], F32)
            lvB = Buf()
            kb.op("dve", lambda e: e.tensor_tensor(out=lt[:], in0=lq[:, 0:4:2, :], in1=lq[:, 1:4:2, :], op=ALU.mult), reads=[lqB], writes=[lvB])
            kb.op("dve", lambda e: e.tensor_reduce(out=lv[:, 0:2], in_=lt[:], axis=AX.X, op=ALU.add), reads=[lvB], writes=[lvB])
            kb.op("act", lambda e: e.activation(out=lv[:, 0:2], in_=lv[:, 0:2], func=AF.Exp), reads=[lvB], writes=[lvB])
            kb.op("dve", lambda e: e.tensor_tensor(out=lv[:, 2:3], in0=lv[:, 1:2], in1=lv[:, 0:1], op=ALU.subtract), reads=[lvB], writes=[lvB])
            kb.op("dve", lambda e: e.tensor_scalar(out=lv[:, 3:4], in0=lv[:, 2:3], scalar1=-lam_init, scalar2=None, op0=ALU.add), reads=[lvB], writes=[lvB])
            nlam = lv[:, 3:4]
            sg = kb.sb([128, 128], F32)
            sgB = Buf()
            kb.dma("sp", sg[:], I["subln_g"][l, :].partition_broadcast(128), writes=[sgB])
            kb.op("dve", lambda e: e.tensor_scalar(out=sg[:], in0=sg[:], scalar1=(1.0 - lam_init), scalar2=None, op0=ALU.mult), reads=[sgB], writes=[sgB])
            qTt = kb.sb([128, T], BF16)
            kTt = kb.sb([128, T], BF16)
            vaug = kb.sb([128, NT, 129], BF16)
            qkvB = Buf()
            kb.op("pool", lambda e: e.memset(vaug[:], 1.0), writes=[qkvB])
            PTring = Ring(kb, 4, [128, NT, 512], BF16)
            att = Ring(kb, 2, [128, 128], F32)
            sm = Ring(kb, 2, [128, 8], F32)
            sqt = Ring(kb, 2, [128, 128], F32)
            sbank = [0]
            for h in range(4):
                kb.dma("sp", qTt[:], QT[h, :, :], reads=dB["QK"][0:NT], writes=[qkvB])
                kb.dma("sp", kTt[:], KT[h, :, :], reads=dB["QK"][0:NT], writes=[qkvB])
                kb.dma("sp", vaug[:, :, 0:128], VV[:, h * 128:(h + 1) * 128].rearrange("(n p) d -> p n d", p=128), reads=dB["VV"][0:NT], writes=[qkvB])
                blocks = [(0, 256, [0, 1])] + [(256 + 512 * j, 512, list(range(NT))) for j in range(8)]
                prevB = None

                def merge_emit(A, Bp):
                    kb.rec = None
                    nA = max(len(A), 1)
                    nB = len(Bp) if Bp else 0
                    jb = 0
                    for ia, (kind, a_) in enumerate(A):
                        (kb.op if kind == "op" else kb.dma)(*a_)
                        if ia % 2 == 1 or ia == len(A) - 1:
                            tgt = nB * (ia + 1) // nA
                            while jb < tgt:
                                kind2, b_ = Bp[jb]
                                (kb.op if kind2 == "op" else kb.dma)(*b_)
                                jb += 1
                    while jb < nB:
                        kind2, b_ = Bp[jb]
                        (kb.op if kind2 == "op" else kb.dma)(*b_)
                        jb += 1

                for (q0, nq, kts) in blocks:
                    kb.rec = []
                    PTs, PTB = [None, None], [None, None]
                    for m in range(2):
                        PTs[m], PTB[m] = PTring.next()
                    for ki, kt in enumerate(kts):
                        for m in range(2):
                            pb = sbank[0] % 4
                            sbank[0] += 1
                            kb.op("pe", lambda e, pb=pb, m=m, kt=kt, q0=q0, nq=nq: e.matmul(PS[pb][:, 0:nq], lhsT=kTt[m * 64:(m + 1) * 64, kt * 128:(kt + 1) * 128],
                                                                                     rhs=qTt[m * 64:(m + 1) * 64, q0:q0 + nq], start=True, stop=True),
                                  reads=[qkvB], writes=[PB[pb]])
                            kb.op("act", lambda e, pb=pb, pt_=PTs[m], ki=ki, nq=nq: e.activation(out=pt_[:, ki, 0:nq], in_=PS[pb][:, 0:nq], func=AF.Exp, scale=0.125),
                                  reads=[PB[pb]], writes=[PTB[m]])
                    recA = kb.rec
                    kb.rec = []
                    for qs in range(nq // 128):
                        for m in range(2):
                            ob = 4 + 2 * (qs % 2) + m
                            for ki, kt in enumerate(kts):
                                kb.op("pe", lambda e, ob=ob, pt_=PTs[m], ki=ki, kt=kt, qs=qs, n=len(kts): e.matmul(PS[ob][:, 0:129], lhsT=pt_[:, ki, qs * 128:(qs + 1) * 128], rhs=vaug[:, kt, :],
                                                                                            start=(ki == 0), stop=(ki == n - 1)),
                                      reads=[PTB[m], qkvB], writes=[PB[ob]])
                        o0 = 4 + 2 * (qs % 2)
                        o1 = o0 + 1
                        st_, sB = sm.next()
                        at, aB = att.next()
                        sq_, sqB = sqt.next()
                        kb.op("dve", lambda e, st_=st_, o0=o0: e.reciprocal(out=st_[:, 0:1], in_=PS[o0][:, 128:129]), reads=[PB[o0]], writes=[sB])
                        kb.op("dve", lambda e, st_=st_, o1=o1: e.reciprocal(out=st_[:, 1:2], in_=PS[o1][:, 128:129]), reads=[PB[o1]], writes=[sB])
                        kb.op("dve", lambda e, st_=st_: e.tensor_tensor(out=st_[:, 2:3], in0=st_[:, 1:2], in1=nlam, op=ALU.mult), reads=[sB, lvB], writes=[sB])
                        kb.op("dve", lambda e, st_=st_, at=at, o0=o0: e.tensor_scalar(out=at[:], in0=PS[o0][:, 0:128], scalar1=st_[:, 0:1], scalar2=None, op0=ALU.mult),
                              reads=[PB[o0], sB], writes=[aB])
                        kb.op("dve", lambda e, st_=st_, at=at, o1=o1: e.scalar_tensor_tensor(out=at[:], in0=PS[o1][:, 0:128], scalar=st_[:, 2:3], in1=at[:], op0=ALU.mult, op1=ALU.add),
                              reads=[PB[o1], sB, aB], writes=[aB])
                        kb.op("pool", lambda e, at=at, sq_=sq_: e.tensor_tensor(out=sq_[:], in0=at[:], in1=at[:], op=ALU.mult), reads=[aB], writes=[sqB])
                        kb.op("dve", lambda e, st_=st_, sq_=sq_: e.tensor_reduce(out=st_[:, 3:4], in_=sq_[:], axis=AX.X, op=ALU.add), reads=[sqB], writes=[sB])
                        kb.op("dve", lambda e, st_=st_: e.tensor_scalar(out=st_[:, 4:5], in0=st_[:, 3:4], scalar1=1.0 / 128, scalar2=SUBLN_EPS, op0=ALU.mult, op1=ALU.add), reads=[sB], writes=[sB])
                        kb.op("act", lambda e, st_=st_: e.activation(out=st_[:, 5:6], in_=st_[:, 4:5], func=AF.Sqrt), reads=[sB], writes=[sB])
                        kb.op("dve", lambda e, st_=st_: e.reciprocal(out=st_[:, 6:7], in_=st_[:, 5:6]), reads=[sB], writes=[sB])
                        kb.op("dve", lambda e, st_=st_, at=at: e.scalar_tensor_tensor(out=at[:], in0=at[:], scalar=st_[:, 6:7], in1=sg[:], op0=ALU.mult, op1=ALU.mult),
                              reads=[sB, aB, sgB], writes=[aB])
                        t0 = q0 + qs * 128
                        kb.dma("sp", MIX[t0:t0 + 128, h * 128:(h + 1) * 128], at[:], reads=[aB], writes=[dB["MIX"][t0 // 128]])
                    recB = kb.rec
                    merge_emit(recA, prevB)
                    prevB = recB
                merge_emit([], prevB)

        with kb.phase():
            cw = kb.sb([128, 3, 256], F32)
            cwB = Buf()
            for j in range(3):
                kb.dma("sp", cw[:, j, :], I["conv_w"][l, j, :].partition_broadcast(128), writes=[cwB])
            c3 = Ring(kb, 2, [128, 3, 768], F32)
            u3 = Ring(kb, 2, [128, 3, 256], F32)
            co = Ring(kb, 2, [128, 256], F32)
            for i in kb.interleave(range(NT), 2):
                pr0 = prow(i * 128)
                ct, cB = c3.next()
                for j in range(3):
                    kb.dma("sp", ct[:, j, :], CIN[pr0 - 1 + j:pr0 - 1 + j + 128, :], reads=dB["CIN"][max(i - 1, 0):i + 2], writes=[cB])
                ut, uB = u3.next()
                ot, oB = co.next()
                kb.op("pool", lambda e, ct=ct, ut=ut: e.tensor_tensor(out=ut[:], in0=ct[:, :, 512:768], in1=ct[:, :, 0:256], op=ALU.mult), reads=[cB], writes=[uB])
                kb.op("dve", lambda e, ut=ut: e.tensor_tensor(out=ut[:], in0=ut[:], in1=cw[:], op=ALU.mult), reads=[uB, cwB], writes=[uB])
                kb.op("dve", lambda e, ut=ut, ot=ot: e.tensor_tensor(out=ot[:], in0=ut[:, 0, :], in1=ut[:, 1, :], op=ALU.add), reads=[uB], writes=[oB])
                kb.op("dve", lambda e, ut=ut, ot=ot: e.tensor_tensor(out=ot[:], in0=ot[:], in1=ut[:, 2, :], op=ALU.add), reads=[uB, oB], writes=[oB])
                kb.op("dve", lambda e, ct=ct, ot=ot: e.tensor_tensor(out=ot[:], in0=ot[:], in1=ct[:, 1, 256:512], op=ALU.mult), reads=[cB, oB], writes=[oB])
                kb.dma("sp", MIX[i * 128:(i + 1) * 128, 768:1024], ot[:], reads=[oB], writes=[dB["MIX"][i]])

        with kb.phase():
            cw3 = kb.sb([128, 3, 768], F32)
            cw3B = Buf()
            for j in range(3):
                kb.dma("sp", cw3[:, j, :], I["rkv_conv"][l, j, :].partition_broadcast(128), writes=[cw3B])
            vecs = kb.sb([128, 3, 256], F32)
            vecsB = Buf()
            for j, n in enumerate(("k_k", "k_a", "r_k")):
                kb.dma("sp", vecs[:, j, :], I[n][l, :].partition_broadcast(128), writes=[vecsB])
            wup = kb.sb([33, 2, 256], F32)
            aup = kb.sb([33, 2, 256], F32)
            gup = kb.sb([64, 256], F32)
            wB = Buf()
            for d in range(2):
                kb.dma("sp", wup[0:32, d, :], I["decay_up"][l, d, :, :], writes=[wB])
                kb.dma("sp", wup[32:33, d, :], I["decay_w0"][l, d:d + 1, :], writes=[wB])
                kb.dma("sp", aup[0:32, d, :], I["iclr_up"][l, d, :, :], writes=[wB])
                kb.dma("sp", aup[32:33, d, :], I["iclr_a0"][l, d:d + 1, :], writes=[wB])
            kb.dma("sp", gup[:], I["gate_up"][l, :, :], writes=[wB])
            lwl = Ring(kb, 2, [33, 2, 128], F32)
            lal = Ring(kb, 2, [33, 2, 128], F32)
            lgl = Ring(kb, 2, [64, 128], F32)
            for rg in (lwl, lal):
                for t_, b_ in zip(rg.t, rg.b):
                    kb.op("pool", lambda e, t_=t_: e.memset(t_[:], 1.0), writes=[b_])
            r3 = Ring(kb, 2, [128, 3, 768], F32)
            lo = Ring(kb, 2, [128, 192], F32)
            rkvr = Ring(kb, 2, [128, 768], F32)
            po = Ring(kb, 2, [128, 2564], F32)
            av = Ring(kb, 2, [128, 512], F32)
            tw = Ring(kb, 2, [128, 512], F32)
            krr = Ring(kb, 2, [128, 256], F32)
            t1r = Ring(kb, 2, [128, 256], F32)
            t2r = Ring(kb, 2, [128, 256], F32)
            smr = Ring(kb, 2, [128, 16], F32)
            for i in kb.interleave(range(NT), 2):
                pq = 4 * (i % 2)
                pr0 = prow(i * 128)
                rt, rB = r3.next()
                for j in range(3):
                    kb.dma("sp", rt[:, j, :], RKV[pr0 - 1 + j:pr0 - 1 + j + 128, :], reads=dB["RKV"][max(i - 1, 0):i + 2], writes=[rB])
                lt_, lB = lo.next()
                kb.dma("sp", lt_[:], LORA[i * 128:(i + 1) * 128, :], reads=[dB["LORA"][i]], writes=[lB])
                kv, kvB = rkvr.next()
                kb.op("pool", lambda e, rt=rt: e.tensor_tensor(out=rt[:], in0=rt[:], in1=cw3[:], op=ALU.mult), reads=[rB, cw3B], writes=[rB])
                kb.op("dve", lambda e, rt=rt, kv=kv: e.tensor_tensor(out=kv[:], in0=rt[:, 0, :], in1=rt[:, 1, :], op=ALU.add), reads=[rB], writes=[kvB])
                kb.op("dve", lambda e, rt=rt, kv=kv: e.tensor_tensor(out=kv[:], in0=kv[:], in1=rt[:, 2, :], op=ALU.add), reads=[rB, kvB], writes=[kvB])
                r_ = kv[:, 0:256]
                k_ = kv[:, 256:512]
                v_ = kv[:, 512:768]
                for j in range(4):
                    kb.op("pe", lambda e, pq=pq, j=j, lt_=lt_: e.transpose(out=PS[pq + 0][0:32, j * 128:(j + 1) * 128], in_=lt_[:, j * 32:(j + 1) * 32], identity=ident), reads=[lB, cstB], writes=[PB[pq + 0]])
                kb.op("pe", lambda e, pq=pq, lt_=lt_: e.transpose(out=PS[pq + 1][0:64, 0:128], in_=lt_[:, 128:192], identity=ident), reads=[lB, cstB], writes=[PB[pq + 1]])
                wl_, wlB = lwl.next()
                al_, alB = lal.next()
                gl_, glB = lgl.next()
                kb.op("act", lambda e, pq=pq, wl_=wl_: e.activation(out=wl_[0:32, :, :], in_=PS[pq + 0][0:32, 0:256].rearrange("p (d n) -> p d n", d=2), func=AF.Tanh), reads=[PB[pq + 0]], writes=[wlB])
                kb.op("act", lambda e, pq=pq, al_=al_: e.activation(out=al_[0:32, :, :], in_=PS[pq + 0][0:32, 256:512].rearrange("p (d n) -> p d n", d=2), func=AF.Identity), reads=[PB[pq + 0]], writes=[alB])
                kb.op("act", lambda e, pq=pq, gl_=gl_: e.activation(out=gl_[:], in_=PS[pq + 1][0:64, 0:128], func=AF.Sigmoid), reads=[PB[pq + 1]], writes=[glB])
                for d in range(2):
                    kb.op("pe", lambda e, pq=pq, d=d, wl_=wl_: e.matmul(PS[pq + 2][:, d * 256:(d + 1) * 256], lhsT=wl_[0:33, d, :], rhs=wup[0:33, d, :], start=True, stop=True), reads=[wlB, wB], writes=[PB[pq + 2]])
                    kb.op("pe", lambda e, pq=pq, d=d, al_=al_: e.matmul(PS[pq + 3][:, d * 256:(d + 1) * 256], lhsT=al_[0:33, d, :], rhs=aup[0:33, d, :], start=True, stop=True), reads=[alB, wB], writes=[PB[pq + 3]])
                kb.op("pe", lambda e, pq=pq, gl_=gl_: e.matmul(PS[pq + 1][:, 256:512], lhsT=gl_[:], rhs=gup[:], start=True, stop=True), reads=[glB, wB], writes=[PB[pq + 1]])
                pt, pB = po.next()
                a_, aB = av.next()
                w_, wwB = tw.next()
                kb.op("act", lambda e, pq=pq, w_=w_: e.activation(out=w_[:], in_=PS[pq + 2][:, :], func=AF.Sigmoid), reads=[PB[pq + 2]], writes=[wwB])
                kb.op("act", lambda e, pq=pq, a_=a_: e.activation(out=a_[:], in_=PS[pq + 3][:, :], func=AF.Sigmoid), reads=[PB[pq + 3]], writes=[aB])
                kb.op("act", lambda e, pq=pq, pt=pt: e.activation(out=pt[:, 2304:2560], in_=PS[pq + 1][:, 256:512], func=AF.Identity), reads=[PB[pq + 1]], writes=[pB])
                for d in range(2):
                    kb.op("dve", lambda e, d=d, w_=w_, pt=pt: e.tensor_scalar(out=pt[:, d * 1536:d * 1536 + 256], in0=w_[:, d * 256:(d + 1) * 256], scalar1=-0.6065306597126334, scalar2=None, op0=ALU.mult),
                          reads=[wwB], writes=[pB])
                kr, krB = krr.next()
                t1, t1B = t1r.next()
                t2, t2B = t2r.next()
                sm_, smB = smr.next()
                kb.op("dve", lambda e, kr=kr, k_=k_: e.tensor_tensor(out=kr[:], in0=k_, in1=vecs[:, 0, :], op=ALU.mult), reads=[kvB, vecsB], writes=[krB])
                kb.op("pool", lambda e, kr=kr, t1=t1: e.tensor_tensor(out=t1[:], in0=kr[:], in1=kr[:], op=ALU.mult), reads=[krB], writes=[t1B])
                kb.op("dve", lambda e, t1=t1, sm_=sm_: e.tensor_reduce(out=sm_[:, 0:4], in_=t1[:].rearrange("p (h n) -> p h n", h=4), axis=AX.X, op=ALU.add), reads=[t1B], writes=[smB])
                kb.op("dve", lambda e, sm_=sm_: e.tensor_scalar(out=sm_[:, 0:4], in0=sm_[:, 0:4], scalar1=1e-24, scalar2=None, op0=ALU.max), reads=[smB], writes=[smB])
                kb.op("act", lambda e, sm_=sm_: e.activation(out=sm_[:, 4:8], in_=sm_[:, 0:4], func=AF.Sqrt), reads=[smB], writes=[smB])
                kb.op("dve", lambda e, sm_=sm_: e.reciprocal(out=sm_[:, 8:12], in_=sm_[:, 4:8]), reads=[smB], writes=[smB])
                kb.op("dve", lambda e, sm_=sm_, kr=kr, pt=pt: e.tensor_tensor(out=pt[:, 768:1024].rearrange("p (h n) -> p h n", h=4), in0=kr[:].rearrange("p (h n) -> p h n", h=4),
                                                                       in1=sm_[:, 8:12].unsqueeze(2).to_broadcast([128, 4, 64]), op=ALU.mult), reads=[smB, krB], writes=[pB])
                kb.op("pool", lambda e, pt=pt, r_=r_: e.tensor_copy(out=pt[:, 1024:1280], in_=r_), reads=[kvB], writes=[pB])
                kb.op("pool", lambda e, pt=pt, v_=v_: e.tensor_copy(out=pt[:, 1280:1536], in_=v_), reads=[kvB], writes=[pB])
                for d in range(2):
                    ob = d * 1536
                    kb.op("dve", lambda e, d=d, a_=a_, t1=t1: e.scalar_tensor_tensor(out=t1[:], in0=a_[:, d * 256:(d + 1) * 256], scalar=-1.0, in1=vecs[:, 1, :], op0=ALU.add, op1=ALU.mult),
                          reads=[aB, vecsB, t1B], writes=[t1B])
                    kb.op("dve", lambda e, ob=ob, t1=t1, pt=pt, k_=k_: e.scalar_tensor_tensor(out=pt[:, ob + 512:ob + 768], in0=t1[:], scalar=1.0, in1=k_, op0=ALU.add, op1=ALU.mult),
                          reads=[t1B, kvB], writes=[pB])
                    kb.op("pool", lambda e, ob=ob, d=d, a_=a_, pt=pt: e.tensor_tensor(out=pt[:, ob + 256:ob + 512], in0=pt[:, 768:1024], in1=a_[:, d * 256:(d + 1) * 256], op=ALU.mult),
                          reads=[aB, pB], writes=[pB])
                kb.op("pool", lambda e, pt=pt, t2=t2: e.tensor_tensor(out=t2[:], in0=pt[:, 512:768], in1=pt[:, 2048:2304], op=ALU.add), reads=[pB], writes=[t2B])
                kb.op("pool", lambda e, t2=t2, r_=r_: e.tensor_tensor(out=t2[:], in0=t2[:], in1=r_, op=ALU.mult), reads=[kvB, t2B], writes=[t2B])
                kb.op("pool", lambda e, t2=t2: e.tensor_tensor(out=t2[:], in0=t2[:], in1=vecs[:, 2, :], op=ALU.mult), reads=[vecsB, t2B], writes=[t2B])
                kb.op("dve", lambda e, t2=t2, pt=pt: e.tensor_reduce(out=pt[:, 2560:2564], in_=t2[:].rearrange("p (h n) -> p h n", h=4), axis=AX.X, op=ALU.add), reads=[t2B], writes=[pB])
                kb.dma("sp", PREP[i * 128:(i + 1) * 128, :], pt[:], reads=[pB], writes=[dB["PREP"][i]])
        if upto == "PREP":
            break

        with kb.phase():
            idR = kb.sb([64, 64], F32R)
            idRB = Buf()
            kb.op("act", lambda e: e.activation(out=idR[:], in_=cst[0:64, 0:64], func=AF.Identity), reads=[cstB], writes=[idRB])
            id64 = cst[0:64, 0:64]

            def dir_gen(d):
                B0, B1, B2, B3 = 4 * d, 4 * d + 1, 4 * d + 2, 4 * d + 3
                ST = kb.sb([64, 4, 64], F32R)
                STB = Buf()
                chk = Ring(kb, 3, [64, 1536], F32)
                Er = Ring(kb, 2, [64, 3, 256], F32)
                HTr = Ring(kb, 2, [64, 4, 256], F32R)
                FMr = Ring(kb, 2, [64, 4, 4, 64], F32R)
                Gr = Ring(kb, 2, [64, 4, 2, 128], F32R)
                NTr = Ring(kb, 2, [64, 4, 64], F32R)
                Pmr = Ring(kb, 2, [64, 4, 64], F32R)
                Nar = Ring(kb, 2, [64, 4, 64], F32R)
                NTar = Ring(kb, 2, [64, 4, 64], F32R)
                Xr = Ring(kb, 2, [64, 4, 64], F32R)
                Ur = Ring(kb, 2, [64, 4, 64], F32R)
                Yr = Ring(kb, 2, [64, 256], F32)
                PCr = Ring(kb, 2, [64, 4], F32)
                PPr = Ring(kb, 2, [64, 256], F32)
                vRr = Ring(kb, 2, [64, 256], F32R)
                for h in range(4):
                    kb.op("act", lambda e, h=h: e.activation(out=ST[:, h, :], in_=cst[0:64, 0:64], func=AF.Identity, scale=0.0), reads=[cstB], writes=[STB])
                if d == 0:
                    o_lw, o_b, o_kd, o_kk, o_r, o_v, c0 = 0, 256, 512, 768, 1024, 1280, 0
                else:
                    o_kk, o_r, o_v, o_lw, o_b, o_kd, c0 = 0, 256, 512, 768, 1024, 1280, 768
                mask = cst[0:64, 128 + d * 128:256 + d * 128]
                maskT = cst[0:64, 384 + d * 64:448 + d * 64]
                tri = cst[0:64, 512 + d * 64:576 + d * 64]
                order = range(NCH) if d == 0 else ([3, 2, 1, 0] + list(range(NCH - 1, 3, -1)))
                for c in order:
                    ck, ckB = chk.next()
                    kb.dma("sp", ck[:], PREP[c * 64:(c + 1) * 64, c0:c0 + 1536], reads=[dB["PREP"][c // 2]], writes=[ckB])
                    lw = ck[:, o_lw:o_lw + 256]
                    kb.op("pe", lambda e: e.matmul(PS[B0][0:64, 0:256], lhsT=tri, rhs=lw, start=True, stop=True), reads=[ckB, cstB], writes=[PB[B0]])
                    for hf in range(4):
                        kb.op("pe", lambda e, hf=hf: e.matmul(PS[B0][0:64, 256 + 2 * hf:258 + 2 * hf], lhsT=lw[:, hf * 64:(hf + 1) * 64], rhs=cst[0:64, 640:642], start=True, stop=True),
                              reads=[ckB, cstB], writes=[PB[B0]])
                    yield
                    E, EB = Er.next()
                    PC, PCB = PCr.next()
                    kb.op("act", lambda e: e.activation(out=E[:, 0, :], in_=PS[B0][0:64, 0:256], func=AF.Exp), reads=[PB[B0]], writes=[EB])
                    kb.op("act", lambda e: e.activation(out=E[:, 1, :], in_=PS[B0][0:64, 0:256], func=AF.Exp, scale=-1.0), reads=[PB[B0]], writes=[EB])
                    kb.op("act", lambda e: e.activation(out=E[:, 2, :], in_=lw, func=AF.Exp, scale=-1.0), reads=[ckB], writes=[EB])
                    kb.op("act", lambda e: e.activation(out=PC[:], in_=PS[B0][0:64, 256:264:2], func=AF.Exp), reads=[PB[B0]], writes=[PCB])
                    vR, vRB = vRr.next()
                    kb.op("pool", lambda e: e.tensor_copy(out=vR[:], in_=ck[:, o_v:o_v + 256]), reads=[ckB], writes=[vRB])
                    yield
                    HT, HTB = HTr.next()
                    kb.op("dve", lambda e: e.tensor_tensor(out=HT[:, 0, :], in0=ck[:, o_b:o_b + 256], in1=E[:, 1, :], op=ALU.mult), reads=[ckB, EB], writes=[HTB])
                    kb.op("pool", lambda e: e.tensor_tensor(out=HT[:, 1, :], in0=ck[:, o_kd:o_kd + 256], in1=E[:, 1, :], op=ALU.mult), reads=[ckB, EB], writes=[HTB])
                    kb.op("dve", lambda e: e.scalar_tensor_tensor(out=HT[:, 2, :], in0=ck[:, o_kk:o_kk + 256], scalar=-1.0, in1=E[:, 2, :], op0=ALU.mult, op1=ALU.mult),
                          reads=[ckB, EB], writes=[HTB])
                    kb.op("pool", lambda e: e.tensor_tensor(out=HT[:, 3, :], in0=ck[:, o_r:o_r + 256], in1=E[:, 0, :], op=ALU.mult), reads=[ckB, EB], writes=[HTB])
                    yield
                    kb.op("dve", lambda e: e.tensor_tensor(out=HT[:, 2, :], in0=HT[:, 2, :].bitcast(F32), in1=E[:, 0, :], op=ALU.mult), reads=[EB, HTB], writes=[HTB])
                    yield
                    FM, FMB = FMr.next()
                    for hh in range(2):
                        for h in (2 * hh, 2 * hh + 1):
                            for q in range(4):
                                o = ((h % 2) * 4 + q) * 64
                                kb.op("pe", lambda e, q=q, h=h, o=o: e.transpose(out=PS[B1][0:64, o:o + 64], in_=HT[:, q, h * 64:(h + 1) * 64].bitcast(F32), identity=id64), reads=[HTB, cstB], writes=[PB[B1]])
                        yield
                        kb.op("act", lambda e, hh=hh: e.activation(out=FM[:, 2 * hh:2 * hh + 2, :, :].rearrange("p a q n -> p (a q n)"), in_=PS[B1][0:64, :], func=AF.Identity), reads=[PB[B1]], writes=[FMB])
                        yield
                    G, GB = Gr.next()
                    for hh in range(2):
                        for h in (2 * hh, 2 * hh + 1):
                            for g in range(2):
                                o = ((h % 2) * 2 + g) * 128
                                kb.op("pe", lambda e, h=h, g=g, o=o: e.matmul(PS[B2][0:64, o:o + 128], lhsT=FM[:, h, g, :], rhs=FM[:, h, 2:4, :].rearrange("p a n -> p (a n)"), start=True, stop=True),
                                      reads=[FMB], writes=[PB[B2]])
                        if hh == 0:
                            for h in range(4):
                                kb.op("pe", lambda e, h=h: e.matmul(PS[B3][0:64, h * 64:(h + 1) * 64], lhsT=FM[:, h, 2, :], rhs=FM[:, h, 0, :], start=True, stop=True), reads=[FMB], writes=[PB[B3]])
                        yield
                        kb.op("act", lambda e, hh=hh: e.activation(out=G[:, 2 * hh:2 * hh + 2, :, :].rearrange("p h g n -> p (h g n)"), in_=PS[B2][0:64, :], func=AF.Identity), reads=[PB[B2]], writes=[GB])
                        yield
                    NTt, NTB = NTr.next()
                    kb.op("act", lambda e: e.activation(out=NTt[:].rearrange("p h n -> p (h n)"), in_=PS[B3][0:64, 0:256], func=AF.Identity), reads=[PB[B3]], writes=[NTB])
                    kb.op("pool", lambda e: e.tensor_tensor(out=G[:].rearrange("p h g n -> p (h g) n"), in0=G[:].bitcast(F32).rearrange("p h g n -> p (h g) n"),
                                                           in1=mask.unsqueeze(1).to_broadcast([64, 8, 128]), op=ALU.mult), reads=[GB, cstB], writes=[GB])
                    yield
                    kb.op("dve", lambda e: e.tensor_tensor(out=NTt[:], in0=NTt[:].bitcast(F32), in1=maskT.unsqueeze(1).to_broadcast([64, 4, 64]), op=ALU.mult), reads=[NTB, cstB], writes=[NTB])
                    Pm, PmB = Pmr.next()
                    kb.op("pool", lambda e: e.tensor_tensor(out=Pm[:], in0=G[:, :, 0, 0:64].bitcast(F32), in1=id64.unsqueeze(1).to_broadcast([64, 4, 64]), op=ALU.add), reads=[GB, cstB], writes=[PmB])
                    yield
                    Nc = lambda h: G[:, h, 0, 0:64]
                    NTc = lambda h: NTt[:, h, :]
                    NcB, NTcB = GB, NTB
                    for s_ in range(5):
                        lastS = (s_ == 4)
                        Na, NaB = Nar.next()
                        NTa, NTaB = NTar.next()
                        if not lastS:
                            for h in range(4):
                                kb.op("pe", lambda e, h=h, Nc=Nc, NTc=NTc: e.matmul(PS[B1][0:64, h * 64:(h + 1) * 64], lhsT=NTc(h), rhs=Nc(h), start=True, stop=True), reads=[NcB, NTcB], writes=[PB[B1]])
                        for h in range(4):
                            kb.op("pe", lambda e, h=h, Nc=Nc, NTc=NTc: e.matmul(PS[B2][0:64, h * 64:(h + 1) * 64], lhsT=Nc(h), rhs=NTc(h), start=True, stop=True), reads=[NcB, NTcB], writes=[PB[B2]])
                        yield
                        kb.op("act", lambda e, NTa=NTa: e.activation(out=NTa[:].rearrange("p h n -> p (h n)"), in_=PS[B2][0:64, 0:256], func=AF.Identity), reads=[PB[B2]], writes=[NTaB])
                        if not lastS:
                            kb.op("act", lambda e, Na=Na: e.activation(out=Na[:].rearrange("p h n -> p (h n)"), in_=PS[B1][0:64, 0:256], func=AF.Identity), reads=[PB[B1]], writes=[NaB])
                        yield
                        for h in range(4):
                            kb.op("pe", lambda e, h=h, NTa=NTa: e.matmul(PS[B3][0:64, h * 64:(h + 1) * 64], lhsT=NTa[:, h, :], rhs=Pm[:, h, :], start=True, stop=True), reads=[NTaB, PmB], writes=[PB[B3]])
                        yield
                        PPt, PPB = PPr.next()
                        kb.op("act", lambda e, PPt=PPt: e.activation(out=PPt[:], in_=PS[B3][0:64, 0:256], func=AF.Identity), reads=[PB[B3]], writes=[PPB])
                        yield
                        kb.op("dve", lambda e, PPt=PPt: e.tensor_tensor(out=Pm[:].rearrange("p h n -> p (h n)"), in0=Pm[:].bitcast(F32).rearrange("p h n -> p (h n)"), in1=PPt[:], op=ALU.add),
                              reads=[PPB, PmB], writes=[PmB])
                        Nc = lambda h, Na=Na: Na[:, h, :]
                        NTc = lambda h, NTa=NTa: NTa[:, h, :]
                        NcB, NTcB = NaB, NTaB
                    yield
                    Xs, XB = Xr.next()
                    Us, UB = Ur.next()
                    Ys, YB = Yr.next()
                    vh = lambda h: vR[:, h * 64:(h + 1) * 64]
                    for h in range(4):
                        kb.op("pe", lambda e, h=h: e.matmul(PS[B1][0:64, h * 64:(h + 1) * 64], lhsT=FM[:, h, 2, :], rhs=ST[:, h, :], start=True, stop=False), reads=[FMB, STB], writes=[PB[B1]])
                        kb.op("pe", lambda e, h=h: e.matmul(PS[B1][0:64, h * 64:(h + 1) * 64], lhsT=G[:, h, 1, 0:64], rhs=vh(h), start=False, stop=True), reads=[GB, vRB], writes=[PB[B1]])
                    yield
                    kb.op("act", lambda e: e.activation(out=Xs[:].rearrange("p h n -> p (h n)"), in_=PS[B1][0:64, 0:256], func=AF.Identity), reads=[PB[B1]], writes=[XB])
                    yield
                    for h in range(4):
                        kb.op("pe", lambda e, h=h: e.matmul(PS[B2][0:64, h * 64:(h + 1) * 64], lhsT=Pm[:, h, :], rhs=Xs[:, h, :], start=True, stop=True), reads=[PmB, XB], writes=[PB[B2]])
                    yield
                    kb.op("act", lambda e: e.activation(out=Us[:].rearrange("p h n -> p (h n)"), in_=PS[B2][0:64, 0:256], func=AF.Identity), reads=[PB[B2]], writes=[UB])
                    yield
                    for h in range(4):
                        kb.op("pe", lambda e, h=h: e.matmul(PS[B3][0:64, h * 64:(h + 1) * 64], lhsT=FM[:, h, 3, :], rhs=ST[:, h, :], start=True, stop=False), reads=[FMB, STB], writes=[PB[B3]])
                        kb.op("pe", lambda e, h=h: e.matmul(PS[B3][0:64, h * 64:(h + 1) * 64], lhsT=G[:, h, 0, 64:128], rhs=Us[:, h, :], start=False, stop=False), reads=[GB, UB], writes=[PB[B3]])
                        kb.op("pe", lambda e, h=h: e.matmul(PS[B3][0:64, h * 64:(h + 1) * 64], lhsT=G[:, h, 1, 64:128], rhs=vh(h), start=False, stop=True), reads=[GB, vRB], writes=[PB[B3]])
                    for h in range(4):
                        kb.op("pe", lambda e, h=h: e.matmul(PS[B0][0:64, h * 64:(h + 1) * 64], lhsT=idR[:], rhs=ST[:, h, :], start=True, stop=False), reads=[STB, idRB], writes=[PB[B0]])
                        kb.op("pe", lambda e, h=h: e.matmul(PS[B0][0:64, h * 64:(h + 1) * 64], lhsT=HT[:, 0, h * 64:(h + 1) * 64], rhs=Us[:, h, :], start=False, stop=False), reads=[HTB, UB], writes=[PB[B0]])
                        kb.op("pe", lambda e, h=h: e.matmul(PS[B0][0:64, h * 64:(h + 1) * 64], lhsT=HT[:, 1, h * 64:(h + 1) * 64], rhs=vh(h), start=False, stop=True), reads=[HTB, vRB], writes=[PB[B0]])
                    yield
                    kb.op("act", lambda e: e.activation(out=Ys[:], in_=PS[B3][0:64, 0:256], func=AF.Identity), reads=[PB[B3]], writes=[YB])
                    for h in range(4):
                        kb.op("act", lambda e, h=h: e.activation(out=ST[:, h, :], in_=PS[B0][0:64, h * 64:(h + 1) * 64], func=AF.Identity, scale=PC[:, h:h + 1]), reads=[PB[B0], PCB], writes=[STB])
                    kb.dma("sp", YD[d, c * 64:(c + 1) * 64, :], Ys[:], reads=[YB], writes=[dB["YD%d" % d][c // 2]])
                    yield

            gens = [dir_gen(0), dir_gen(1)]
            while gens:
                for g_ in list(gens):
                    try:
                        next(g_)
                    except StopIteration:
                        gens.remove(g_)
        if upto == "YD":
            break

        with kb.phase():
            gnv = kb.sb([128, 2, 256], F32)
            gnB = Buf()
            kb.dma("sp", gnv[:, 0, :], I["gn_g"][l, :].partition_broadcast(128), writes=[gnB])
            kb.dma("sp", gnv[:, 1, :], I["gn_b"][l, :].partition_broadcast(128), writes=[gnB])
            yr = Ring(kb, 2, [128, 2, 256], F32)
            vr = Ring(kb, 2, [128, 256], F32)
            gr = Ring(kb, 2, [128, 260], F32)
            ycr = Ring(kb, 2, [128, 256], F32)
            sqr = Ring(kb, 2, [128, 256], F32)
            smr = Ring(kb, 2, [128, 16], F32)
            for i in kb.interleave(range(NT), 2):
                yt, yB = yr.next()
                vt_, vB = vr.next()
                gt, gB = gr.next()
                for d in range(2):
                    kb.dma("sp", yt[:, d, :], YD[d, i * 128:(i + 1) * 128, :], reads=[dB["YD%d" % d][i]], writes=[yB])
                kb.dma("sp", vt_[:], PREP[i * 128:(i + 1) * 128, 1280:1536], reads=[dB["PREP"][i]], writes=[vB])
                kb.dma("sp", gt[:], PREP[i * 128:(i + 1) * 128, 2304:2564], reads=[dB["PREP"][i]], writes=[gB])
                yc, ycB = ycr.next()
                sq_, sqB = sqr.next()
                sm_, smB = smr.next()
                v4 = lambda t_: t_.rearrange("p (h n) -> p h n", h=4)
                bc = lambda a_: a_.unsqueeze(2).to_broadcast([128, 4, 64])
                kb.op("dve", lambda e, yt=yt, yc=yc: e.tensor_tensor(out=yc[:], in0=yt[:, 0, :], in1=yt[:, 1, :], op=ALU.add), reads=[yB], writes=[ycB])
                kb.op("dve", lambda e, yc=yc, sm_=sm_: e.tensor_reduce(out=sm_[:, 0:4], in_=v4(yc[:]), axis=AX.X, op=ALU.add), reads=[ycB], writes=[smB])
                kb.op("dve", lambda e, sm_=sm_: e.tensor_scalar(out=sm_[:, 0:4], in0=sm_[:, 0:4], scalar1=-1.0 / 64, scalar2=None, op0=ALU.mult), reads=[smB], writes=[smB])
                kb.op("dve", lambda e, yc=yc, sm_=sm_: e.tensor_tensor(out=v4(yc[:]), in0=v4(yc[:]), in1=bc(sm_[:, 0:4]), op=ALU.add), reads=[smB, ycB], writes=[ycB])
                kb.op("pool", lambda e, yc=yc, sq_=sq_: e.tensor_tensor(out=sq_[:], in0=yc[:], in1=yc[:], op=ALU.mult), reads=[ycB], writes=[sqB])
                kb.op("dve", lambda e, sq_=sq_, sm_=sm_: e.tensor_reduce(out=sm_[:, 4:8], in_=v4(sq_[:]), axis=AX.X, op=ALU.add), reads=[sqB], writes=[smB])
                kb.op("dve", lambda e, sm_=sm_: e.tensor_scalar(out=sm_[:, 4:8], in0=sm_[:, 4:8], scalar1=1.0 / 64, scalar2=GN_EPS, op0=ALU.mult, op1=ALU.add), reads=[smB], writes=[smB])
                kb.op("act", lambda e, sm_=sm_: e.activation(out=sm_[:, 8:12], in_=sm_[:, 4:8], func=AF.Sqrt), reads=[smB], writes=[smB])
                kb.op("dve", lambda e, sm_=sm_: e.reciprocal(out=sm_[:, 12:16], in_=sm_[:, 8:12]), reads=[smB], writes=[smB])
                kb.op("dve", lambda e, yc=yc, sm_=sm_: e.tensor_tensor(out=v4(yc[:]), in0=v4(yc[:]), in1=bc(sm_[:, 12:16]), op=ALU.mult), reads=[smB, ycB], writes=[ycB])
                kb.op("dve", lambda e, yc=yc: e.tensor_tensor(out=yc[:], in0=yc[:], in1=gnv[:, 0, :], op=ALU.mult), reads=[gnB, ycB], writes=[ycB])
                kb.op("dve", lambda e, yc=yc: e.tensor_tensor(out=yc[:], in0=yc[:], in1=gnv[:, 1, :], op=ALU.add), reads=[gnB, ycB], writes=[ycB])
                kb.op("pool", lambda e, vt_=vt_, gt=gt, sq_=sq_: e.tensor_tensor(out=v4(sq_[:]), in0=v4(vt_[:]), in1=bc(gt[:, 256:260]), op=ALU.mult), reads=[vB, gB, sqB], writes=[sqB])
                kb.op("dve", lambda e, yc=yc, sq_=sq_: e.tensor_tensor(out=yc[:], in0=yc[:], in1=sq_[:], op=ALU.add), reads=[sqB, ycB], writes=[ycB])
                kb.op("dve", lambda e, yc=yc, gt=gt: e.tensor_tensor(out=yc[:], in0=yc[:], in1=gt[:, 0:256], op=ALU.mult), reads=[gB, ycB], writes=[ycB])
                kb.dma("sp", MIX[i * 128:(i + 1) * 128, 512:768], yc[:], reads=[ycB], writes=[dB["MIX"][i]])
        if upto == "MIX":
            break

        def layer_norm_tile(t_, tB, sq_, sqB, sm_, smB, lnp, lnpB):
            gi = 0
            kb.op("dve", lambda e: e.tensor_reduce(out=sm_[:, 0:1], in_=t_[:], axis=AX.X, op=ALU.add), reads=[tB], writes=[smB])
            kb.op("dve", lambda e: e.tensor_scalar(out=sm_[:, 1:2], in0=sm_[:, 0:1], scalar1=-1.0 / D, scalar2=None, op0=ALU.mult), reads=[smB], writes=[smB])
            kb.op("dve", lambda e: e.tensor_scalar(out=t_[:], in0=t_[:], scalar1=sm_[:, 1:2], scalar2=None, op0=ALU.add), reads=[smB, tB], writes=[tB])
            kb.op("pool", lambda e: e.tensor_tensor(out=sq_[:], in0=t_[:], in1=t_[:], op=ALU.mult), reads=[tB], writes=[sqB])
            kb.op("dve", lambda e: e.tensor_reduce(out=sm_[:, 2:3], in_=sq_[:], axis=AX.X, op=ALU.add), reads=[sqB], writes=[smB])
            kb.op("dve", lambda e: e.tensor_scalar(out=sm_[:, 3:4], in0=sm_[:, 2:3], scalar1=1.0 / D, scalar2=LN_EPS, op0=ALU.mult, op1=ALU.add), reads=[smB], writes=[smB])
            kb.op("act", lambda e: e.activation(out=sm_[:, 4:5], in_=sm_[:, 3:4], func=AF.Sqrt), reads=[smB], writes=[smB])
            kb.op("dve", lambda e: e.reciprocal(out=sm_[:, 5:6], in_=sm_[:, 4:5]), reads=[smB], writes=[smB])
            kb.op("dve", lambda e: e.scalar_tensor_tensor(out=t_[:], in0=t_[:], scalar=sm_[:, 5:6], in1=lnp[:, gi, :], op0=ALU.mult, op1=ALU.mult), reads=[smB, tB, lnpB], writes=[tB])
            kb.op("dve", lambda e: e.tensor_tensor(out=t_[:], in0=t_[:], in1=lnp[:, gi + 1, :], op=ALU.add), reads=[tB, lnpB], writes=[tB])

        with kb.phase():
            gate1, gate1B, ln1t, ln1B = load_gate_ln(0)
            wo = kb.sb([128, 8, D], BF16)
            woB = Buf()
            for c in range(8):
                kb.dma("pool", wo[:, c, :], I["w_out"][l, c * 128:(c + 1) * 128, :], writes=[woB])
            mr = Ring(kb, 2, [128, D], F32)
            mTr = Ring(kb, 2, [128, 8, 128], BF16)
            xr = Ring(kb, 2, [128, D], F32)
            tr_ = Ring(kb, 2, [128, D], F32)
            sqr = Ring(kb, 2, [128, D], F32)
            smr = Ring(kb, 2, [128, 8], F32)
            for i in kb.interleave(range(NT), 2):
                pq = 4 * (i % 2)
                w = 1 if i < 2 else 0
                mt, mB = mr.next()
                xt, xB = xr.next()
                kb.dma("sp", mt[:], MIX[i * 128:(i + 1) * 128, :], reads=[dB["MIX"][i]], writes=[mB])
                kb.dma("sp", xt[:], xsrc(l, i), reads=([dB["X"][i]] if l > 0 else []), writes=[xB])
                for c in range(8):
                    kb.op("pe", lambda e, pq=pq, c=c, mt=mt: e.transpose(out=PS[pq + c // 4][:, (c % 4) * 128:(c % 4 + 1) * 128], in_=mt[:, c * 128:(c + 1) * 128], identity=ident), reads=[mB, cstB], writes=[PB[pq + c // 4]])
                mT, mTB = mTr.next()
                kb.op("act", lambda e, pq=pq, mT=mT: e.activation(out=mT[:, 0:4, :].rearrange("p c n -> p (c n)"), in_=PS[pq][:, :], func=AF.Identity), reads=[PB[pq]], writes=[mTB])
                kb.op("dve", lambda e, pq=pq, mT=mT: e.tensor_copy(out=mT[:, 4:8, :].rearrange("p c n -> p (c n)"), in_=PS[pq + 1][:, :]), reads=[PB[pq + 1]], writes=[mTB])
                t_, tB = tr_.next()
                for hf in range(2):
                    for c in range(8):
                        kb.op("pe", lambda e, pq=pq, c=c, hf=hf, mT=mT: e.matmul(PS[pq + 2 + hf][:, :], lhsT=mT[:, c, :], rhs=wo[:, c, hf * 512:(hf + 1) * 512], start=(c == 0), stop=(c == 7)), reads=[mTB, woB], writes=[PB[pq + 2 + hf]])
                    kb.op("dve", lambda e, pq=pq, hf=hf, t_=t_, w=w: e.tensor_tensor(out=t_[:, hf * 512:(hf + 1) * 512], in0=PS[pq + 2 + hf][:, :], in1=gate1[:, w, hf * 512:(hf + 1) * 512], op=ALU.mult), reads=[PB[pq + 2 + hf], gate1B], writes=[tB])
                kb.op("dve", lambda e, t_=t_, xt=xt: e.scalar_tensor_tensor(out=t_[:], in0=xt[:], scalar=DN_ALPHA, in1=t_[:], op0=ALU.mult, op1=ALU.add), reads=[xB, tB], writes=[tB])
                sq_, sqB = sqr.next()
                sm_, smB = smr.next()
                layer_norm_tile(t_, tB, sq_, sqB, sm_, smB, ln1t, ln1B)
                kb.dma("sp", X1[i * 128:(i + 1) * 128, :], t_[:], reads=[tB], writes=[dB["X1"][i]])
        if upto == "X1":
            break

        with kb.phase():
            jj = l // 2
            moe = (l % 2 == 1)
            gate2, gate2B, ln2t, ln2B = load_gate_ln(1)
            if moe:
                experts = [(I["moe_w1"][jj, e], I["moe_w3"][jj, e], I["moe_w2"][jj, e]) for e in range(NE)]
            else:
                experts = [(I["ffn_w1"][jj, :, e * DFE:(e + 1) * DFE], I["ffn_w3"][jj, :, e * DFE:(e + 1) * DFE], I["ffn_w2"][jj, e * DFE:(e + 1) * DFE, :]) for e in range(2)]
            NFC = DFE // 128
            x1g = kb.sb([128, 4, D], F32)
            x1B = Buf()
            hT = kb.sb([128, 8, 512], BF16)
            hTB = Buf()
            hTfr = Ring(kb, 2, [128, 8, 128], F32)
            gTs = [kb.sb([128, NFC, 512], BF16) for _ in range(2)]
            gTBs = [Buf(), Buf()]
            acc = kb.sb([128, 4, D], F32)
            accB = Buf()
            w2e = Ring(kb, 2, [128, NFC, D], BF16)
            w1r = Ring(kb, 4, [128, 8, 128], BF16)
            w3r = Ring(kb, 4, [128, 8, 128], BF16)
            sar = Ring(kb, 2, [128, 512], F32)
            gates = kb.sb([128, 4, NE], F32)
            gatesB = Buf()
            rt = kb.sb([128, 4, 24], F32)
            rtB = Buf()
            sqr = Ring(kb, 2, [128, D], F32)
            smr = Ring(kb, 2, [128, 8], F32)
            if moe:
                rw = kb.sb([128, 8, NE], F32)
                rb = kb.sb([128, NE], F32)
                rwB = Buf()
                kb.dma("sp", rw[:], I["router_w"][jj].rearrange("(c p) e -> p c e", p=128), writes=[rwB])
                kb.dma("sp", rb[:], I["router_b"][jj, :].partition_broadcast(128), writes=[rwB])
            groups = [(0, 2)] + [(2 + 4 * g_, 4) for g_ in range(8)]
            for (i0, nt_) in groups:
                w = 1 if i0 < 2 else 0
                ng = nt_ * 128
                for ti in range(nt_):
                    i = i0 + ti
                    kb.dma("sp", x1g[:, ti, :], X1[i * 128:(i + 1) * 128, :], reads=[dB["X1"][i]], writes=[x1B])
                    for c in range(8):
                        kb.op("pe", lambda e, c=c, ti=ti: e.transpose(out=PS[c // 4][:, (c % 4) * 128:(c % 4 + 1) * 128], in_=x1g[:, ti, c * 128:(c + 1) * 128], identity=ident), reads=[x1B, cstB], writes=[PB[c // 4]])
                    hf_, hfB = hTfr.next()
                    for c in range(8):
                        kb.op("act", lambda e, c=c, hf_=hf_, w=w: e.activation(out=hf_[:, c, :], in_=PS[c // 4][:, (c % 4) * 128:(c % 4 + 1) * 128], func=AF.Identity,
                                                                          scale=modS[:, w, 3, c:c + 1], bias=modS[:, w, 2, c:c + 1]), reads=[PB[c // 4], modSB], writes=[hfB])
                    kb.op("pool", lambda e, hf_=hf_, ti=ti: e.tensor_copy(out=hT[:, :, ti * 128:(ti + 1) * 128], in_=hf_[:]), reads=[hfB], writes=[hTB])
                    if moe:
                        for c in range(8):
                            kb.op("pe", lambda e, c=c, hf_=hf_: e.matmul(PS[2][:, 0:NE], lhsT=hf_[:, c, :], rhs=rw[:, c, :], start=(c == 0), stop=(c == 7)), reads=[hfB, rwB], writes=[PB[2]])
                        r_ = rt[:, ti, :]
                        kb.op("dve", lambda e, r_=r_: e.tensor_tensor(out=r_[:, 0:8], in0=PS[2][:, 0:NE], in1=rb[:], op=ALU.add), reads=[PB[2], rwB], writes=[rtB])
                        kb.op("dve", lambda e, r_=r_: e.tensor_reduce(out=r_[:, 16:17], in_=r_[:, 0:8], axis=AX.X, op=ALU.max), reads=[rtB], writes=[rtB])
                        kb.op("dve", lambda e, r_=r_: e.tensor_scalar(out=r_[:, 8:16], in0=r_[:, 0:8], scalar1=r_[:, 16:17], scalar2=None, op0=ALU.is_equal), reads=[rtB], writes=[rtB])
                        kb.op("dve", lambda e, r_=r_: e.scalar_tensor_tensor(out=r_[:, 0:8], in0=r_[:, 8:16], scalar=-1e30, in1=r_[:, 0:8], op0=ALU.mult, op1=ALU.add), reads=[rtB], writes=[rtB])
                        kb.op("dve", lambda e, r_=r_: e.tensor_reduce(out=r_[:, 17:18], in_=r_[:, 0:8], axis=AX.X, op=ALU.max), reads=[rtB], writes=[rtB])
                        kb.op("dve", lambda e, r_=r_: e.tensor_scalar(out=r_[:, 0:8], in0=r_[:, 0:8], scalar1=r_[:, 17:18], scalar2=None, op0=ALU.is_equal), reads=[rtB], writes=[rtB])
                        kb.op("dve", lambda e, r_=r_: e.tensor_tensor(out=r_[:, 18:19], in0=r_[:, 16:17], in1=r_[:, 17:18], op=ALU.subtract), reads=[rtB], writes=[rtB])
                        kb.op("act", lambda e, r_=r_: e.activation(out=r_[:, 19:20], in_=r_[:, 18:19], func=AF.Sigmoid), reads=[rtB], writes=[rtB])
                        kb.op("act", lambda e, r_=r_: e.activation(out=r_[:, 20:21], in_=r_[:, 18:19], func=AF.Sigmoid, scale=-1.0), reads=[rtB], writes=[rtB])
                        kb.op("dve", lambda e, r_=r_: e.tensor_scalar(out=r_[:, 8:16], in0=r_[:, 8:16], scalar1=r_[:, 19:20], scalar2=None, op0=ALU.mult), reads=[rtB], writes=[rtB])
                        kb.op("dve", lambda e, r_=r_, ti=ti: e.scalar_tensor_tensor(out=gates[:, ti, :], in0=r_[:, 0:8], scalar=r_[:, 20:21], in1=r_[:, 8:16], op0=ALU.mult, op1=ALU.add), reads=[rtB], writes=[gatesB])
                for ei, (W1, W3, W2) in enumerate(experts):
                    gT, gTB = gTs[ei % 2], gTBs[ei % 2]
                    w2t, w2B = w2e.next()
                    kb.dma("pool", w2t[:], W2.rearrange("(c p) n -> p c n", p=128), writes=[w2B])
                    for fc in range(NFC):
                        w1t, w1B = w1r.next()
                        w3t, w3B = w3r.next()
                        kb.dma("pool", w1t[:], W1[:, fc * 128:(fc + 1) * 128].rearrange("(c p) f -> p c f", p=128), writes=[w1B])
                        kb.dma("pool", w3t[:], W3[:, fc * 128:(fc + 1) * 128].rearrange("(c p) f -> p c f", p=128), writes=[w3B])
                        pa = 2 + (fc % 2)
                        pbb = 4 + (fc % 2)
                        for c in range(8):
                            kb.op("pe", lambda e, c=c, w1t=w1t, ng=ng, pa=pa: e.matmul(PS[pa][:, 0:ng], lhsT=w1t[:, c, :], rhs=hT[:, c, 0:ng], start=(c == 0), stop=(c == 7)), reads=[w1B, hTB], writes=[PB[pa]])
                        for c in range(8):
                            kb.op("pe", lambda e, c=c, w3t=w3t, ng=ng, pbb=pbb: e.matmul(PS[pbb][:, 0:ng], lhsT=w3t[:, c, :], rhs=hT[:, c, 0:ng], start=(c == 0), stop=(c == 7)), reads=[w3B, hTB], writes=[PB[pbb]])
                        sa, saB = sar.next()
                        kb.op("act", lambda e, sa=sa, ng=ng, pa=pa: e.activation(out=sa[:, 0:ng], in_=PS[pa][:, 0:ng], func=AF.Silu), reads=[PB[pa]], writes=[saB])
                        kb.op("dve", lambda e, sa=sa, fc=fc, ng=ng, pbb=pbb: e.tensor_tensor(out=gT[:, fc, 0:ng], in0=PS[pbb][:, 0:ng], in1=sa[:, 0:ng], op=ALU.mult), reads=[PB[pbb], saB], writes=[gTB])
                    for ti in range(nt_):
                        for hf in range(2):
                            for fc in range(NFC):
                                kb.op("pe", lambda e, fc=fc, ti=ti, hf=hf, w2t=w2t: e.matmul(PS[6 + hf][:, :], lhsT=gT[:, fc, ti * 128:(ti + 1) * 128], rhs=w2t[:, fc, hf * 512:(hf + 1) * 512],
                                                                                     start=(fc == 0), stop=(fc == NFC - 1)), reads=[gTB, w2B], writes=[PB[6 + hf]])
                            a_ = acc[:, ti, hf * 512:(hf + 1) * 512]
                            if moe:
                                gsc = gates[:, ti, ei:ei + 1]
                                if ei == 0:
                                    kb.op("dve", lambda e, a_=a_, hf=hf, gsc=gsc: e.tensor_scalar(out=a_, in0=PS[6 + hf][:, :], scalar1=gsc, scalar2=None, op0=ALU.mult), reads=[PB[6 + hf], gatesB], writes=[accB])
                                else:
                                    kb.op("dve", lambda e, a_=a_, hf=hf, gsc=gsc: e.scalar_tensor_tensor(out=a_, in0=PS[6 + hf][:, :], scalar=gsc, in1=a_, op0=ALU.mult, op1=ALU.add),
                                          reads=[PB[6 + hf], gatesB, accB], writes=[accB])
                            else:
                                if ei == 0:
                                    kb.op("act", lambda e, a_=a_, hf=hf: e.activation(out=a_, in_=PS[6 + hf][:, :], func=AF.Identity), reads=[PB[6 + hf]], writes=[accB])
                                else:
                                    kb.op("dve", lambda e, a_=a_, hf=hf: e.tensor_tensor(out=a_, in0=PS[6 + hf][:, :], in1=a_, op=ALU.add), reads=[PB[6 + hf], accB], writes=[accB])
                for ti in range(nt_):
                    i = i0 + ti
                    if last and i < 2:
                        continue
                    a_ = acc[:, ti, :]
                    kb.op("dve", lambda e, a_=a_, w=w: e.tensor_tensor(out=a_, in0=a_, in1=gate2[:, w, :], op=ALU.mult), reads=[accB, gate2B], writes=[accB])
                    kb.op("dve", lambda e, a_=a_, ti=ti: e.scalar_tensor_tensor(out=a_, in0=x1g[:, ti, :], scalar=DN_ALPHA, in1=a_, op0=ALU.mult, op1=ALU.add), reads=[x1B, accB], writes=[accB])
                    sq_, sqB = sqr.next()
                    sm_, smB = smr.next()
                    layer_norm_tile(a_, accB, sq_, sqB, sm_, smB, ln2t, ln2B)
                    if last:
                        kb.dma("sp", OUT[(i - 2) * 128:(i - 1) * 128, :], a_, reads=[accB])
                    else:
                        kb.dma("sp", X[i * 128:(i + 1) * 128, :], a_, reads=[accB], writes=[dB["X"][i]])
        if upto == "X":
            break
        lstack.close()
        kb.stacks.pop()
    kb.barrier()
    return nc, dumps


def host_inputs(inputs, b):
    m = {n: np.ascontiguousarray(np.asarray(inputs[n], dtype=np.float32).reshape(s)) for n, s in PARAMS}
    m["x"] = np.ascontiguousarray(inputs["x"][b], dtype=np.float32)
    m["ctx"] = np.ascontiguousarray(inputs["ctx"][b], dtype=np.float32)
    cv = np.zeros((16, 128), np.float32)
    cv[0:8] = np.asarray(inputs["c"][b], np.float32).reshape(8, 128)
    cv[8:16] = np.asarray(inputs["c_ctx"], np.float32).reshape(8, 128)
    m["cvec"] = cv
    m["consts"] = make_consts()
    m["rope"] = make_rope()
    return m


def kernel(**inputs):
    nc, _ = build(n_layers=DEPTH, dump=None)
    base = {n: np.ascontiguousarray(np.asarray(inputs[n], dtype=np.float32).reshape(s)) for n, s in PARAMS}
    consts = make_consts()
    rope = make_rope()
    in_maps = []
    for b in range(8):
        m = dict(base)
        m["x"] = np.ascontiguousarray(np.asarray(inputs["x"][b], dtype=np.float32))
        m["ctx"] = np.ascontiguousarray(np.asarray(inputs["ctx"][b], dtype=np.float32))
        cv = np.zeros((16, 128), np.float32)
        cv[0:8] = np.asarray(inputs["c"][b], np.float32).reshape(8, 128)
        cv[8:16] = np.asarray(inputs["c_ctx"], np.float32).reshape(8, 128)
        m["cvec"] = cv
        m["consts"] = consts
        m["rope"] = rope
        in_maps.append(m)
    res = run_bass_kernel_spmd(nc, in_maps, core_ids=list(range(8)))
    out = np.stack([np.asarray(res.results[b]["out"], dtype=np.float32) for b in range(8)], 0)
    return out
```

```python
import math
import contextlib
import bisect
import numpy as np
import concourse.bass as bass
import concourse.mybir as mybir
from concourse.bass_utils import run_bass_kernel_spmd

F32 = mybir.dt.float32
BF16 = mybir.dt.bfloat16
F32R = mybir.dt.float32r
ALU = mybir.AluOpType
AF = mybir.ActivationFunctionType
AX = mybir.AxisListType

D = 1024
TC = 256
TL = 4096
T = TC + TL
NT = T // 128
DEPTH = 4
INW = 3264
DFF = 2816
DFE = 1408
NE = 8
DN_ALPHA = (2 * DEPTH) ** 0.25
LN_EPS = 1e-5
GN_EPS = 64e-5
SUBLN_EPS = 1e-5
CH = 64
NCH = T // CH
PR = T + 3
import os
SCAN_LIMIT = int(os.environ.get('SCAN_LIMIT', '0'))
STAGE = int(os.environ.get('STAGE', '99'))
SUB = int(os.environ.get('SUB', '0'))


def prow(tok):
    return 1 + tok if tok < TC else 2 + tok


class Buf:
    __slots__ = ("w", "r", "excl")

    def __init__(self, excl=False):
        self.w = None
        self.r = {}
        self.excl = excl


class KB:
    KD = 8

    def __init__(self, nc):
        self.nc = nc
        self.eng = {"pe": nc.tensor, "act": nc.scalar, "dve": nc.vector, "pool": nc.gpsimd, "sp": nc.sync}
        self.sem = {e: nc.alloc_semaphore("s_" + e) for e in self.eng}
        self.cnt = {e: 0 for e in self.eng}
        self.ins = {e: [] for e in self.eng}
        self.sigi = {e: [] for e in self.eng}
        self.seen = {e: {} for e in self.eng}
        self.dsem = {q: [nc.alloc_semaphore("d_%s%d" % (q, i)) for i in range(self.KD)] for q in ("sp", "pool", "act")}
        self.duse = {q: [0] * self.KD for q in self.dsem}
        self.di = {q: 0 for q in self.dsem}
        self.nsb = 0
        self.rec = None
        self.stacks = [contextlib.ExitStack()]

    def sb(self, shape, dt=F32, name=None):
        self.nsb += 1
        return self.stacks[-1].enter_context(self.nc.sbuf_tensor("t%d" % self.nsb, list(shape), dt))

    @contextlib.contextmanager
    def phase(self):
        self.stacks.append(contextlib.ExitStack())
        try:
            yield
        finally:
            self.barrier()
            self.stacks.pop().close()

    def wait(self, e, ev):
        if ev is None:
            return
        sem, val, key = ev
        if key == "pe" and e == "pe":
            return
        if sem is None:
            sl = self.sigi[key]
            j = bisect.bisect_left(sl, val)
            if j == len(sl):
                self.ins[key][val - 1].then_inc(self.sem[key], 1)
                sl.append(val)
            val = j + 1
            sem = self.sem[key]
        if self.seen[e].get(key, 0) >= val:
            return
        self.eng[e].wait_ge(sem, val)
        self.seen[e][key] = val

    def _deps(self, e, reads, writes):
        for b in reads:
            self.wait(e, b.w)
            if b.excl:
                for k_, ev in b.r.items():
                    if k_ != e:
                        self.wait(e, ev)
        for b in writes:
            self.wait(e, b.w)
            for ev in b.r.values():
                self.wait(e, ev)

    def _post(self, ev, reads, writes):
        for b in reads:
            b.r[ev[2]] = ev
        for b in writes:
            b.w = ev
            b.r = {}

    def interleave(self, it, k=2):
        items = list(it)
        for j in range(0, len(items), k):
            recs = []
            for x in items[j:j + k]:
                self.rec = []
                yield x
                recs.append(self.rec)
            self.rec = None
            for t in range(max(len(r) for r in recs)):
                for r in recs:
                    if t < len(r):
                        kind, a = r[t]
                        if kind == "op":
                            self.op(*a)
                        else:
                            self.dma(*a)

    def op(self, e, fn, reads=(), writes=()):
        if self.rec is not None:
            self.rec.append(("op", (e, fn, tuple(reads), tuple(writes))))
            return None
        self._deps(e, reads, writes)
        ins = fn(self.eng[e])
        self.cnt[e] += 1
        self.ins[e].append(ins)
        ev = (None, self.cnt[e], e)
        self._post(ev, reads, writes)
        return ev

    def dma(self, q, out, in_, reads=(), writes=()):
        if self.rec is not None:
            self.rec.append(("dma", (q, out, in_, tuple(reads), tuple(writes))))
            return None
        self._deps(q, reads, writes)
        k = self.di[q] % self.KD
        self.di[q] += 1
        key = "d_%s%d" % (q, k)
        sem = self.dsem[q][k]
        if self.duse[q][k] > 0:
            self.wait(q, (sem, 16 * self.duse[q][k], key))
        ins = self.eng[q].dma_start(out=out, in_=in_)
        self.duse[q][k] += 1
        ins.then_inc(sem, 16)
        ev = (sem, 16 * self.duse[q][k], key)
        self._post(ev, reads, writes)
        return ev

    def barrier(self):
        evs = [(None, self.cnt[e], e) for e in self.eng if self.cnt[e] > 0]
        for q in self.dsem:
            for k in range(self.KD):
                if self.duse[q][k] > 0:
                    evs.append((self.dsem[q][k], 16 * self.duse[q][k], "d_%s%d" % (q, k)))
        for e in self.eng:
            for ev in evs:
                self.wait(e, ev)


class Ring:
    def __init__(self, kb, n, shape, dt=F32):
        self.t = [kb.sb(shape, dt) for _ in range(n)]
        self.b = [Buf() for _ in range(n)]
        self.i = 0

    def next(self):
        j = self.i % len(self.t)
        self.i += 1
        return self.t[j], self.b[j]


def make_consts():
    c = np.zeros((128, 1024), np.float32)
    c[:, 0:128] = np.eye(128, dtype=np.float32)
    ii = np.arange(CH)
    for d in range(2):
        before = (ii[:, None] > ii[None, :]) if d == 1 else (ii[:, None] < ii[None, :])
        beq = before | np.eye(CH, dtype=bool)
        c[0:CH, 128 + d * 128:128 + d * 128 + 64] = before
        c[0:CH, 128 + d * 128 + 64:128 + d * 128 + 128] = beq
        c[0:CH, 384 + d * 64:384 + d * 64 + 64] = before.T
        c[0:CH, 512 + d * 64:512 + d * 64 + 64] = beq
    c[:, 640:768] = 1.0
    c[0, 768:896] = 1.0
    c[1, 896:1024] = 1.0
    return c


def make_rope():
    rows = TL // 64
    row = np.repeat(np.arange(rows), 64).astype(np.float64)
    col = np.tile(np.arange(64), rows).astype(np.float64)
    inv = 10000.0 ** (-np.arange(0, 32, 2, dtype=np.float64) / 32)
    ar = row[:, None] * inv
    ac = col[:, None] * inv
    cosT = np.concatenate([np.cos(ar), np.cos(ar), np.cos(ac), np.cos(ac)], 1)
    sinT = np.concatenate([-np.sin(ar), np.sin(ar), -np.sin(ac), np.sin(ac)], 1)
    return np.concatenate([cosT, sinT], 1).astype(np.float32)


PARAMS = [("w_ada", [DEPTH, D, 6 * D]), ("b_ada", [DEPTH, 6 * D]), ("w_in", [DEPTH, D, INW]), ("w_out", [DEPTH, D, D]),
          ("ln1_g", [DEPTH, D]), ("ln1_b", [DEPTH, D]), ("ln2_g", [DEPTH, D]), ("ln2_b", [DEPTH, D]),
          ("lam_q1", [DEPTH, 64]), ("lam_k1", [DEPTH, 64]), ("lam_q2", [DEPTH, 64]), ("lam_k2", [DEPTH, 64]),
          ("subln_g", [DEPTH, 128]), ("rkv_conv", [DEPTH, 3, 768]), ("decay_w0", [DEPTH, 2, 256]),
          ("decay_up", [DEPTH, 2, 32, 256]), ("iclr_a0", [DEPTH, 2, 256]), ("iclr_up", [DEPTH, 2, 32, 256]),
          ("gate_up", [DEPTH, 64, 256]), ("k_k", [DEPTH, 256]), ("k_a", [DEPTH, 256]), ("r_k", [DEPTH, 256]),
          ("gn_g", [DEPTH, 256]), ("gn_b", [DEPTH, 256]), ("conv_w", [DEPTH, 3, 256]),
          ("ffn_w1", [2, D, DFF]), ("ffn_w3", [2, D, DFF]), ("ffn_w2", [2, DFF, D]),
          ("router_w", [2, D, NE]), ("router_b", [2, NE]),
          ("moe_w1", [2, NE, D, DFE]), ("moe_w3", [2, NE, D, DFE]), ("moe_w2", [2, NE, DFE, D])]


def build(n_layers=DEPTH, dump=None):
    nc = bass.Bass("TRN2", target_bir_lowering=False)
    kb = KB(nc)
    I = {}
    I["x"] = nc.dram_tensor("x", [TL, D], F32, kind="ExternalInput").ap()
    I["ctx"] = nc.dram_tensor("ctx", [TC, D], F32, kind="ExternalInput").ap()
    I["cvec"] = nc.dram_tensor("cvec", [16, 128], F32, kind="ExternalInput").ap()
    I["consts"] = nc.dram_tensor("consts", [128, 1024], F32, kind="ExternalInput").ap()
    I["rope"] = nc.dram_tensor("rope", [TL, 128], F32, kind="ExternalInput").ap()
    for n, s in PARAMS:
        I[n] = nc.dram_tensor(n, s, F32, kind="ExternalInput").ap()
    OUT = nc.dram_tensor("out", [TL, D], F32, kind="ExternalOutput").ap()
    dumps = {}

    def scratch(name, shape, dt=F32):
        if dump and name in dump:
            dumps[name] = nc.dram_tensor("dump_" + name, shape, dt, kind="ExternalOutput").ap()
            return dumps[name]
        return nc.dram_tensor("scr_" + name, shape, dt).ap()

    X = scratch("X", [T, D])
    X1 = scratch("X1", [T, D])
    QT = scratch("QT", [4, 128, T], BF16)
    KT = scratch("KT", [4, 128, T], BF16)
    VV = scratch("VV", [T, 512], BF16)
    RKV = scratch("RKV", [PR, 768])
    CIN = scratch("CIN", [PR, 768])
    LORA = scratch("LORA", [T, 192])
    MIX = scratch("MIX", [T, D])
    PREP = scratch("PREP", [T, 2564])
    YD = scratch("YD", [2, T, 256])
    MODD = scratch("MODD", [2, 6 * D])
    dB = {n: [Buf() for _ in range(NT + 2)] for n in ("X", "X1", "QK", "VV", "RKV", "CIN", "LORA", "MIX", "PREP", "YD0", "YD1")}

    cst = kb.sb([128, 1024], F32, "cst")
    cstB = Buf()
    kb.dma("sp", cst[:], I["consts"][:, :], writes=[cstB])
    ident = cst[:, 0:128]
    zrow = kb.sb([1, 768], F32, "zrow")
    zB = Buf()
    kb.op("pool", lambda e: e.memset(zrow[:], 0.0), writes=[zB])
    for r_ in (0, TC + 1, PR - 1):
        kb.dma("sp", RKV[r_:r_ + 1, :], zrow[:], reads=[zB])
        kb.dma("sp", CIN[r_:r_ + 1, :], zrow[:], reads=[zB])
    PS = [nc.alloc_psum_tensor("ps%d" % i, [128, 512], F32) for i in range(8)]
    PB = [Buf(excl=True) for _ in range(8)]
    kb.barrier()

    def xsrc(l, i):
        if l == 0:
            return I["ctx"][i * 128:(i + 1) * 128, :] if i < 2 else I["x"][(i - 2) * 128:(i - 1) * 128, :]
        return X[i * 128:(i + 1) * 128, :]


    upto = dump[0] if dump else None
    stop = [False]

    for l in range(n_layers):
        last = (l == DEPTH - 1)
        lam_init = 0.8 - 0.6 * math.exp(-0.3 * l)
        lstack = contextlib.ExitStack()
        kb.stacks.append(lstack)
        modS = kb.sb([128, 2, 4, 8], F32)
        modSB = Buf()
        mrowDB = Buf()

        def load_gate_ln(which):
            gB_ = kb.sb([128, 2, D], F32)
            gBB_ = Buf()
            ln_ = kb.sb([128, 2, D], F32)
            lnB_ = Buf()
            v = 2 if which == 0 else 5
            for w_ in range(2):
                kb.dma("sp", gB_[:, w_, :], MODD[w_, v * D:(v + 1) * D].partition_broadcast(128), reads=[mrowDB], writes=[gBB_])
            for j, n in enumerate((("ln1_g", "ln1_b") if which == 0 else ("ln2_g", "ln2_b"))):
                kb.dma("sp", ln_[:, j, :], I[n][l, :].partition_broadcast(128), writes=[lnB_])
            return gB_, gBB_, ln_, lnB_

        with kb.phase():
            cv = kb.sb([16, 128], F32)
            cvB = Buf()
            kb.dma("sp", cv[:], I["cvec"][:, :], writes=[cvB])
            s2 = kb.sb([128, 8, 2], F32)
            s2B = Buf()
            kb.op("pe", lambda e: e.transpose(out=PS[0][:, 0:16], in_=cv[:], identity=cst[0:16, 0:16]), reads=[cvB, cstB], writes=[PB[0]])
            kb.op("act", lambda e: e.activation(out=s2[:].rearrange("p c w -> p w c"), in_=PS[0][:, 0:16].rearrange("p (w c) -> p w c", w=2), func=AF.Silu),
                  reads=[PB[0]], writes=[s2B])
            wr = Ring(kb, 2, [128, 8, 512], F32)
            ba = kb.sb([2, 6 * D], F32)
            baB = Buf()
            kb.dma("sp", ba[0:1, :], I["b_ada"][l:l + 1, :], writes=[baB])
            kb.dma("sp", ba[1:2, :], I["b_ada"][l:l + 1, :], writes=[baB])
            mrow = kb.sb([2, 6 * D], F32)
            mrowB = Buf()
            for n in range(12):
                wt, wb = wr.next()
                kb.dma("sp", wt[:], I["w_ada"][l, :, n * 512:(n + 1) * 512].rearrange("(c p) n -> p c n", p=128), writes=[wb])
                pb = 1 + (n % 2)
                for c in range(8):
                    kb.op("pe", lambda e, c=c, wt=wt, pb=pb: e.matmul(PS[pb][0:2, :], lhsT=s2[:, c, :], rhs=wt[:, c, :], start=(c == 0), stop=(c == 7)),
                          reads=[s2B, wb], writes=[PB[pb]])
                kb.op("dve", lambda e, n=n, pb=pb: e.tensor_tensor(out=mrow[:, n * 512:(n + 1) * 512], in0=PS[pb][0:2, :], in1=ba[:, n * 512:(n + 1) * 512], op=ALU.add),
                      reads=[PB[pb], baB], writes=[mrowB])
            for v in (1, 4):
                kb.op("dve", lambda e, v=v: e.tensor_scalar(out=mrow[:, v * D:(v + 1) * D], in0=mrow[:, v * D:(v + 1) * D], scalar1=1.0, scalar2=None, op0=ALU.add),
                      reads=[mrowB], writes=[mrowB])
            kb.dma("sp", MODD[:, :], mrow[:], reads=[mrowB], writes=[mrowDB])
            for j, v in enumerate((0, 1, 3, 4)):
                for c in range(8):
                    o = (j * 8 + c) * 2
                    kb.op("pe", lambda e, o=o, v=v, c=c: e.matmul(PS[3][:, o:o + 2], lhsT=mrow[0:2, v * D + c * 128:v * D + (c + 1) * 128],
                                                              rhs=cst[0:2, 0:2], start=True, stop=True), reads=[mrowB, cstB], writes=[PB[3]])
            kb.op("act", lambda e: e.activation(out=modS[:].rearrange("p w j c -> p j c w"), in_=PS[3][:, 0:64].rearrange("p (j c w) -> p j c w", j=4, c=8), func=AF.Identity),
                  reads=[PB[3]], writes=[modSB])
        if upto == "MODD":
            break

        with kb.phase():
            win = kb.sb([128, 8, INW], BF16)
            winB = Buf()
            for c in range(8):
                kb.dma("pool", win[:, c, :], I["w_in"][l, c * 128:(c + 1) * 128, :], writes=[winB])
            xr = Ring(kb, 2, [128, D], F32)
            hT = Ring(kb, 2, [128, 8, 128], BF16)
            qk = Ring(kb, 2, [128, 1024], F32)
            qkr = Ring(kb, 2, [128, 1024], F32)
            qkt = Ring(kb, 2, [128, 1024], F32)
            rp = Ring(kb, 2, [128, 128], F32)
            qkT = Ring(kb, 2, [128, 8, 128], BF16)
            vb = Ring(kb, 2, [128, 512], BF16)
            pj = Ring(kb, 2, [128, 1728], F32)
            chunks = [(0, 512), (512, 1024), (1024, 1536), (1536, 2048), (2048, 2496), (2496, 3008), (3008, 3264)]
            for i in range(NT):
                w = 1 if i < 2 else 0
                pr0 = prow(i * 128)
                xt, xb = xr.next()
                kb.dma("sp", xt[:], xsrc(l, i), reads=([dB["X"][i]] if l > 0 else []), writes=[xb])
                for c in range(8):
                    kb.op("pe", lambda e, c=c, xt=xt: e.transpose(out=PS[c // 4][:, (c % 4) * 128:(c % 4 + 1) * 128], in_=xt[:, c * 128:(c + 1) * 128], identity=ident),
                          reads=[xb, cstB], writes=[PB[c // 4]])
                ht, hb = hT.next()
                for c in range(8):
                    kb.op("act", lambda e, c=c, ht=ht, w=w: e.activation(out=ht[:, c, :], in_=PS[c // 4][:, (c % 4) * 128:(c % 4 + 1) * 128], func=AF.Identity,
                                                                      scale=modS[:, w, 1, c:c + 1], bias=modS[:, w, 0, c:c + 1]),
                          reads=[PB[c // 4], modSB], writes=[hb])
                qt, qb = qk.next()
                vt, vbb = vb.next()
                pt, pjb = pj.next()
                for n, (a, b) in enumerate(chunks):
                    pb = 2 + (n % 6)
                    for c in range(8):
                        kb.op("pe", lambda e, c=c, ht=ht, pb=pb, a=a, b=b: e.matmul(PS[pb][:, 0:b - a], lhsT=ht[:, c, :], rhs=win[:, c, a:b], start=(c == 0), stop=(c == 7)),
                              reads=[hb, winB], writes=[PB[pb]])
                    if n < 2:
                        kb.op("act", lambda e, n=n, pb=pb, qt=qt: e.activation(out=qt[:, n * 512:(n + 1) * 512], in_=PS[pb][:, :], func=AF.Identity), reads=[PB[pb]], writes=[qb])
                    elif n == 2:
                        kb.op("dve", lambda e, pb=pb, vt=vt: e.tensor_copy(out=vt[:], in_=PS[pb][:, :]), reads=[PB[pb]], writes=[vbb])
                    else:
                        eng = "dve" if n % 2 == 0 else "act"
                        if eng == "dve":
                            kb.op("dve", lambda e, pb=pb, pt=pt, a=a, b=b: e.tensor_copy(out=pt[:, a - 1536:b - 1536], in_=PS[pb][:, 0:b - a]), reads=[PB[pb]], writes=[pjb])
                        else:
                            kb.op("act", lambda e, pb=pb, pt=pt, a=a, b=b: e.activation(out=pt[:, a - 1536:b - 1536], in_=PS[pb][:, 0:b - a], func=AF.Identity), reads=[PB[pb]], writes=[pjb])
                kb.dma("sp", VV[i * 128:(i + 1) * 128, :], vt[:], reads=[vbb], writes=[dB["VV"][i]])
                kb.dma("sp", RKV[pr0:pr0 + 128, :], pt[:, 0:768], reads=[pjb], writes=[dB["RKV"][i]])
                kb.dma("sp", LORA[i * 128:(i + 1) * 128, :], pt[:, 768:960], reads=[pjb], writes=[dB["LORA"][i]])
                kb.dma("sp", CIN[pr0:pr0 + 128, :], pt[:, 960:1728], reads=[pjb], writes=[dB["CIN"][i]])
                src, srcb = qt, qb
                if i >= 2:
                    rt, rb = rp.next()
                    kb.dma("sp", rt[:], I["rope"][(i - 2) * 128:(i - 1) * 128, :], writes=[rb])
                    q1, q1b = qkr.next()
                    q2, q2b = qkt.next()
                    qv = qt[:].rearrange("p (g a h n) -> p g a h n", g=16, a=2, h=2)
                    kb.op("dve", lambda e, qt=qt, q1=q1, rt=rt: e.tensor_tensor(out=q1[:].rearrange("p (g n) -> p g n", g=16), in0=qt[:].rearrange("p (g n) -> p g n", g=16),
                                                                       in1=rt[:, 0:64].unsqueeze(1).to_broadcast([128, 16, 64]), op=ALU.mult), reads=[qb, rb], writes=[q1b])
                    q2v = q2[:].rearrange("p (g a h n) -> p g a h n", g=16, a=2, h=2)
                    sv = rt[:, 64:128].rearrange("p (a h n) -> p a h n", a=2, h=2)
                    for hh in range(2):
                        kb.op("pool", lambda e, hh=hh, qv=qv, q2v=q2v, sv=sv: e.tensor_tensor(out=q2v[:, :, :, hh, :], in0=qv[:, :, :, 1 - hh, :],
                                                                                     in1=sv[:, :, hh, :].unsqueeze(1).to_broadcast([128, 16, 2, 16]), op=ALU.mult),
                              reads=[qb, rb], writes=[q2b])
                    kb.op("dve", lambda e, q1=q1, q2=q2: e.tensor_tensor(out=q1[:], in0=q1[:], in1=q2[:], op=ALU.add), reads=[q1b, q2b], writes=[q1b])
                    src, srcb = q1, q1b
                for c in range(8):
                    kb.op("pe", lambda e, c=c, src=src: e.transpose(out=PS[c // 4][:, (c % 4) * 128:(c % 4 + 1) * 128], in_=src[:, c * 128:(c + 1) * 128], identity=ident),
                          reads=[srcb, cstB], writes=[PB[c // 4]])
                tt, tb = qkT.next()
                for hf in range(2):
                    kb.op("dve" if hf == 0 else "act",
                          (lambda e, tt=tt: e.tensor_copy(out=tt[:, 0:4, :], in_=PS[0][:, :].rearrange("p (c n) -> p c n", c=4))) if hf == 0 else
                          (lambda e, tt=tt: e.activation(out=tt[:, 4:8, :], in_=PS[1][:, :].rearrange("p (c n) -> p c n", c=4), func=AF.Identity)),
                          reads=[PB[hf]], writes=[tb])
                kb.dma("sp", QT[:, :, i * 128:(i + 1) * 128].rearrange("h p t -> p h t"), tt[:, 0:4, :], reads=[tb], writes=[dB["QK"][i]])
                kb.dma("sp", KT[:, :, i * 128:(i + 1) * 128].rearrange("h p t -> p h t"), tt[:, 4:8, :], reads=[tb], writes=[dB["QK"][i]])
        if upto in ("QT", "RKV", "VV"):
            break

        with kb.phase():
            lq = kb.sb([128, 4, 64], F32)
            lqB = Buf()
            for j, n in enumerate(("lam_q1", "lam_k1", "lam_q2", "lam_k2")):
                kb.dma("sp", lq[:, j, :], I[n][l, :].partition_broadcast(128), writes=[lqB])
            lt = kb.sb([128, 2, 64], F32)
            lv = kb.sb([128, 4], F32)
            lvB = Buf()
            kb.op("dve", lambda e: e.tensor_tensor(out=lt[:], in0=lq[:, 0:4:2, :], in1=lq[:, 1:4:2, :], op=ALU.mult), reads=[lqB], writes=[lvB])
            kb.op("dve", lambda e: e.tensor_reduce(out=lv[:, 0:2], in_=lt[:], axis=AX.X, op=ALU.add), reads=[lvB], writes=[lvB])
            kb.op("act", lambda e: e.activation(out=lv[:, 0:2], in_=lv[:, 0:2], func=AF.Exp), reads=[lvB], writes=[lvB])
            kb.op("dve", lambda e: e.tensor_tensor(out=lv[:, 2:3], in0=lv[:, 1:2], in1=lv[:, 0:1], op=ALU.subtract), reads=[lvB], writes=[lvB])
            kb.op("dve", lambda e: e.tensor_scalar(out=lv[:, 3:4], in0=lv[:, 2:3], scalar1=-lam_init, scalar2=None, op0=ALU.add), reads=[lvB], writes=[lvB])
            nlam = lv[:, 3:4]
            sg = kb.sb([128, 128], F32)
            sgB = Buf()
            kb.dma("sp", sg[:], I["subln_g"][l, :].partition_broadcast(128), writes=[sgB])
            kb.op("dve", lambda e: e.tensor_scalar(out=sg[:], in0=sg[:], scalar1=(1.0 - lam_init), scalar2=None, op0=ALU.mult), reads=[sgB], writes=[sgB])
            qT0 = kb.sb([128, T], BF16)
            qT1 = kb.sb([128, T], BF16)
            qTm = [qT0, qT1]
            kTt = kb.sb([128, T], BF16)
            vaug = kb.sb([128, NT, 129], BF16)
            qkvB = Buf()
            kb.op("pool", lambda e: e.memset(vaug[:], 1.0), writes=[qkvB])
            kb.op("pool", lambda e: e.memset(qT0[:], 0.0), writes=[qkvB])
            kb.op("pool", lambda e: e.memset(qT1[:], 0.0), writes=[qkvB])
            PTring = Ring(kb, 4, [128, NT, 512], BF16)
            att = Ring(kb, 2, [128, 128], F32)
            sm = Ring(kb, 2, [128, 8], F32)
            sqt = Ring(kb, 2, [128, 128], F32)
            sbank = [0]
            for h in range(4):
                kb.dma("sp", qT0[0:64, :], QT[h, 0:64, :], reads=dB["QK"][0:NT], writes=[qkvB])
                kb.dma("sp", qT1[64:128, :], QT[h, 64:128, :], reads=dB["QK"][0:NT], writes=[qkvB])
                kb.dma("sp", kTt[:], KT[h, :, :], reads=dB["QK"][0:NT], writes=[qkvB])
                kb.dma("sp", vaug[:, :, 0:128], VV[:, h * 128:(h + 1) * 128].rearrange("(n p) d -> p n d", p=128), reads=dB["VV"][0:NT], writes=[qkvB])
                blocks = ([] if last else [(0, 256, [0, 1])]) + [(256 + 512 * j, 512, list(range(NT))) for j in range(8)]
                prevB = None

                def merge_emit(A, Bp):
                    kb.rec = None
                    nA = max(len(A), 1)
                    nB = len(Bp) if Bp else 0
                    jb = 0
                    for ia, (kind, a_) in enumerate(A):
                        (kb.op if kind == "op" else kb.dma)(*a_)
                        if ia % 2 == 1 or ia == len(A) - 1:
                            tgt = nB * (ia + 1) // nA
                            while jb < tgt:
                                kind2, b_ = Bp[jb]
                                (kb.op if kind2 == "op" else kb.dma)(*b_)
                                jb += 1
                    while jb < nB:
                        kind2, b_ = Bp[jb]
                        (kb.op if kind2 == "op" else kb.dma)(*b_)
                        jb += 1

                for (q0, nq, kts) in blocks:
                    kb.rec = []
                    PTs, PTB = [None, None], [None, None]
                    for m in range(2):
                        PTs[m], PTB[m] = PTring.next()
                    for ki, kt in enumerate(kts):
                        for m in range(2):
                            pb = sbank[0] % 4
                            sbank[0] += 1
                            kb.op("pe", lambda e, pb=pb, m=m, kt=kt, q0=q0, nq=nq: e.matmul(PS[pb][:, 0:nq], lhsT=kTt[:, kt * 128:(kt + 1) * 128],
                                                                                     rhs=qTm[m][:, q0:q0 + nq], start=True, stop=True),
                                  reads=[qkvB], writes=[PB[pb]])
                            kb.op("act", lambda e, pb=pb, pt_=PTs[m], ki=ki, nq=nq: e.activation(out=pt_[:, ki, 0:nq], in_=PS[pb][:, 0:nq], func=AF.Exp, scale=0.125),
                                  reads=[PB[pb]], writes=[PTB[m]])
                    recA = kb.rec
                    kb.rec = []
                    for qs in range(nq // 128):
                        for m in range(2):
                            ob = 4 + 2 * (qs % 2) + m
                            for ki, kt in enumerate(kts):
                                kb.op("pe", lambda e, ob=ob, pt_=PTs[m], ki=ki, kt=kt, qs=qs, n=len(kts): e.matmul(PS[ob][:, 0:129], lhsT=pt_[:, ki, qs * 128:(qs + 1) * 128], rhs=vaug[:, kt, :],
                                                                                            start=(ki == 0), stop=(ki == n - 1)),
                                      reads=[PTB[m], qkvB], writes=[PB[ob]])
                        o0 = 4 + 2 * (qs % 2)
                        o1 = o0 + 1
                        st_, sB = sm.next()
                        at, aB = att.next()
                        sq_, sqB = sqt.next()
                        kb.op("dve", lambda e, st_=st_, o0=o0: e.reciprocal(out=st_[:, 0:1], in_=PS[o0][:, 128:129]), reads=[PB[o0]], writes=[sB])
                        kb.op("dve", lambda e, st_=st_, o1=o1: e.reciprocal(out=st_[:, 1:2], in_=PS[o1][:, 128:129]), reads=[PB[o1]], writes=[sB])
                        kb.op("dve", lambda e, st_=st_: e.tensor_tensor(out=st_[:, 2:3], in0=st_[:, 1:2], in1=nlam, op=ALU.mult), reads=[sB, lvB], writes=[sB])
                        kb.op("dve", lambda e, st_=st_, at=at, o0=o0: e.tensor_scalar(out=at[:], in0=PS[o0][:, 0:128], scalar1=st_[:, 0:1], scalar2=None, op0=ALU.mult),
                              reads=[PB[o0], sB], writes=[aB])
                        kb.op("dve", lambda e, st_=st_, at=at, o1=o1: e.scalar_tensor_tensor(out=at[:], in0=PS[o1][:, 0:128], scalar=st_[:, 2:3], in1=at[:], op0=ALU.mult, op1=ALU.add),
                              reads=[PB[o1], sB, aB], writes=[aB])
                        kb.op("pool", lambda e, at=at, sq_=sq_: e.tensor_tensor(out=sq_[:], in0=at[:], in1=at[:], op=ALU.mult), reads=[aB], writes=[sqB])
                        kb.op("dve", lambda e, st_=st_, sq_=sq_: e.tensor_reduce(out=st_[:, 3:4], in_=sq_[:], axis=AX.X, op=ALU.add), reads=[sqB], writes=[sB])
                        kb.op("dve", lambda e, st_=st_: e.tensor_scalar(out=st_[:, 4:5], in0=st_[:, 3:4], scalar1=1.0 / 128, scalar2=SUBLN_EPS, op0=ALU.mult, op1=ALU.add), reads=[sB], writes=[sB])
                        kb.op("act", lambda e, st_=st_: e.activation(out=st_[:, 5:6], in_=st_[:, 4:5], func=AF.Sqrt), reads=[sB], writes=[sB])
                        kb.op("dve", lambda e, st_=st_: e.reciprocal(out=st_[:, 6:7], in_=st_[:, 5:6]), reads=[sB], writes=[sB])
                        kb.op("dve", lambda e, st_=st_, at=at: e.scalar_tensor_tensor(out=at[:], in0=at[:], scalar=st_[:, 6:7], in1=sg[:], op0=ALU.mult, op1=ALU.mult),
                              reads=[sB, aB, sgB], writes=[aB])
                        t0 = q0 + qs * 128
                        kb.dma("sp", MIX[t0:t0 + 128, h * 128:(h + 1) * 128], at[:], reads=[aB], writes=[dB["MIX"][t0 // 128]])
                    recB = kb.rec
                    merge_emit(recA, prevB)
                    prevB = recB
                merge_emit([], prevB)

        with kb.phase():
            cw = kb.sb([128, 3, 256], F32)
            cwB = Buf()
            for j in range(3):
                kb.dma("sp", cw[:, j, :], I["conv_w"][l, j, :].partition_broadcast(128), writes=[cwB])
            c3 = Ring(kb, 2, [128, 3, 768], F32)
            u3 = Ring(kb, 2, [128, 3, 256], F32)
            co = Ring(kb, 2, [128, 256], F32)
            for i in kb.interleave(range(2 if last else 0, NT), 2):
                pr0 = prow(i * 128)
                ct, cB = c3.next()
                for j in range(3):
                    kb.dma("sp", ct[:, j, :], CIN[pr0 - 1 + j:pr0 - 1 + j + 128, :], reads=dB["CIN"][max(i - 1, 0):i + 2], writes=[cB])
                ut, uB = u3.next()
                ot, oB = co.next()
                kb.op("pool", lambda e, ct=ct, ut=ut: e.tensor_tensor(out=ut[:], in0=ct[:, :, 512:768], in1=ct[:, :, 0:256], op=ALU.mult), reads=[cB], writes=[uB])
                kb.op("dve", lambda e, ut=ut: e.tensor_tensor(out=ut[:], in0=ut[:], in1=cw[:], op=ALU.mult), reads=[uB, cwB], writes=[uB])
                kb.op("dve", lambda e, ut=ut, ot=ot: e.tensor_tensor(out=ot[:], in0=ut[:, 0, :], in1=ut[:, 1, :], op=ALU.add), reads=[uB], writes=[oB])
                kb.op("dve", lambda e, ut=ut, ot=ot: e.tensor_tensor(out=ot[:], in0=ot[:], in1=ut[:, 2, :], op=ALU.add), reads=[uB, oB], writes=[oB])
                kb.op("dve", lambda e, ct=ct, ot=ot: e.tensor_tensor(out=ot[:], in0=ot[:], in1=ct[:, 1, 256:512], op=ALU.mult), reads=[cB, oB], writes=[oB])
                kb.dma("sp", MIX[i * 128:(i + 1) * 128, 768:1024], ot[:], reads=[oB], writes=[dB["MIX"][i]])

        with kb.phase():
            cw3 = kb.sb([128, 3, 768], F32)
            cw3B = Buf()
            for j in range(3):
                kb.dma("sp", cw3[:, j, :], I["rkv_conv"][l, j, :].partition_broadcast(128), writes=[cw3B])
            vecs = kb.sb([128, 3, 256], F32)
            vecsB = Buf()
            for j, n in enumerate(("k_k", "k_a", "r_k")):
                kb.dma("sp", vecs[:, j, :], I[n][l, :].partition_broadcast(128), writes=[vecsB])
            wup = kb.sb([33, 2, 256], F32)
            aup = kb.sb([33, 2, 256], F32)
            gup = kb.sb([64, 256], F32)
            wB = Buf()
            for d in range(2):
                kb.dma("sp", wup[0:32, d, :], I["decay_up"][l, d, :, :], writes=[wB])
                kb.dma("sp", wup[32:33, d, :], I["decay_w0"][l, d:d + 1, :], writes=[wB])
                kb.dma("sp", aup[0:32, d, :], I["iclr_up"][l, d, :, :], writes=[wB])
                kb.dma("sp", aup[32:33, d, :], I["iclr_a0"][l, d:d + 1, :], writes=[wB])
            kb.dma("sp", gup[:], I["gate_up"][l, :, :], writes=[wB])
            lwl = Ring(kb, 2, [33, 2, 128], F32)
            lal = Ring(kb, 2, [33, 2, 128], F32)
            lgl = Ring(kb, 2, [64, 128], F32)
            for rg in (lwl, lal):
                for t_, b_ in zip(rg.t, rg.b):
                    kb.op("pool", lambda e, t_=t_: e.memset(t_[:], 1.0), writes=[b_])
            r3 = Ring(kb, 2, [128, 3, 768], F32)
            lo = Ring(kb, 2, [128, 192], F32)
            rkvr = Ring(kb, 2, [128, 768], F32)
            po = Ring(kb, 2, [128, 2564], F32)
            av = Ring(kb, 2, [128, 512], F32)
            tw = Ring(kb, 2, [128, 512], F32)
            krr = Ring(kb, 2, [128, 256], F32)
            t1r = Ring(kb, 2, [128, 256], F32)
            t2r = Ring(kb, 2, [128, 256], F32)
            smr = Ring(kb, 2, [128, 16], F32)
            for i in kb.interleave(range(NT), 2):
                pq = 4 * (i % 2)
                pr0 = prow(i * 128)
                rt, rB = r3.next()
                for j in range(3):
                    kb.dma("sp", rt[:, j, :], RKV[pr0 - 1 + j:pr0 - 1 + j + 128, :], reads=dB["RKV"][max(i - 1, 0):i + 2], writes=[rB])
                lt_, lB = lo.next()
                kb.dma("sp", lt_[:], LORA[i * 128:(i + 1) * 128, :], reads=[dB["LORA"][i]], writes=[lB])
                kv, kvB = rkvr.next()
                kb.op("pool", lambda e, rt=rt: e.tensor_tensor(out=rt[:], in0=rt[:], in1=cw3[:], op=ALU.mult), reads=[rB, cw3B], writes=[rB])
                kb.op("dve", lambda e, rt=rt, kv=kv: e.tensor_tensor(out=kv[:], in0=rt[:, 0, :], in1=rt[:, 1, :], op=ALU.add), reads=[rB], writes=[kvB])
                kb.op("dve", lambda e, rt=rt, kv=kv: e.tensor_tensor(out=kv[:], in0=kv[:], in1=rt[:, 2, :], op=ALU.add), reads=[rB, kvB], writes=[kvB])
                r_ = kv[:, 0:256]
                k_ = kv[:, 256:512]
                v_ = kv[:, 512:768]
                for j in range(4):
                    kb.op("pe", lambda e, pq=pq, j=j, lt_=lt_: e.transpose(out=PS[pq + 0][0:32, j * 128:(j + 1) * 128], in_=lt_[:, j * 32:(j + 1) * 32], identity=ident), reads=[lB, cstB], writes=[PB[pq + 0]])
                kb.op("pe", lambda e, pq=pq, lt_=lt_: e.transpose(out=PS[pq + 1][0:64, 0:128], in_=lt_[:, 128:192], identity=ident), reads=[lB, cstB], writes=[PB[pq + 1]])
                wl_, wlB = lwl.next()
                al_, alB = lal.next()
                gl_, glB = lgl.next()
                kb.op("act", lambda e, pq=pq, wl_=wl_: e.activation(out=wl_[0:32, :, :], in_=PS[pq + 0][0:32, 0:256].rearrange("p (d n) -> p d n", d=2), func=AF.Tanh), reads=[PB[pq + 0]], writes=[wlB])
                kb.op("act", lambda e, pq=pq, al_=al_: e.activation(out=al_[0:32, :, :], in_=PS[pq + 0][0:32, 256:512].rearrange("p (d n) -> p d n", d=2), func=AF.Identity), reads=[PB[pq + 0]], writes=[alB])
                kb.op("act", lambda e, pq=pq, gl_=gl_: e.activation(out=gl_[:], in_=PS[pq + 1][0:64, 0:128], func=AF.Sigmoid), reads=[PB[pq + 1]], writes=[glB])
                for d in range(2):
                    kb.op("pe", lambda e, pq=pq, d=d, wl_=wl_: e.matmul(PS[pq + 2][:, d * 256:(d + 1) * 256], lhsT=wl_[0:33, d, :], rhs=wup[0:33, d, :], start=True, stop=True), reads=[wlB, wB], writes=[PB[pq + 2]])
                    kb.op("pe", lambda e, pq=pq, d=d, al_=al_: e.matmul(PS[pq + 3][:, d * 256:(d + 1) * 256], lhsT=al_[0:33, d, :], rhs=aup[0:33, d, :], start=True, stop=True), reads=[alB, wB], writes=[PB[pq + 3]])
                kb.op("pe", lambda e, pq=pq, gl_=gl_: e.matmul(PS[pq + 1][:, 256:512], lhsT=gl_[:], rhs=gup[:], start=True, stop=True), reads=[glB, wB], writes=[PB[pq + 1]])
                pt, pB = po.next()
                a_, aB = av.next()
                w_, wwB = tw.next()
                kb.op("act", lambda e, pq=pq, w_=w_: e.activation(out=w_[:], in_=PS[pq + 2][:, :], func=AF.Sigmoid), reads=[PB[pq + 2]], writes=[wwB])
                kb.op("act", lambda e, pq=pq, a_=a_: e.activation(out=a_[:], in_=PS[pq + 3][:, :], func=AF.Sigmoid), reads=[PB[pq + 3]], writes=[aB])
                kb.op("act", lambda e, pq=pq, pt=pt: e.activation(out=pt[:, 2304:2560], in_=PS[pq + 1][:, 256:512], func=AF.Identity), reads=[PB[pq + 1]], writes=[pB])
                for d in range(2):
                    kb.op("dve", lambda e, d=d, w_=w_, pt=pt: e.tensor_scalar(out=pt[:, d * 1536:d * 1536 + 256], in0=w_[:, d * 256:(d + 1) * 256], scalar1=-0.6065306597126334, scalar2=None, op0=ALU.mult),
                          reads=[wwB], writes=[pB])
                kr, krB = krr.next()
                t1, t1B = t1r.next()
                t2, t2B = t2r.next()
                sm_, smB = smr.next()
                kb.op("dve", lambda e, kr=kr, k_=k_: e.tensor_tensor(out=kr[:], in0=k_, in1=vecs[:, 0, :], op=ALU.mult), reads=[kvB, vecsB], writes=[krB])
                kb.op("pool", lambda e, kr=kr, t1=t1: e.tensor_tensor(out=t1[:], in0=kr[:], in1=kr[:], op=ALU.mult), reads=[krB], writes=[t1B])
                kb.op("dve", lambda e, t1=t1, sm_=sm_: e.tensor_reduce(out=sm_[:, 0:4], in_=t1[:].rearrange("p (h n) -> p h n", h=4), axis=AX.X, op=ALU.add), reads=[t1B], writes=[smB])
                kb.op("dve", lambda e, sm_=sm_: e.tensor_scalar(out=sm_[:, 0:4], in0=sm_[:, 0:4], scalar1=1e-24, scalar2=None, op0=ALU.max), reads=[smB], writes=[smB])
                kb.op("act", lambda e, sm_=sm_: e.activation(out=sm_[:, 4:8], in_=sm_[:, 0:4], func=AF.Sqrt), reads=[smB], writes=[smB])
                kb.op("dve", lambda e, sm_=sm_: e.reciprocal(out=sm_[:, 8:12], in_=sm_[:, 4:8]), reads=[smB], writes=[smB])
                kb.op("dve", lambda e, sm_=sm_, kr=kr, pt=pt: e.tensor_tensor(out=pt[:, 768:1024].rearrange("p (h n) -> p h n", h=4), in0=kr[:].rearrange("p (h n) -> p h n", h=4),
                                                                       in1=sm_[:, 8:12].unsqueeze(2).to_broadcast([128, 4, 64]), op=ALU.mult), reads=[smB, krB], writes=[pB])
                kb.op("pool", lambda e, pt=pt, r_=r_: e.tensor_copy(out=pt[:, 1024:1280], in_=r_), reads=[kvB], writes=[pB])
                kb.op("pool", lambda e, pt=pt, v_=v_: e.tensor_copy(out=pt[:, 1280:1536], in_=v_), reads=[kvB], writes=[pB])
                for d in range(2):
                    ob = d * 1536
                    kb.op("dve", lambda e, d=d, a_=a_, t1=t1: e.scalar_tensor_tensor(out=t1[:], in0=a_[:, d * 256:(d + 1) * 256], scalar=-1.0, in1=vecs[:, 1, :], op0=ALU.add, op1=ALU.mult),
                          reads=[aB, vecsB, t1B], writes=[t1B])
                    kb.op("dve", lambda e, ob=ob, t1=t1, pt=pt, k_=k_: e.scalar_tensor_tensor(out=pt[:, ob + 512:ob + 768], in0=t1[:], scalar=1.0, in1=k_, op0=ALU.add, op1=ALU.mult),
                          reads=[t1B, kvB], writes=[pB])
                    kb.op("pool", lambda e, ob=ob, d=d, a_=a_, pt=pt: e.tensor_tensor(out=pt[:, ob + 256:ob + 512], in0=pt[:, 768:1024], in1=a_[:, d * 256:(d + 1) * 256], op=ALU.mult),
                          reads=[aB, pB], writes=[pB])
                kb.op("pool", lambda e, pt=pt, t2=t2: e.tensor_tensor(out=t2[:], in0=pt[:, 512:768], in1=pt[:, 2048:2304], op=ALU.add), reads=[pB], writes=[t2B])
                kb.op("pool", lambda e, t2=t2, r_=r_: e.tensor_tensor(out=t2[:], in0=t2[:], in1=r_, op=ALU.mult), reads=[kvB, t2B], writes=[t2B])
                kb.op("pool", lambda e, t2=t2: e.tensor_tensor(out=t2[:], in0=t2[:], in1=vecs[:, 2, :], op=ALU.mult), reads=[vecsB, t2B], writes=[t2B])
                kb.op("dve", lambda e, t2=t2, pt=pt: e.tensor_reduce(out=pt[:, 2560:2564], in_=t2[:].rearrange("p (h n) -> p h n", h=4), axis=AX.X, op=ALU.add), reads=[t2B], writes=[pB])
                kb.dma("sp", PREP[i * 128:(i + 1) * 128, :], pt[:], reads=[pB], writes=[dB["PREP"][i]])
        if upto == "PREP":
            break

        with kb.phase():
            idR = kb.sb([64, 64], F32R)
            idRB = Buf()
            kb.op("act", lambda e: e.activation(out=idR[:], in_=cst[0:64, 0:64], func=AF.Identity), reads=[cstB], writes=[idRB])
            id64 = cst[0:64, 0:64]

            def dir_gen(d):
                B0, B1, B2, B3 = 4 * d, 4 * d + 1, 4 * d + 2, 4 * d + 3
                ST = kb.sb([64, 4, 64], F32R)
                STB = Buf()
                chk = Ring(kb, 3, [64, 1536], F32)
                Er = Ring(kb, 2, [64, 3, 256], F32)
                HTr = Ring(kb, 2, [64, 4, 256], F32R)
                FMr = Ring(kb, 2, [64, 4, 4, 64], F32R)
                Gr = Ring(kb, 2, [64, 4, 2, 128], F32R)
                NTr = Ring(kb, 2, [64, 4, 64], F32R)
                Pmr = Ring(kb, 2, [64, 4, 64], F32R)
                Nar = Ring(kb, 2, [64, 4, 64], F32R)
                NTar = Ring(kb, 2, [64, 4, 64], F32R)
                Xr = Ring(kb, 2, [64, 4, 64], F32R)
                Ur = Ring(kb, 2, [64, 4, 64], F32R)
                Yr = Ring(kb, 2, [64, 256], F32)
                PCr = Ring(kb, 2, [64, 4], F32)
                PPr = Ring(kb, 2, [64, 256], F32)
                vRr = Ring(kb, 2, [64, 256], F32R)
                for h in range(4):
                    kb.op("act", lambda e, h=h: e.activation(out=ST[:, h, :], in_=cst[0:64, 0:64], func=AF.Identity, scale=0.0), reads=[cstB], writes=[STB])
                if d == 0:
                    o_lw, o_b, o_kd, o_kk, o_r, o_v, c0 = 0, 256, 512, 768, 1024, 1280, 0
                else:
                    o_kk, o_r, o_v, o_lw, o_b, o_kd, c0 = 0, 256, 512, 768, 1024, 1280, 768
                mask = cst[0:64, 128 + d * 128:256 + d * 128]
                maskT = cst[0:64, 384 + d * 64:448 + d * 64]
                tri = cst[0:64, 512 + d * 64:576 + d * 64]
                order = range(NCH) if d == 0 else ([3, 2, 1, 0] + list(range(NCH - 1, 3, -1)))
                for c in order:
                    ck, ckB = chk.next()
                    kb.dma("sp", ck[:], PREP[c * 64:(c + 1) * 64, c0:c0 + 1536], reads=[dB["PREP"][c // 2]], writes=[ckB])
                    lw = ck[:, o_lw:o_lw + 256]
                    kb.op("pe", lambda e: e.matmul(PS[B0][0:64, 0:256], lhsT=tri, rhs=lw, start=True, stop=True), reads=[ckB, cstB], writes=[PB[B0]])
                    for hf in range(4):
                        kb.op("pe", lambda e, hf=hf: e.matmul(PS[B0][0:64, 256 + 2 * hf:258 + 2 * hf], lhsT=lw[:, hf * 64:(hf + 1) * 64], rhs=cst[0:64, 640:642], start=True, stop=True),
                              reads=[ckB, cstB], writes=[PB[B0]])
                    yield
                    E, EB = Er.next()
                    PC, PCB = PCr.next()
                    kb.op("act", lambda e: e.activation(out=E[:, 0, :], in_=PS[B0][0:64, 0:256], func=AF.Exp), reads=[PB[B0]], writes=[EB])
                    kb.op("act", lambda e: e.activation(out=E[:, 1, :], in_=PS[B0][0:64, 0:256], func=AF.Exp, scale=-1.0), reads=[PB[B0]], writes=[EB])
                    kb.op("act", lambda e: e.activation(out=E[:, 2, :], in_=lw, func=AF.Exp, scale=-1.0), reads=[ckB], writes=[EB])
                    kb.op("act", lambda e: e.activation(out=PC[:], in_=PS[B0][0:64, 256:264:2], func=AF.Exp), reads=[PB[B0]], writes=[PCB])
                    vR, vRB = vRr.next()
                    kb.op("pool", lambda e: e.tensor_copy(out=vR[:], in_=ck[:, o_v:o_v + 256]), reads=[ckB], writes=[vRB])
                    yield
                    HT, HTB = HTr.next()
                    kb.op("dve", lambda e: e.tensor_tensor(out=HT[:, 0, :], in0=ck[:, o_b:o_b + 256], in1=E[:, 1, :], op=ALU.mult), reads=[ckB, EB], writes=[HTB])
                    kb.op("pool", lambda e: e.tensor_tensor(out=HT[:, 1, :], in0=ck[:, o_kd:o_kd + 256], in1=E[:, 1, :], op=ALU.mult), reads=[ckB, EB], writes=[HTB])
                    kb.op("dve", lambda e: e.scalar_tensor_tensor(out=HT[:, 2, :], in0=ck[:, o_kk:o_kk + 256], scalar=-1.0, in1=E[:, 2, :], op0=ALU.mult, op1=ALU.mult),
                          reads=[ckB, EB], writes=[HTB])
                    kb.op("pool", lambda e: e.tensor_tensor(out=HT[:, 3, :], in0=ck[:, o_r:o_r + 256], in1=E[:, 0, :], op=ALU.mult), reads=[ckB, EB], writes=[HTB])
                    yield
                    kb.op("dve", lambda e: e.tensor_tensor(out=HT[:, 2, :], in0=HT[:, 2, :].bitcast(F32), in1=E[:, 0, :], op=ALU.mult), reads=[EB, HTB], writes=[HTB])
                    yield
                    FM, FMB = FMr.next()
                    for hh in range(2):
                        for h in (2 * hh, 2 * hh + 1):
                            for q in range(4):
                                o = ((h % 2) * 4 + q) * 64
                                kb.op("pe", lambda e, q=q, h=h, o=o: e.transpose(out=PS[B1][0:64, o:o + 64], in_=HT[:, q, h * 64:(h + 1) * 64].bitcast(F32), identity=id64), reads=[HTB, cstB], writes=[PB[B1]])
                        yield
                        kb.op("act", lambda e, hh=hh: e.activation(out=FM[:, 2 * hh:2 * hh + 2, :, :].rearrange("p a q n -> p (a q n)"), in_=PS[B1][0:64, :], func=AF.Identity), reads=[PB[B1]], writes=[FMB])
                        yield
                    G, GB = Gr.next()
                    for hh in range(2):
                        for h in (2 * hh, 2 * hh + 1):
                            for g in range(2):
                                o = ((h % 2) * 2 + g) * 128
                                kb.op("pe", lambda e, h=h, g=g, o=o: e.matmul(PS[B2][0:64, o:o + 128], lhsT=FM[:, h, g, :], rhs=FM[:, h, 2:4, :].rearrange("p a n -> p (a n)"), start=True, stop=True),
                                      reads=[FMB], writes=[PB[B2]])
                        if hh == 0:
                            for h in range(4):
                                kb.op("pe", lambda e, h=h: e.matmul(PS[B3][0:64, h * 64:(h + 1) * 64], lhsT=FM[:, h, 2, :], rhs=FM[:, h, 0, :], start=True, stop=True), reads=[FMB], writes=[PB[B3]])
                        yield
                        kb.op("act", lambda e, hh=hh: e.activation(out=G[:, 2 * hh:2 * hh + 2, :, :].rearrange("p h g n -> p (h g n)"), in_=PS[B2][0:64, :], func=AF.Identity), reads=[PB[B2]], writes=[GB])
                        yield
                    NTt, NTB = NTr.next()
                    kb.op("act", lambda e: e.activation(out=NTt[:].rearrange("p h n -> p (h n)"), in_=PS[B3][0:64, 0:256], func=AF.Identity), reads=[PB[B3]], writes=[NTB])
                    kb.op("pool", lambda e: e.tensor_tensor(out=G[:].rearrange("p h g n -> p (h g) n"), in0=G[:].bitcast(F32).rearrange("p h g n -> p (h g) n"),
                                                           in1=mask.unsqueeze(1).to_broadcast([64, 8, 128]), op=ALU.mult), reads=[GB, cstB], writes=[GB])
                    yield
                    kb.op("dve", lambda e: e.tensor_tensor(out=NTt[:], in0=NTt[:].bitcast(F32), in1=maskT.unsqueeze(1).to_broadcast([64, 4, 64]), op=ALU.mult), reads=[NTB, cstB], writes=[NTB])
                    Pm, PmB = Pmr.next()
                    kb.op("pool", lambda e: e.tensor_tensor(out=Pm[:], in0=G[:, :, 0, 0:64].bitcast(F32), in1=id64.unsqueeze(1).to_broadcast([64, 4, 64]), op=ALU.add), reads=[GB, cstB], writes=[PmB])
                    yield
                    Nc = lambda h: G[:, h, 0, 0:64]
                    NTc = lambda h: NTt[:, h, :]
                    NcB, NTcB = GB, NTB
                    for s_ in range(5):
                        lastS = (s_ == 4)
                        Na, NaB = Nar.next()
                        NTa, NTaB = NTar.next()
                        if not lastS:
                            for h in range(4):
                                kb.op("pe", lambda e, h=h, Nc=Nc, NTc=NTc: e.matmul(PS[B1][0:64, h * 64:(h + 1) * 64], lhsT=NTc(h), rhs=Nc(h), start=True, stop=True), reads=[NcB, NTcB], writes=[PB[B1]])
                        for h in range(4):
                            kb.op("pe", lambda e, h=h, Nc=Nc, NTc=NTc: e.matmul(PS[B2][0:64, h * 64:(h + 1) * 64], lhsT=Nc(h), rhs=NTc(h), start=True, stop=True), reads=[NcB, NTcB], writes=[PB[B2]])
                        yield
                        kb.op("act", lambda e, NTa=NTa: e.activation(out=NTa[:].rearrange("p h n -> p (h n)"), in_=PS[B2][0:64, 0:256], func=AF.Identity), reads=[PB[B2]], writes=[NTaB])
                        if not lastS:
                            kb.op("act", lambda e, Na=Na: e.activation(out=Na[:].rearrange("p h n -> p (h n)"), in_=PS[B1][0:64, 0:256], func=AF.Identity), reads=[PB[B1]], writes=[NaB])
                        yield
                        for h in range(4):
                            kb.op("pe", lambda e, h=h, NTa=NTa: e.matmul(PS[B3][0:64, h * 64:(h + 1) * 64], lhsT=NTa[:, h, :], rhs=Pm[:, h, :], start=True, stop=True), reads=[NTaB, PmB], writes=[PB[B3]])
                        yield
                        PPt, PPB = PPr.next()
                        kb.op("act", lambda e, PPt=PPt: e.activation(out=PPt[:], in_=PS[B3][0:64, 0:256], func=AF.Identity), reads=[PB[B3]], writes=[PPB])
                        yield
                        kb.op("dve", lambda e, PPt=PPt: e.tensor_tensor(out=Pm[:].rearrange("p h n -> p (h n)"), in0=Pm[:].bitcast(F32).rearrange("p h n -> p (h n)"), in1=PPt[:], op=ALU.add),
                              reads=[PPB, PmB], writes=[PmB])
                        Nc = lambda h, Na=Na: Na[:, h, :]
                        NTc = lambda h, NTa=NTa: NTa[:, h, :]
                        NcB, NTcB = NaB, NTaB
                    yield
                    Xs, XB = Xr.next()
                    Us, UB = Ur.next()
                    Ys, YB = Yr.next()
                    vh = lambda h: vR[:, h * 64:(h + 1) * 64]
                    for h in range(4):
                        kb.op("pe", lambda e, h=h: e.matmul(PS[B1][0:64, h * 64:(h + 1) * 64], lhsT=FM[:, h, 2, :], rhs=ST[:, h, :], start=True, stop=False), reads=[FMB, STB], writes=[PB[B1]])
                        kb.op("pe", lambda e, h=h: e.matmul(PS[B1][0:64, h * 64:(h + 1) * 64], lhsT=G[:, h, 1, 0:64], rhs=vh(h), start=False, stop=True), reads=[GB, vRB], writes=[PB[B1]])
                    yield
                    kb.op("act", lambda e: e.activation(out=Xs[:].rearrange("p h n -> p (h n)"), in_=PS[B1][0:64, 0:256], func=AF.Identity), reads=[PB[B1]], writes=[XB])
                    yield
                    for h in range(4):
                        kb.op("pe", lambda e, h=h: e.matmul(PS[B2][0:64, h * 64:(h + 1) * 64], lhsT=Pm[:, h, :], rhs=Xs[:, h, :], start=True, stop=True), reads=[PmB, XB], writes=[PB[B2]])
                    yield
                    kb.op("act", lambda e: e.activation(out=Us[:].rearrange("p h n -> p (h n)"), in_=PS[B2][0:64, 0:256], func=AF.Identity), reads=[PB[B2]], writes=[UB])
                    yield
                    for h in range(4):
                        kb.op("pe", lambda e, h=h: e.matmul(PS[B3][0:64, h * 64:(h + 1) * 64], lhsT=FM[:, h, 3, :], rhs=ST[:, h, :], start=True, stop=False), reads=[FMB, STB], writes=[PB[B3]])
                        kb.op("pe", lambda e, h=h: e.matmul(PS[B3][0:64, h * 64:(h + 1) * 64], lhsT=G[:, h, 0, 64:128], rhs=Us[:, h, :], start=False, stop=False), reads=[GB, UB], writes=[PB[B3]])
                        kb.op("pe", lambda e, h=h: e.matmul(PS[B3][0:64, h * 64:(h + 1) * 64], lhsT=G[:, h, 1, 64:128], rhs=vh(h), start=False, stop=True), reads=[GB, vRB], writes=[PB[B3]])
                    for h in range(4):
                        kb.op("pe", lambda e, h=h: e.matmul(PS[B0][0:64, h * 64:(h + 1) * 64], lhsT=idR[:], rhs=ST[:, h, :], start=True, stop=False), reads=[STB, idRB], writes=[PB[B0]])
                        kb.op("pe", lambda e, h=h: e.matmul(PS[B0][0:64, h * 64:(h + 1) * 64], lhsT=HT[:, 0, h * 64:(h + 1) * 64], rhs=Us[:, h, :], start=False, stop=False), reads=[HTB, UB], writes=[PB[B0]])
                        kb.op("pe", lambda e, h=h: e.matmul(PS[B0][0:64, h * 64:(h + 1) * 64], lhsT=HT[:, 1, h * 64:(h + 1) * 64], rhs=vh(h), start=False, stop=True), reads=[HTB, vRB], writes=[PB[B0]])
                    yield
                    kb.op("act", lambda e: e.activation(out=Ys[:], in_=PS[B3][0:64, 0:256], func=AF.Identity), reads=[PB[B3]], writes=[YB])
                    for h in range(4):
                        kb.op("act", lambda e, h=h: e.activation(out=ST[:, h, :], in_=PS[B0][0:64, h * 64:(h + 1) * 64], func=AF.Identity, scale=PC[:, h:h + 1]), reads=[PB[B0], PCB], writes=[STB])
                    kb.dma("sp", YD[d, c * 64:(c + 1) * 64, :], Ys[:], reads=[YB], writes=[dB["YD%d" % d][c // 2]])
                    yield

            gens = [dir_gen(0), dir_gen(1)]
            while gens:
                for g_ in list(gens):
                    try:
                        next(g_)
                    except StopIteration:
                        gens.remove(g_)
        if upto == "YD":
            break

        with kb.phase():
            gnv = kb.sb([128, 2, 256], F32)
            gnB = Buf()
            kb.dma("sp", gnv[:, 0, :], I["gn_g"][l, :].partition_broadcast(128), writes=[gnB])
            kb.dma("sp", gnv[:, 1, :], I["gn_b"][l, :].partition_broadcast(128), writes=[gnB])
            yr = Ring(kb, 2, [128, 2, 256], F32)
            vr = Ring(kb, 2, [128, 256], F32)
            gr = Ring(kb, 2, [128, 260], F32)
            ycr = Ring(kb, 2, [128, 256], F32)
            sqr = Ring(kb, 2, [128, 256], F32)
            smr = Ring(kb, 2, [128, 16], F32)
            for i in kb.interleave(range(2 if last else 0, NT), 2):
                yt, yB = yr.next()
                vt_, vB = vr.next()
                gt, gB = gr.next()
                for d in range(2):
                    kb.dma("sp", yt[:, d, :], YD[d, i * 128:(i + 1) * 128, :], reads=[dB["YD%d" % d][i]], writes=[yB])
                kb.dma("sp", vt_[:], PREP[i * 128:(i + 1) * 128, 1280:1536], reads=[dB["PREP"][i]], writes=[vB])
                kb.dma("sp", gt[:], PREP[i * 128:(i + 1) * 128, 2304:2564], reads=[dB["PREP"][i]], writes=[gB])
                yc, ycB = ycr.next()
                sq_, sqB = sqr.next()
                sm_, smB = smr.next()
                v4 = lambda t_: t_.rearrange("p (h n) -> p h n", h=4)
                bc = lambda a_: a_.unsqueeze(2).to_broadcast([128, 4, 64])
                kb.op("dve", lambda e, yt=yt, yc=yc: e.tensor_tensor(out=yc[:], in0=yt[:, 0, :], in1=yt[:, 1, :], op=ALU.add), reads=[yB], writes=[ycB])
                kb.op("dve", lambda e, yc=yc, sm_=sm_: e.tensor_reduce(out=sm_[:, 0:4], in_=v4(yc[:]), axis=AX.X, op=ALU.add), reads=[ycB], writes=[smB])
                kb.op("dve", lambda e, sm_=sm_: e.tensor_scalar(out=sm_[:, 0:4], in0=sm_[:, 0:4], scalar1=-1.0 / 64, scalar2=None, op0=ALU.mult), reads=[smB], writes=[smB])
                kb.op("dve", lambda e, yc=yc, sm_=sm_: e.tensor_tensor(out=v4(yc[:]), in0=v4(yc[:]), in1=bc(sm_[:, 0:4]), op=ALU.add), reads=[smB, ycB], writes=[ycB])
                kb.op("pool", lambda e, yc=yc, sq_=sq_: e.tensor_tensor(out=sq_[:], in0=yc[:], in1=yc[:], op=ALU.mult), reads=[ycB], writes=[sqB])
                kb.op("dve", lambda e, sq_=sq_, sm_=sm_: e.tensor_reduce(out=sm_[:, 4:8], in_=v4(sq_[:]), axis=AX.X, op=ALU.add), reads=[sqB], writes=[smB])
                kb.op("dve", lambda e, sm_=sm_: e.tensor_scalar(out=sm_[:, 4:8], in0=sm_[:, 4:8], scalar1=1.0 / 64, scalar2=GN_EPS, op0=ALU.mult, op1=ALU.add), reads=[smB], writes=[smB])
                kb.op("act", lambda e, sm_=sm_: e.activation(out=sm_[:, 8:12], in_=sm_[:, 4:8], func=AF.Sqrt), reads=[smB], writes=[smB])
                kb.op("dve", lambda e, sm_=sm_: e.reciprocal(out=sm_[:, 12:16], in_=sm_[:, 8:12]), reads=[smB], writes=[smB])
                kb.op("dve", lambda e, yc=yc, sm_=sm_: e.tensor_tensor(out=v4(yc[:]), in0=v4(yc[:]), in1=bc(sm_[:, 12:16]), op=ALU.mult), reads=[smB, ycB], writes=[ycB])
                kb.op("dve", lambda e, yc=yc: e.tensor_tensor(out=yc[:], in0=yc[:], in1=gnv[:, 0, :], op=ALU.mult), reads=[gnB, ycB], writes=[ycB])
                kb.op("dve", lambda e, yc=yc: e.tensor_tensor(out=yc[:], in0=yc[:], in1=gnv[:, 1, :], op=ALU.add), reads=[gnB, ycB], writes=[ycB])
                kb.op("pool", lambda e, vt_=vt_, gt=gt, sq_=sq_: e.tensor_tensor(out=v4(sq_[:]), in0=v4(vt_[:]), in1=bc(gt[:, 256:260]), op=ALU.mult), reads=[vB, gB, sqB], writes=[sqB])
                kb.op("dve", lambda e, yc=yc, sq_=sq_: e.tensor_tensor(out=yc[:], in0=yc[:], in1=sq_[:], op=ALU.add), reads=[sqB, ycB], writes=[ycB])
                kb.op("dve", lambda e, yc=yc, gt=gt: e.tensor_tensor(out=yc[:], in0=yc[:], in1=gt[:, 0:256], op=ALU.mult), reads=[gB, ycB], writes=[ycB])
                kb.dma("sp", MIX[i * 128:(i + 1) * 128, 512:768], yc[:], reads=[ycB], writes=[dB["MIX"][i]])
        if upto == "MIX":
            break

        def layer_norm_tile(t_, tB, sq_, sqB, sm_, smB, lnp, lnpB):
            gi = 0
            kb.op("dve", lambda e: e.tensor_reduce(out=sm_[:, 0:1], in_=t_[:], axis=AX.X, op=ALU.add), reads=[tB], writes=[smB])
            kb.op("dve", lambda e: e.tensor_scalar(out=sm_[:, 1:2], in0=sm_[:, 0:1], scalar1=-1.0 / D, scalar2=None, op0=ALU.mult), reads=[smB], writes=[smB])
            kb.op("dve", lambda e: e.tensor_scalar(out=t_[:], in0=t_[:], scalar1=sm_[:, 1:2], scalar2=None, op0=ALU.add), reads=[smB, tB], writes=[tB])
            kb.op("pool", lambda e: e.tensor_tensor(out=sq_[:], in0=t_[:], in1=t_[:], op=ALU.mult), reads=[tB], writes=[sqB])
            kb.op("dve", lambda e: e.tensor_reduce(out=sm_[:, 2:3], in_=sq_[:], axis=AX.X, op=ALU.add), reads=[sqB], writes=[smB])
            kb.op("dve", lambda e: e.tensor_scalar(out=sm_[:, 3:4], in0=sm_[:, 2:3], scalar1=1.0 / D, scalar2=LN_EPS, op0=ALU.mult, op1=ALU.add), reads=[smB], writes=[smB])
            kb.op("act", lambda e: e.activation(out=sm_[:, 4:5], in_=sm_[:, 3:4], func=AF.Sqrt), reads=[smB], writes=[smB])
            kb.op("dve", lambda e: e.reciprocal(out=sm_[:, 5:6], in_=sm_[:, 4:5]), reads=[smB], writes=[smB])
            kb.op("dve", lambda e: e.scalar_tensor_tensor(out=t_[:], in0=t_[:], scalar=sm_[:, 5:6], in1=lnp[:, gi, :], op0=ALU.mult, op1=ALU.mult), reads=[smB, tB, lnpB], writes=[tB])
            kb.op("dve", lambda e: e.tensor_tensor(out=t_[:], in0=t_[:], in1=lnp[:, gi + 1, :], op=ALU.add), reads=[tB, lnpB], writes=[tB])

        with kb.phase():
            gate1, gate1B, ln1t, ln1B = load_gate_ln(0)
            wo = kb.sb([128, 8, D], BF16)
            woB = Buf()
            for c in range(8):
                kb.dma("pool", wo[:, c, :], I["w_out"][l, c * 128:(c + 1) * 128, :], writes=[woB])
            mr = Ring(kb, 2, [128, D], F32)
            mTr = Ring(kb, 2, [128, 8, 128], BF16)
            xr = Ring(kb, 2, [128, D], F32)
            tr_ = Ring(kb, 2, [128, D], F32)
            sqr = Ring(kb, 2, [128, D], F32)
            smr = Ring(kb, 2, [128, 8], F32)
            for i in kb.interleave(range(2 if last else 0, NT), 2):
                pq = 4 * (i % 2)
                w = 1 if i < 2 else 0
                mt, mB = mr.next()
                xt, xB = xr.next()
                kb.dma("sp", mt[:], MIX[i * 128:(i + 1) * 128, :], reads=[dB["MIX"][i]], writes=[mB])
                kb.dma("sp", xt[:], xsrc(l, i), reads=([dB["X"][i]] if l > 0 else []), writes=[xB])
                for c in range(8):
                    kb.op("pe", lambda e, pq=pq, c=c, mt=mt: e.transpose(out=PS[pq + c // 4][:, (c % 4) * 128:(c % 4 + 1) * 128], in_=mt[:, c * 128:(c + 1) * 128], identity=ident), reads=[mB, cstB], writes=[PB[pq + c // 4]])
                mT, mTB = mTr.next()
                kb.op("act", lambda e, pq=pq, mT=mT: e.activation(out=mT[:, 0:4, :].rearrange("p c n -> p (c n)"), in_=PS[pq][:, :], func=AF.Identity), reads=[PB[pq]], writes=[mTB])
                kb.op("dve", lambda e, pq=pq, mT=mT: e.tensor_copy(out=mT[:, 4:8, :].rearrange("p c n -> p (c n)"), in_=PS[pq + 1][:, :]), reads=[PB[pq + 1]], writes=[mTB])
                t_, tB = tr_.next()
                for hf in range(2):
                    for c in range(8):
                        kb.op("pe", lambda e, pq=pq, c=c, hf=hf, mT=mT: e.matmul(PS[pq + 2 + hf][:, :], lhsT=mT[:, c, :], rhs=wo[:, c, hf * 512:(hf + 1) * 512], start=(c == 0), stop=(c == 7)), reads=[mTB, woB], writes=[PB[pq + 2 + hf]])
                    kb.op("dve", lambda e, pq=pq, hf=hf, t_=t_, w=w: e.tensor_tensor(out=t_[:, hf * 512:(hf + 1) * 512], in0=PS[pq + 2 + hf][:, :], in1=gate1[:, w, hf * 512:(hf + 1) * 512], op=ALU.mult), reads=[PB[pq + 2 + hf], gate1B], writes=[tB])
                kb.op("dve", lambda e, t_=t_, xt=xt: e.scalar_tensor_tensor(out=t_[:], in0=xt[:], scalar=DN_ALPHA, in1=t_[:], op0=ALU.mult, op1=ALU.add), reads=[xB, tB], writes=[tB])
                sq_, sqB = sqr.next()
                sm_, smB = smr.next()
                layer_norm_tile(t_, tB, sq_, sqB, sm_, smB, ln1t, ln1B)
                kb.dma("sp", X1[i * 128:(i + 1) * 128, :], t_[:], reads=[tB], writes=[dB["X1"][i]])
        if upto == "X1":
            break

        with kb.phase():
            jj = l // 2
            moe = (l % 2 == 1)
            gate2, gate2B, ln2t, ln2B = load_gate_ln(1)
            if moe:
                experts = [(I["moe_w1"][jj, e], I["moe_w3"][jj, e], I["moe_w2"][jj, e]) for e in range(NE)]
            else:
                experts = [(I["ffn_w1"][jj, :, e * DFE:(e + 1) * DFE], I["ffn_w3"][jj, :, e * DFE:(e + 1) * DFE], I["ffn_w2"][jj, e * DFE:(e + 1) * DFE, :]) for e in range(2)]
            NFC = DFE // 128
            x1g = kb.sb([128, 4, D], F32)
            x1B = Buf()
            hT = kb.sb([128, 8, 512], BF16)
            hTB = Buf()
            hTfr = Ring(kb, 2, [128, 8, 128], F32)
            gTs = [kb.sb([128, NFC, 512], BF16) for _ in range(2)]
            gTBs = [Buf(), Buf()]
            acc = kb.sb([128, 4, D], F32)
            accB = Buf()
            w2e = Ring(kb, 2, [128, NFC, D], BF16)
            w1r = Ring(kb, 4, [128, 8, 128], BF16)
            w3r = Ring(kb, 4, [128, 8, 128], BF16)
            sar = Ring(kb, 2, [128, 512], F32)
            gates = kb.sb([128, 4, NE], F32)
            gatesB = Buf()
            rt = kb.sb([128, 4, 24], F32)
            rtB = Buf()
            sqr = Ring(kb, 2, [128, D], F32)
            smr = Ring(kb, 2, [128, 8], F32)
            if moe:
                rw = kb.sb([128, 8, NE], F32)
                rb = kb.sb([128, NE], F32)
                rwB = Buf()
                kb.dma("sp", rw[:], I["router_w"][jj].rearrange("(c p) e -> p c e", p=128), writes=[rwB])
                kb.dma("sp", rb[:], I["router_b"][jj, :].partition_broadcast(128), writes=[rwB])
            groups = ([] if last else [(0, 2)]) + [(2 + 4 * g_, 4) for g_ in range(8)]
            for (i0, nt_) in groups:
                w = 1 if i0 < 2 else 0
                ng = nt_ * 128
                for ti in range(nt_):
                    i = i0 + ti
                    kb.dma("sp", x1g[:, ti, :], X1[i * 128:(i + 1) * 128, :], reads=[dB["X1"][i]], writes=[x1B])
                    for c in range(8):
                        kb.op("pe", lambda e, c=c, ti=ti: e.transpose(out=PS[c // 4][:, (c % 4) * 128:(c % 4 + 1) * 128], in_=x1g[:, ti, c * 128:(c + 1) * 128], identity=ident), reads=[x1B, cstB], writes=[PB[c // 4]])
                    hf_, hfB = hTfr.next()
                    for c in range(8):
                        kb.op("act", lambda e, c=c, hf_=hf_, w=w: e.activation(out=hf_[:, c, :], in_=PS[c // 4][:, (c % 4) * 128:(c % 4 + 1) * 128], func=AF.Identity,
                                                                          scale=modS[:, w, 3, c:c + 1], bias=modS[:, w, 2, c:c + 1]), reads=[PB[c // 4], modSB], writes=[hfB])
                    kb.op("pool", lambda e, hf_=hf_, ti=ti: e.tensor_copy(out=hT[:, :, ti * 128:(ti + 1) * 128], in_=hf_[:]), reads=[hfB], writes=[hTB])
                    if moe:
                        for c in range(8):
                            kb.op("pe", lambda e, c=c, hf_=hf_: e.matmul(PS[2][:, 0:NE], lhsT=hf_[:, c, :], rhs=rw[:, c, :], start=(c == 0), stop=(c == 7)), reads=[hfB, rwB], writes=[PB[2]])
                        r_ = rt[:, ti, :]
                        kb.op("dve", lambda e, r_=r_: e.tensor_tensor(out=r_[:, 0:8], in0=PS[2][:, 0:NE], in1=rb[:], op=ALU.add), reads=[PB[2], rwB], writes=[rtB])
                        kb.op("dve", lambda e, r_=r_: e.tensor_reduce(out=r_[:, 16:17], in_=r_[:, 0:8], axis=AX.X, op=ALU.max), reads=[rtB], writes=[rtB])
                        kb.op("dve", lambda e, r_=r_: e.tensor_scalar(out=r_[:, 8:16], in0=r_[:, 0:8], scalar1=r_[:, 16:17], scalar2=None, op0=ALU.is_equal), reads=[rtB], writes=[rtB])
                        kb.op("dve", lambda e, r_=r_: e.scalar_tensor_tensor(out=r_[:, 0:8], in0=r_[:, 8:16], scalar=-1e30, in1=r_[:, 0:8], op0=ALU.mult, op1=ALU.add), reads=[rtB], writes=[rtB])
                        kb.op("dve", lambda e, r_=r_: e.tensor_reduce(out=r_[:, 17:18], in_=r_[:, 0:8], axis=AX.X, op=ALU.max), reads=[rtB], writes=[rtB])
                        kb.op("dve", lambda e, r_=r_: e.tensor_scalar(out=r_[:, 0:8], in0=r_[:, 0:8], scalar1=r_[:, 17:18], scalar2=None, op0=ALU.is_equal), reads=[rtB], writes=[rtB])
                        kb.op("dve", lambda e, r_=r_: e.tensor_tensor(out=r_[:, 18:19], in0=r_[:, 16:17], in1=r_[:, 17:18], op=ALU.subtract), reads=[rtB], writes=[rtB])
                        kb.op("act", lambda e, r_=r_: e.activation(out=r_[:, 19:20], in_=r_[:, 18:19], func=AF.Sigmoid), reads=[rtB], writes=[rtB])
                        kb.op("act", lambda e, r_=r_: e.activation(out=r_[:, 20:21], in_=r_[:, 18:19], func=AF.Sigmoid, scale=-1.0), reads=[rtB], writes=[rtB])
                        kb.op("dve", lambda e, r_=r_: e.tensor_scalar(out=r_[:, 8:16], in0=r_[:, 8:16], scalar1=r_[:, 19:20], scalar2=None, op0=ALU.mult), reads=[rtB], writes=[rtB])
                        kb.op("dve", lambda e, r_=r_, ti=ti: e.scalar_tensor_tensor(out=gates[:, ti, :], in0=r_[:, 0:8], scalar=r_[:, 20:21], in1=r_[:, 8:16], op0=ALU.mult, op1=ALU.add), reads=[rtB], writes=[gatesB])
                for ei, (W1, W3, W2) in enumerate(experts):
                    gT, gTB = gTs[ei % 2], gTBs[ei % 2]
                    w2t, w2B = w2e.next()
                    kb.dma("pool", w2t[:], W2.rearrange("(c p) n -> p c n", p=128), writes=[w2B])
                    for fc in range(NFC):
                        w1t, w1B = w1r.next()
                        w3t, w3B = w3r.next()
                        kb.dma("pool", w1t[:], W1[:, fc * 128:(fc + 1) * 128].rearrange("(c p) f -> p c f", p=128), writes=[w1B])
                        kb.dma("pool", w3t[:], W3[:, fc * 128:(fc + 1) * 128].rearrange("(c p) f -> p c f", p=128), writes=[w3B])
                        pa = 2 + (fc % 2)
                        pbb = 4 + (fc % 2)
                        for c in range(8):
                            kb.op("pe", lambda e, c=c, w1t=w1t, ng=ng, pa=pa: e.matmul(PS[pa][:, 0:ng], lhsT=w1t[:, c, :], rhs=hT[:, c, 0:ng], start=(c == 0), stop=(c == 7)), reads=[w1B, hTB], writes=[PB[pa]])
                        for c in range(8):
                            kb.op("pe", lambda e, c=c, w3t=w3t, ng=ng, pbb=pbb: e.matmul(PS[pbb][:, 0:ng], lhsT=w3t[:, c, :], rhs=hT[:, c, 0:ng], start=(c == 0), stop=(c == 7)), reads=[w3B, hTB], writes=[PB[pbb]])
                        sa, saB = sar.next()
                        kb.op("act", lambda e, sa=sa, ng=ng, pa=pa: e.activation(out=sa[:, 0:ng], in_=PS[pa][:, 0:ng], func=AF.Silu), reads=[PB[pa]], writes=[saB])
                        kb.op("dve", lambda e, sa=sa, fc=fc, ng=ng, pbb=pbb: e.tensor_tensor(out=gT[:, fc, 0:ng], in0=PS[pbb][:, 0:ng], in1=sa[:, 0:ng], op=ALU.mult), reads=[PB[pbb], saB], writes=[gTB])
                    for ti in range(nt_):
                        for hf in range(2):
                            for fc in range(NFC):
                                kb.op("pe", lambda e, fc=fc, ti=ti, hf=hf, w2t=w2t: e.matmul(PS[6 + hf][:, :], lhsT=gT[:, fc, ti * 128:(ti + 1) * 128], rhs=w2t[:, fc, hf * 512:(hf + 1) * 512],
                                                                                     start=(fc == 0), stop=(fc == NFC - 1)), reads=[gTB, w2B], writes=[PB[6 + hf]])
                            a_ = acc[:, ti, hf * 512:(hf + 1) * 512]
                            if moe:
                                gsc = gates[:, ti, ei:ei + 1]
                                if ei == 0:
                                    kb.op("dve", lambda e, a_=a_, hf=hf, gsc=gsc: e.tensor_scalar(out=a_, in0=PS[6 + hf][:, :], scalar1=gsc, scalar2=None, op0=ALU.mult), reads=[PB[6 + hf], gatesB], writes=[accB])
                                else:
                                    kb.op("dve", lambda e, a_=a_, hf=hf, gsc=gsc: e.scalar_tensor_tensor(out=a_, in0=PS[6 + hf][:, :], scalar=gsc, in1=a_, op0=ALU.mult, op1=ALU.add),
                                          reads=[PB[6 + hf], gatesB, accB], writes=[accB])
                            else:
                                if ei == 0:
                                    kb.op("act", lambda e, a_=a_, hf=hf: e.activation(out=a_, in_=PS[6 + hf][:, :], func=AF.Identity), reads=[PB[6 + hf]], writes=[accB])
                                else:
                                    kb.op("dve", lambda e, a_=a_, hf=hf: e.tensor_tensor(out=a_, in0=PS[6 + hf][:, :], in1=a_, op=ALU.add), reads=[PB[6 + hf], accB], writes=[accB])
                for ti in range(nt_):
                    i = i0 + ti
                    if last and i < 2:
                        continue
                    a_ = acc[:, ti, :]
                    kb.op("dve", lambda e, a_=a_, w=w: e.tensor_tensor(out=a_, in0=a_, in1=gate2[:, w, :], op=ALU.mult), reads=[accB, gate2B], writes=[accB])
                    kb.op("dve", lambda e, a_=a_, ti=ti: e.scalar_tensor_tensor(out=a_, in0=x1g[:, ti, :], scalar=DN_ALPHA, in1=a_, op0=ALU.mult, op1=ALU.add), reads=[x1B, accB], writes=[accB])
                    sq_, sqB = sqr.next()
                    sm_, smB = smr.next()
                    layer_norm_tile(a_, accB, sq_, sqB, sm_, smB, ln2t, ln2B)
                    if last:
                        kb.dma("sp", OUT[(i - 2) * 128:(i - 1) * 128, :], a_, reads=[accB])
                    else:
                        kb.dma("sp", X[i * 128:(i + 1) * 128, :], a_, reads=[accB], writes=[dB["X"][i]])
        if upto == "X":
            break
        lstack.close()
        kb.stacks.pop()
    kb.barrier()
    return nc, dumps


def host_inputs(inputs, b):
    m = {n: np.ascontiguousarray(np.asarray(inputs[n], dtype=np.float32).reshape(s)) for n, s in PARAMS}
    m["x"] = np.ascontiguousarray(inputs["x"][b], dtype=np.float32)
    m["ctx"] = np.ascontiguousarray(inputs["ctx"][b], dtype=np.float32)
    cv = np.zeros((16, 128), np.float32)
    cv[0:8] = np.asarray(inputs["c"][b], np.float32).reshape(8, 128)
    cv[8:16] = np.asarray(inputs["c_ctx"], np.float32).reshape(8, 128)
    m["cvec"] = cv
    m["consts"] = make_consts()
    m["rope"] = make_rope()
    return m


def kernel(**inputs):
    nc, _ = build(n_layers=DEPTH, dump=None)
    base = {n: np.ascontiguousarray(np.asarray(inputs[n], dtype=np.float32).reshape(s)) for n, s in PARAMS}
    consts = make_consts()
    rope = make_rope()
    in_maps = []
    for b in range(8):
        m = dict(base)
        m["x"] = np.ascontiguousarray(np.asarray(inputs["x"][b], dtype=np.float32))
        m["ctx"] = np.ascontiguousarray(np.asarray(inputs["ctx"][b], dtype=np.float32))
        cv = np.zeros((16, 128), np.float32)
        cv[0:8] = np.asarray(inputs["c"][b], np.float32).reshape(8, 128)
        cv[8:16] = np.asarray(inputs["c_ctx"], np.float32).reshape(8, 128)
        m["cvec"] = cv
        m["consts"] = consts
        m["rope"] = rope
        in_maps.append(m)
    res = run_bass_kernel_spmd(nc, in_maps, core_ids=list(range(8)))
    out = np.stack([np.asarray(res.results[b]["out"], dtype=np.float32) for b in range(8)], 0)
    return out
```

```python
import math
import contextlib
import bisect
import numpy as np
import concourse.bass as bass
import concourse.mybir as mybir
from concourse.bass_utils import run_bass_kernel_spmd

F32 = mybir.dt.float32
BF16 = mybir.dt.bfloat16
F32R = mybir.dt.float32r
ALU = mybir.AluOpType
AF = mybir.ActivationFunctionType
AX = mybir.AxisListType

D = 1024
TC = 256
TL = 4096
T = TC + TL
NT = T // 128
DEPTH = 4
INW = 3264
DFF = 2816
DFE = 1408
NE = 8
DN_ALPHA = (2 * DEPTH) ** 0.25
LN_EPS = 1e-5
GN_EPS = 64e-5
SUBLN_EPS = 1e-5
CH = 64
NCH = T // CH
PR = T + 3
import os
SCAN_LIMIT = int(os.environ.get('SCAN_LIMIT', '0'))
STAGE = int(os.environ.get('STAGE', '99'))
SUB = int(os.environ.get('SUB', '0'))


def prow(tok):
    return 1 + tok if tok < TC else 2 + tok


class Buf:
    __slots__ = ("w", "r", "excl")

    def __init__(self, excl=False):
        self.w = None
        self.r = {}
        self.excl = excl


class KB:
    KD = 8

    def __init__(self, nc):
        self.nc = nc
        self.eng = {"pe": nc.tensor, "act": nc.scalar, "dve": nc.vector, "pool": nc.gpsimd, "sp": nc.sync}
        self.sem = {e: nc.alloc_semaphore("s_" + e) for e in self.eng}
        self.cnt = {e: 0 for e in self.eng}
        self.ins = {e: [] for e in self.eng}
        self.sigi = {e: [] for e in self.eng}
        self.seen = {e: {} for e in self.eng}
        self.dsem = {q: [nc.alloc_semaphore("d_%s%d" % (q, i)) for i in range(self.KD)] for q in ("sp", "pool", "act")}
        self.duse = {q: [0] * self.KD for q in self.dsem}
        self.di = {q: 0 for q in self.dsem}
        self.nsb = 0
        self.rec = None
        self.stacks = [contextlib.ExitStack()]

    def sb(self, shape, dt=F32, name=None):
        self.nsb += 1
        return self.stacks[-1].enter_context(self.nc.sbuf_tensor("t%d" % self.nsb, list(shape), dt))

    @contextlib.contextmanager
    def phase(self):
        self.stacks.append(contextlib.ExitStack())
        try:
            yield
        finally:
            self.barrier()
            self.stacks.pop().close()

    def wait(self, e, ev):
        if ev is None:
            return
        sem, val, key = ev
        if key == "pe" and e == "pe":
            return
        if sem is None:
            sl = self.sigi[key]
            j = bisect.bisect_left(sl, val)
            if j == len(sl):
                self.ins[key][val - 1].then_inc(self.sem[key], 1)
                sl.append(val)
            val = j + 1
            sem = self.sem[key]
        if self.seen[e].get(key, 0) >= val:
            return
        self.eng[e].wait_ge(sem, val)
        self.seen[e][key] = val

    def _deps(self, e, reads, writes):
        for b in reads:
            self.wait(e, b.w)
            if b.excl:
                for k_, ev in b.r.items():
                    if k_ != e:
                        self.wait(e, ev)
        for b in writes:
            self.wait(e, b.w)
            for ev in b.r.values():
                self.wait(e, ev)

    def _post(self, ev, reads, writes):
        for b in reads:
            b.r[ev[2]] = ev
        for b in writes:
            b.w = ev
            b.r = {}

    def interleave(self, it, k=2):
        items = list(it)
        for j in range(0, len(items), k):
            recs = []
            for x in items[j:j + k]:
                self.rec = []
                yield x
                recs.append(self.rec)
            self.rec = None
            for t in range(max(len(r) for r in recs)):
                for r in recs:
                    if t < len(r):
                        kind, a = r[t]
                        if kind == "op":
                            self.op(*a)
                        else:
                            self.dma(*a)

    def op(self, e, fn, reads=(), writes=()):
        if self.rec is not None:
            self.rec.append(("op", (e, fn, tuple(reads), tuple(writes))))
            return None
        self._deps(e, reads, writes)
        ins = fn(self.eng[e])
        self.cnt[e] += 1
        self.ins[e].append(ins)
        ev = (None, self.cnt[e], e)
        self._post(ev, reads, writes)
        return ev

    def dma(self, q, out, in_, reads=(), writes=()):
        if self.rec is not None:
            self.rec.append(("dma", (q, out, in_, tuple(reads), tuple(writes))))
            return None
        self._deps(q, reads, writes)
        k = self.di[q] % self.KD
        self.di[q] += 1
        key = "d_%s%d" % (q, k)
        sem = self.dsem[q][k]
        if self.duse[q][k] > 0:
            self.wait(q, (sem, 16 * self.duse[q][k], key))
        ins = self.eng[q].dma_start(out=out, in_=in_)
        self.duse[q][k] += 1
        ins.then_inc(sem, 16)
        ev = (sem, 16 * self.duse[q][k], key)
        self._post(ev, reads, writes)
        return ev

    def barrier(self):
        evs = [(None, self.cnt[e], e) for e in self.eng if self.cnt[e] > 0]
        for q in self.dsem:
            for k in range(self.KD):
                if self.duse[q][k] > 0:
                    evs.append((self.dsem[q][k], 16 * self.duse[q][k], "d_%s%d" % (q, k)))
        for e in self.eng:
            for ev in evs:
                self.wait(e, ev)


class Ring:
    def __init__(self, kb, n, shape, dt=F32):
        self.t = [kb.sb(shape, dt) for _ in range(n)]
        self.b = [Buf() for _ in range(n)]
        self.i = 0

    def next(self):
        j = self.i % len(self.t)
        self.i += 1
        return self.t[j], self.b[j]


def make_consts():
    c = np.zeros((128, 1024), np.float32)
    c[:, 0:128] = np.eye(128, dtype=np.float32)
    ii = np.arange(CH)
    for d in range(2):
        before = (ii[:, None] > ii[None, :]) if d == 1 else (ii[:, None] < ii[None, :])
        beq = before | np.eye(CH, dtype=bool)
        c[0:CH, 128 + d * 128:128 + d * 128 + 64] = before
        c[0:CH, 128 + d * 128 + 64:128 + d * 128 + 128] = beq
        c[0:CH, 384 + d * 64:384 + d * 64 + 64] = before.T
        c[0:CH, 512 + d * 64:512 + d * 64 + 64] = beq
    c[:, 640:768] = 1.0
    c[0, 768:896] = 1.0
    c[1, 896:1024] = 1.0
    return c


def make_rope():
    rows = TL // 64
    row = np.repeat(np.arange(rows), 64).astype(np.float64)
    col = np.tile(np.arange(64), rows).astype(np.float64)
    inv = 10000.0 ** (-np.arange(0, 32, 2, dtype=np.float64) / 32)
    ar = row[:, None] * inv
    ac = col[:, None] * inv
    cosT = np.concatenate([np.cos(ar), np.cos(ar), np.cos(ac), np.cos(ac)], 1)
    sinT = np.concatenate([-np.sin(ar), np.sin(ar), -np.sin(ac), np.sin(ac)], 1)
    return np.concatenate([cosT, sinT], 1).astype(np.float32)


PARAMS = [("w_ada", [DEPTH, D, 6 * D]), ("b_ada", [DEPTH, 6 * D]), ("w_in", [DEPTH, D, INW]), ("w_out", [DEPTH, D, D]),
          ("ln1_g", [DEPTH, D]), ("ln1_b", [DEPTH, D]), ("ln2_g", [DEPTH, D]), ("ln2_b", [DEPTH, D]),
          ("lam_q1", [DEPTH, 64]), ("lam_k1", [DEPTH, 64]), ("lam_q2", [DEPTH, 64]), ("lam_k2", [DEPTH, 64]),
          ("subln_g", [DEPTH, 128]), ("rkv_conv", [DEPTH, 3, 768]), ("decay_w0", [DEPTH, 2, 256]),
          ("decay_up", [DEPTH, 2, 32, 256]), ("iclr_a0", [DEPTH, 2, 256]), ("iclr_up", [DEPTH, 2, 32, 256]),
          ("gate_up", [DEPTH, 64, 256]), ("k_k", [DEPTH, 256]), ("k_a", [DEPTH, 256]), ("r_k", [DEPTH, 256]),
          ("gn_g", [DEPTH, 256]), ("gn_b", [DEPTH, 256]), ("conv_w", [DEPTH, 3, 256]),
          ("ffn_w1", [2, D, DFF]), ("ffn_w3", [2, D, DFF]), ("ffn_w2", [2, DFF, D]),
          ("router_w", [2, D, NE]), ("router_b", [2, NE]),
          ("moe_w1", [2, NE, D, DFE]), ("moe_w3", [2, NE, D, DFE]), ("moe_w2", [2, NE, DFE, D])]


def build(n_layers=DEPTH, dump=None):
    nc = bass.Bass("TRN2", target_bir_lowering=False)
    kb = KB(nc)
    I = {}
    I["x"] = nc.dram_tensor("x", [TL, D], F32, kind="ExternalInput").ap()
    I["ctx"] = nc.dram_tensor("ctx", [TC, D], F32, kind="ExternalInput").ap()
    I["cvec"] = nc.dram_tensor("cvec", [16, 128], F32, kind="ExternalInput").ap()
    I["consts"] = nc.dram_tensor("consts", [128, 1024], F32, kind="ExternalInput").ap()
    I["rope"] = nc.dram_tensor("rope", [TL, 128], F32, kind="ExternalInput").ap()
    for n, s in PARAMS:
        I[n] = nc.dram_tensor(n, s, F32, kind="ExternalInput").ap()
    OUT = nc.dram_tensor("out", [TL, D], F32, kind="ExternalOutput").ap()
    dumps = {}

    def scratch(name, shape, dt=F32):
        if dump and name in dump:
            dumps[name] = nc.dram_tensor("dump_" + name, shape, dt, kind="ExternalOutput").ap()
            return dumps[name]
        return nc.dram_tensor("scr_" + name, shape, dt).ap()

    X = scratch("X", [T, D])
    X1 = scratch("X1", [T, D])
    QT = scratch("QT", [4, 128, T], BF16)
    KT = scratch("KT", [4, 128, T], BF16)
    VV = scratch("VV", [T, 512], BF16)
    RKV = scratch("RKV", [PR, 768])
    CIN = scratch("CIN", [PR, 768])
    LORA = scratch("LORA", [T, 192])
    MIX = scratch("MIX", [T, D])
    PREP = scratch("PREP", [T, 2564])
    YD = scratch("YD", [2, T, 256])
    MODD = scratch("MODD", [2, 6 * D])
    dB = {n: [Buf() for _ in range(NT + 2)] for n in ("X", "X1", "QK", "VV", "RKV", "CIN", "LORA", "MIX", "PREP", "YD0", "YD1")}

    cst = kb.sb([128, 1024], F32, "cst")
    cstB = Buf()
    kb.dma("sp", cst[:], I["consts"][:, :], writes=[cstB])
    ident = cst[:, 0:128]
    zrow = kb.sb([1, 768], F32, "zrow")
    zB = Buf()
    kb.op("pool", lambda e: e.memset(zrow[:], 0.0), writes=[zB])
    for r_ in (0, TC + 1, PR - 1):
        kb.dma("sp", RKV[r_:r_ + 1, :], zrow[:], reads=[zB])
        kb.dma("sp", CIN[r_:r_ + 1, :], zrow[:], reads=[zB])
    PS = [nc.alloc_psum_tensor("ps%d" % i, [128, 512], F32) for i in range(8)]
    PB = [Buf(excl=True) for _ in range(8)]
    kb.barrier()

    def xsrc(l, i):
        if l == 0:
            return I["ctx"][i * 128:(i + 1) * 128, :] if i < 2 else I["x"][(i - 2) * 128:(i - 1) * 128, :]
        return X[i * 128:(i + 1) * 128, :]


    upto = dump[0] if dump else None
    stop = [False]

    for l in range(n_layers):
        last = (l == DEPTH - 1)
        lam_init = 0.8 - 0.6 * math.exp(-0.3 * l)
        lstack = contextlib.ExitStack()
        kb.stacks.append(lstack)
        modS = kb.sb([128, 2, 4, 8], F32)
        modSB = Buf()
        mrowDB = Buf()

        def load_gate_ln(which):
            gB_ = kb.sb([128, 2, D], F32)
            gBB_ = Buf()
            ln_ = kb.sb([128, 2, D], F32)
            lnB_ = Buf()
            v = 2 if which == 0 else 5
            for w_ in range(2):
                kb.dma("sp", gB_[:, w_, :], MODD[w_, v * D:(v + 1) * D].partition_broadcast(128), reads=[mrowDB], writes=[gBB_])
            for j, n in enumerate((("ln1_g", "ln1_b") if which == 0 else ("ln2_g", "ln2_b"))):
                kb.dma("sp", ln_[:, j, :], I[n][l, :].partition_broadcast(128), writes=[lnB_])
            return gB_, gBB_, ln_, lnB_

        with kb.phase():
            cv = kb.sb([16, 128], F32)
            cvB = Buf()
            kb.dma("sp", cv[:], I["cvec"][:, :], writes=[cvB])
            s2 = kb.sb([128, 8, 2], F32)
            s2B = Buf()
            kb.op("pe", lambda e: e.transpose(out=PS[0][:, 0:16], in_=cv[:], identity=cst[0:16, 0:16]), reads=[cvB, cstB], writes=[PB[0]])
            kb.op("act", lambda e: e.activation(out=s2[:].rearrange("p c w -> p w c"), in_=PS[0][:, 0:16].rearrange("p (w c) -> p w c", w=2), func=AF.Silu),
                  reads=[PB[0]], writes=[s2B])
            wr = Ring(kb, 2, [128, 8, 512], F32)
            ba = kb.sb([2, 6 * D], F32)
            baB = Buf()
            kb.dma("sp", ba[0:1, :], I["b_ada"][l:l + 1, :], writes=[baB])
            kb.dma("sp", ba[1:2, :], I["b_ada"][l:l + 1, :], writes=[baB])
            mrow = kb.sb([2, 6 * D], F32)
            mrowB = Buf()
            for n in range(12):
                wt, wb = wr.next()
                kb.dma("sp", wt[:], I["w_ada"][l, :, n * 512:(n + 1) * 512].rearrange("(c p) n -> p c n", p=128), writes=[wb])
                pb = 1 + (n % 2)
                for c in range(8):
                    kb.op("pe", lambda e, c=c, wt=wt, pb=pb: e.matmul(PS[pb][0:2, :], lhsT=s2[:, c, :], rhs=wt[:, c, :], start=(c == 0), stop=(c == 7)),
                          reads=[s2B, wb], writes=[PB[pb]])
                kb.op("dve", lambda e, n=n, pb=pb: e.tensor_tensor(out=mrow[:, n * 512:(n + 1) * 512], in0=PS[pb][0:2, :], in1=ba[:, n * 512:(n + 1) * 512], op=ALU.add),
                      reads=[PB[pb], baB], writes=[mrowB])
            for v in (1, 4):
                kb.op("dve", lambda e, v=v: e.tensor_scalar(out=mrow[:, v * D:(v + 1) * D], in0=mrow[:, v * D:(v + 1) * D], scalar1=1.0, scalar2=None, op0=ALU.add),
                      reads=[mrowB], writes=[mrowB])
            kb.dma("sp", MODD[:, :], mrow[:], reads=[mrowB], writes=[mrowDB])
            for j, v in enumerate((0, 1, 3, 4)):
                for c in range(8):
                    o = (j * 8 + c) * 2
                    kb.op("pe", lambda e, o=o, v=v, c=c: e.matmul(PS[3][:, o:o + 2], lhsT=mrow[0:2, v * D + c * 128:v * D + (c + 1) * 128],
                                                              rhs=cst[0:2, 0:2], start=True, stop=True), reads=[mrowB, cstB], writes=[PB[3]])
            kb.op("act", lambda e: e.activation(out=modS[:].rearrange("p w j c -> p j c w"), in_=PS[3][:, 0:64].rearrange("p (j c w) -> p j c w", j=4, c=8), func=AF.Identity),
                  reads=[PB[3]], writes=[modSB])
        if upto == "MODD":
            break

        with kb.phase():
            win = kb.sb([128, 8, INW], BF16)
            winB = Buf()
            for c in range(8):
                kb.dma("pool", win[:, c, :], I["w_in"][l, c * 128:(c + 1) * 128, :], writes=[winB])
            xr = Ring(kb, 2, [128, D], F32)
            hT = Ring(kb, 2, [128, 8, 128], BF16)
            qk = Ring(kb, 2, [128, 1024], F32)
            qkr = Ring(kb, 2, [128, 1024], F32)
            qkt = Ring(kb, 2, [128, 1024], F32)
            rp = Ring(kb, 2, [128, 128], F32)
            qkT = Ring(kb, 2, [128, 8, 128], BF16)
            vb = Ring(kb, 2, [128, 512], BF16)
            pj = Ring(kb, 2, [128, 1728], F32)
            chunks = [(0, 512), (512, 1024), (1024, 1536), (1536, 2048), (2048, 2496), (2496, 3008), (3008, 3264)]
            for i in range(NT):
                w = 1 if i < 2 else 0
                pr0 = prow(i * 128)
                xt, xb = xr.next()
                kb.dma("sp", xt[:], xsrc(l, i), reads=([dB["X"][i]] if l > 0 else []), writes=[xb])
                for c in range(8):
                    kb.op("pe", lambda e, c=c, xt=xt: e.transpose(out=PS[c // 4][:, (c % 4) * 128:(c % 4 + 1) * 128], in_=xt[:, c * 128:(c + 1) * 128], identity=ident),
                          reads=[xb, cstB], writes=[PB[c // 4]])
                ht, hb = hT.next()
                for c in range(8):
                    kb.op("act", lambda e, c=c, ht=ht, w=w: e.activation(out=ht[:, c, :], in_=PS[c // 4][:, (c % 4) * 128:(c % 4 + 1) * 128], func=AF.Identity,
                                                                      scale=modS[:, w, 1, c:c + 1], bias=modS[:, w, 0, c:c + 1]),
                          reads=[PB[c // 4], modSB], writes=[hb])
                qt, qb = qk.next()
                vt, vbb = vb.next()
                pt, pjb = pj.next()
                for n, (a, b) in enumerate(chunks):
                    pb = 2 + (n % 6)
                    for c in range(8):
                        kb.op("pe", lambda e, c=c, ht=ht, pb=pb, a=a, b=b: e.matmul(PS[pb][:, 0:b - a], lhsT=ht[:, c, :], rhs=win[:, c, a:b], start=(c == 0), stop=(c == 7)),
                              reads=[hb, winB], writes=[PB[pb]])
                    if n < 2:
                        kb.op("act", lambda e, n=n, pb=pb, qt=qt: e.activation(out=qt[:, n * 512:(n + 1) * 512], in_=PS[pb][:, :], func=AF.Identity), reads=[PB[pb]], writes=[qb])
                    elif n == 2:
                        kb.op("dve", lambda e, pb=pb, vt=vt: e.tensor_copy(out=vt[:], in_=PS[pb][:, :]), reads=[PB[pb]], writes=[vbb])
                    else:
                        eng = "dve" if n % 2 == 0 else "act"
                        if eng == "dve":
                            kb.op("dve", lambda e, pb=pb, pt=pt, a=a, b=b: e.tensor_copy(out=pt[:, a - 1536:b - 1536], in_=PS[pb][:, 0:b - a]), reads=[PB[pb]], writes=[pjb])
                        else:
                            kb.op("act", lambda e, pb=pb, pt=pt, a=a, b=b: e.activation(out=pt[:, a - 1536:b - 1536], in_=PS[pb][:, 0:b - a], func=AF.Identity), reads=[PB[pb]], writes=[pjb])
                kb.dma("sp", VV[i * 128:(i + 1) * 128, :], vt[:], reads=[vbb], writes=[dB["VV"][i]])
                kb.dma("sp", RKV[pr0:pr0 + 128, :], pt[:, 0:768], reads=[pjb], writes=[dB["RKV"][i]])
                kb.dma("sp", LORA[i * 128:(i + 1) * 128, :], pt[:, 768:960], reads=[pjb], writes=[dB["LORA"][i]])
                kb.dma("sp", CIN[pr0:pr0 + 128, :], pt[:, 960:1728], reads=[pjb], writes=[dB["CIN"][i]])
                src, srcb = qt, qb
                if i >= 2:
                    rt, rb = rp.next()
                    kb.dma("sp", rt[:], I["rope"][(i - 2) * 128:(i - 1) * 128, :], writes=[rb])
                    q1, q1b = qkr.next()
                    q2, q2b = qkt.next()
                    qv = qt[:].rearrange("p (g a h n) -> p g a h n", g=16, a=2, h=2)
                    kb.op("dve", lambda e, qt=qt, q1=q1, rt=rt: e.tensor_tensor(out=q1[:].rearrange("p (g n) -> p g n", g=16), in0=qt[:].rearrange("p (g n) -> p g n", g=16),
                                                                       in1=rt[:, 0:64].unsqueeze(1).to_broadcast([128, 16, 64]), op=ALU.mult), reads=[qb, rb], writes=[q1b])
                    q2v = q2[:].rearrange("p (g a h n) -> p g a h n", g=16, a=2, h=2)
                    sv = rt[:, 64:128].rearrange("p (a h n) -> p a h n", a=2, h=2)
                    for hh in range(2):
                        kb.op("pool", lambda e, hh=hh, qv=qv, q2v=q2v, sv=sv: e.tensor_tensor(out=q2v[:, :, :, hh, :], in0=qv[:, :, :, 1 - hh, :],
                                                                                     in1=sv[:, :, hh, :].unsqueeze(1).to_broadcast([128, 16, 2, 16]), op=ALU.mult),
                              reads=[qb, rb], writes=[q2b])
                    kb.op("dve", lambda e, q1=q1, q2=q2: e.tensor_tensor(out=q1[:], in0=q1[:], in1=q2[:], op=ALU.add), reads=[q1b, q2b], writes=[q1b])
                    src, srcb = q1, q1b
                for c in range(8):
                    kb.op("pe", lambda e, c=c, src=src: e.transpose(out=PS[c // 4][:, (c % 4) * 128:(c % 4 + 1) * 128], in_=src[:, c * 128:(c + 1) * 128], identity=ident),
                          reads=[srcb, cstB], writes=[PB[c // 4]])
                tt, tb = qkT.next()
                for hf in range(2):
                    kb.op("dve" if hf == 0 else "act",
                          (lambda e, tt=tt: e.tensor_copy(out=tt[:, 0:4, :], in_=PS[0][:, :].rearrange("p (c n) -> p c n", c=4))) if hf == 0 else
                          (lambda e, tt=tt: e.activation(out=tt[:, 4:8, :], in_=PS[1][:, :].rearrange("p (c n) -> p c n", c=4), func=AF.Identity)),
                          reads=[PB[hf]], writes=[tb])
                kb.dma("sp", QT[:, :, i * 128:(i + 1) * 128].rearrange("h p t -> p h t"), tt[:, 0:4, :], reads=[tb], writes=[dB["QK"][i]])
                kb.dma("sp", KT[:, :, i * 128:(i + 1) * 128].rearrange("h p t -> p h t"), tt[:, 4:8, :], reads=[tb], writes=[dB["QK"][i]])
        if upto in ("QT", "RKV", "VV"):
            break

        with kb.phase():
            lq = kb.sb([128, 4, 64], F32)
            lqB = Buf()
            for j, n in enumerate(("lam_q1", "lam_k1", "lam_q2", "lam_k2")):
                kb.dma("sp", lq[:, j, :], I[n][l, :].partition_broadcast(128), writes=[lqB])
            lt = kb.sb([128, 2, 64], F32)
            lv = kb.sb([128, 4], F32)
            lvB = Buf()
            kb.op("dve", lambda e: e.tensor_tensor(out=lt[:], in0=lq[:, 0:4:2, :], in1=lq[:, 1:4:2, :], op=ALU.mult), reads=[lqB], writes=[lvB])
            kb.op("dve", lambda e: e.tensor_reduce(out=lv[:, 0:2], in_=lt[:], axis=AX.X, op=ALU.add), reads=[lvB], writes=[lvB])
            kb.op("act", lambda e: e.activation(out=lv[:, 0:2], in_=lv[:, 0:2], func=AF.Exp), reads=[lvB], writes=[lvB])
            kb.op("dve", lambda e: e.tensor_tensor(out=lv[:, 2:3], in0=lv[:, 1:2], in1=lv[:, 0:1], op=ALU.subtract), reads=[lvB], writes=[lvB])
            kb.op("dve", lambda e: e.tensor_scalar(out=lv[:, 3:4], in0=lv[:, 2:3], scalar1=-lam_init, scalar2=None, op0=ALU.add), reads=[lvB], writes=[lvB])
            nlam = lv[:, 3:4]
            sg = kb.sb([128, 128], F32)
            sgB = Buf()
            kb.dma("sp", sg[:], I["subln_g"][l, :].partition_broadcast(128), writes=[sgB])
            kb.op("dve", lambda e: e.tensor_scalar(out=sg[:], in0=sg[:], scalar1=(1.0 - lam_init), scalar2=None, op0=ALU.mult), reads=[sgB], writes=[sgB])
            qT0 = kb.sb([128, T], BF16)
            qT1 = kb.sb([128, T], BF16)
            qTm = [qT0, qT1]
            kTt = kb.sb([128, T], BF16)
            vaug = kb.sb([128, NT, 129], BF16)
            qkvB = Buf()
            kb.op("pool", lambda e: e.memset(vaug[:], 1.0), writes=[qkvB])
            kb.op("pool", lambda e: e.memset(qT0[:], 0.0), writes=[qkvB])
            kb.op("pool", lambda e: e.memset(qT1[:], 0.0), writes=[qkvB])
            PTring = Ring(kb, 4, [128, NT, 512], BF16)
            att = Ring(kb, 2, [128, 128], F32)
            sm = Ring(kb, 2, [128, 8], F32)
            sqt = Ring(kb, 2, [128, 128], F32)
            sbank = [0]
            for h in range(4):
                kb.dma("sp", qT0[0:64, :], QT[h, 0:64, :], reads=dB["QK"][0:NT], writes=[qkvB])
                kb.dma("sp", qT1[64:128, :], QT[h, 64:128, :], reads=dB["QK"][0:NT], writes=[qkvB])
                kb.dma("sp", kTt[:], KT[h, :, :], reads=dB["QK"][0:NT], writes=[qkvB])
                kb.dma("sp", vaug[:, :, 0:128], VV[:, h * 128:(h + 1) * 128].rearrange("(n p) d -> p n d", p=128), reads=dB["VV"][0:NT], writes=[qkvB])
                blocks = ([] if last else [(0, 256, [0, 1])]) + [(256 + 512 * j, 512, list(range(NT))) for j in range(8)]
                prevB = None

                def merge_emit(A, Bp):
                    kb.rec = None
                    nA = max(len(A), 1)
                    nB = len(Bp) if Bp else 0
                    jb = 0
                    for ia, (kind, a_) in enumerate(A):
                        (kb.op if kind == "op" else kb.dma)(*a_)
                        if ia % 2 == 1 or ia == len(A) - 1:
                            tgt = nB * (ia + 1) // nA
                            while jb < tgt:
                                kind2, b_ = Bp[jb]
                                (kb.op if kind2 == "op" else kb.dma)(*b_)
                                jb += 1
                    while jb < nB:
                        kind2, b_ = Bp[jb]
                        (kb.op if kind2 == "op" else kb.dma)(*b_)
                        jb += 1

                for (q0, nq, kts) in blocks:
                    kb.rec = []
                    PTs, PTB = [None, None], [None, None]
                    for m in range(2):
                        PTs[m], PTB[m] = PTring.next()
                    for ki, kt in enumerate(kts):
                        for m in range(2):
                            pb = sbank[0] % 4
                            sbank[0] += 1
                            kb.op("pe", lambda e, pb=pb, m=m, kt=kt, q0=q0, nq=nq: e.matmul(PS[pb][:, 0:nq], lhsT=kTt[:, kt * 128:(kt + 1) * 128],
                                                                                     rhs=qTm[m][:, q0:q0 + nq], start=True, stop=True),
                                  reads=[qkvB], writes=[PB[pb]])
                            kb.op("act", lambda e, pb=pb, pt_=PTs[m], ki=ki, nq=nq: e.activation(out=pt_[:, ki, 0:nq], in_=PS[pb][:, 0:nq], func=AF.Exp, scale=0.125),
                                  reads=[PB[pb]], writes=[PTB[m]])
                    recA = kb.rec
                    kb.rec = []
                    for qs in range(nq // 128):
                        for m in range(2):
                            ob = 4 + 2 * (qs % 2) + m
                            for ki, kt in enumerate(kts):
                                kb.op("pe", lambda e, ob=ob, pt_=PTs[m], ki=ki, kt=kt, qs=qs, n=len(kts): e.matmul(PS[ob][:, 0:129], lhsT=pt_[:, ki, qs * 128:(qs + 1) * 128], rhs=vaug[:, kt, :],
                                                                                            start=(ki == 0), stop=(ki == n - 1)),
                                      reads=[PTB[m], qkvB], writes=[PB[ob]])
                        o0 = 4 + 2 * (qs % 2)
                        o1 = o0 + 1
                        st_, sB = sm.next()
                        at, aB = att.next()
                        sq_, sqB = sqt.next()
                        kb.op("dve", lambda e, st_=st_, o0=o0: e.reciprocal(out=st_[:, 0:1], in_=PS[o0][:, 128:129]), reads=[PB[o0]], writes=[sB])
                        kb.op("dve", lambda e, st_=st_, o1=o1: e.reciprocal(out=st_[:, 1:2], in_=PS[o1][:, 128:129]), reads=[PB[o1]], writes=[sB])
                        kb.op("dve", lambda e, st_=st_: e.tensor_tensor(out=st_[:, 2:3], in0=st_[:, 1:2], in1=nlam, op=ALU.mult), reads=[sB, lvB], writes=[sB])
                        kb.op("dve", lambda e, st_=st_, at=at, o0=o0: e.tensor_scalar(out=at[:], in0=PS[o0][:, 0:128], scalar1=st_[:, 0:1], scalar2=None, op0=ALU.mult),
                              reads=[PB[o0], sB], writes=[aB])
                        kb.op("dve", lambda e, st_=st_, at=at, o1=o1: e.scalar_tensor_tensor(out=at[:], in0=PS[o1][:, 0:128], scalar=st_[:, 2:3], in1=at[:], op0=ALU.mult, op1=ALU.add),
                              reads=[PB[o1], sB, aB], writes=[aB])
                        kb.op("pool", lambda e, at=at, sq_=sq_: e.tensor_tensor(out=sq_[:], in0=at[:], in1=at[:], op=ALU.mult), reads=[aB], writes=[sqB])
                        kb.op("dve", lambda e, st_=st_, sq_=sq_: e.tensor_reduce(out=st_[:, 3:4], in_=sq_[:], axis=AX.X, op=ALU.add), reads=[sqB], writes=[sB])
                        kb.op("dve", lambda e, st_=st_: e.tensor_scalar(out=st_[:, 4:5], in0=st_[:, 3:4], scalar1=1.0 / 128, scalar2=SUBLN_EPS, op0=ALU.mult, op1=ALU.add), reads=[sB], writes=[sB])
                        kb.op("act", lambda e, st_=st_: e.activation(out=st_[:, 5:6], in_=st_[:, 4:5], func=AF.Sqrt), reads=[sB], writes=[sB])
                        kb.op("dve", lambda e, st_=st_: e.reciprocal(out=st_[:, 6:7], in_=st_[:, 5:6]), reads=[sB], writes=[sB])
                        kb.op("dve", lambda e, st_=st_, at=at: e.scalar_tensor_tensor(out=at[:], in0=at[:], scalar=st_[:, 6:7], in1=sg[:], op0=ALU.mult, op1=ALU.mult),
                              reads=[sB, aB, sgB], writes=[aB])
                        t0 = q0 + qs * 128
                        kb.dma("sp", MIX[t0:t0 + 128, h * 128:(h + 1) * 128], at[:], reads=[aB], writes=[dB["MIX"][t0 // 128]])
                    recB = kb.rec
                    merge_emit(recA, prevB)
                    prevB = recB
                merge_emit([], prevB)

        with kb.phase():
            cw = kb.sb([128, 3, 256], F32)
            cwB = Buf()
            for j in range(3):
                kb.dma("sp", cw[:, j, :], I["conv_w"][l, j, :].partition_broadcast(128), writes=[cwB])
            c3 = Ring(kb, 4, [128, 3, 768], F32)
            u3 = Ring(kb, 4, [128, 3, 256], F32)
            co = Ring(kb, 4, [128, 256], F32)
            for i in kb.interleave(range(2 if last else 0, NT), 4):
                pr0 = prow(i * 128)
                ct, cB = c3.next()
                for j in range(3):
                    kb.dma("sp", ct[:, j, :], CIN[pr0 - 1 + j:pr0 - 1 + j + 128, :], reads=dB["CIN"][max(i - 1, 0):i + 2], writes=[cB])
                ut, uB = u3.next()
                ot, oB = co.next()
                kb.op("pool", lambda e, ct=ct, ut=ut: e.tensor_tensor(out=ut[:], in0=ct[:, :, 512:768], in1=ct[:, :, 0:256], op=ALU.mult), reads=[cB], writes=[uB])
                kb.op("dve", lambda e, ut=ut: e.tensor_tensor(out=ut[:], in0=ut[:], in1=cw[:], op=ALU.mult), reads=[uB, cwB], writes=[uB])
                kb.op("dve", lambda e, ut=ut, ot=ot: e.tensor_tensor(out=ot[:], in0=ut[:, 0, :], in1=ut[:, 1, :], op=ALU.add), reads=[uB], writes=[oB])
                kb.op("dve", lambda e, ut=ut, ot=ot: e.tensor_tensor(out=ot[:], in0=ot[:], in1=ut[:, 2, :], op=ALU.add), reads=[uB, oB], writes=[oB])
                kb.op("dve", lambda e, ct=ct, ot=ot: e.tensor_tensor(out=ot[:], in0=ot[:], in1=ct[:, 1, 256:512], op=ALU.mult), reads=[cB, oB], writes=[oB])
                kb.dma("sp", MIX[i * 128:(i + 1) * 128, 768:1024], ot[:], reads=[oB], writes=[dB["MIX"][i]])

        with kb.phase():
            cw3 = kb.sb([128, 3, 768], F32)
            cw3B = Buf()
            for j in range(3):
                kb.dma("sp", cw3[:, j, :], I["rkv_conv"][l, j, :].partition_broadcast(128), writes=[cw3B])
            vecs = kb.sb([128, 3, 256], F32)
            vecsB = Buf()
            for j, n in enumerate(("k_k", "k_a", "r_k")):
                kb.dma("sp", vecs[:, j, :], I[n][l, :].partition_broadcast(128), writes=[vecsB])
            wup = kb.sb([33, 2, 256], F32)
            aup = kb.sb([33, 2, 256], F32)
            gup = kb.sb([64, 256], F32)
            wB = Buf()
            for d in range(2):
                kb.dma("sp", wup[0:32, d, :], I["decay_up"][l, d, :, :], writes=[wB])
                kb.dma("sp", wup[32:33, d, :], I["decay_w0"][l, d:d + 1, :], writes=[wB])
                kb.dma("sp", aup[0:32, d, :], I["iclr_up"][l, d, :, :], writes=[wB])
                kb.dma("sp", aup[32:33, d, :], I["iclr_a0"][l, d:d + 1, :], writes=[wB])
            kb.dma("sp", gup[:], I["gate_up"][l, :, :], writes=[wB])
            lwl = Ring(kb, 2, [33, 2, 128], F32)
            lal = Ring(kb, 2, [33, 2, 128], F32)
            lgl = Ring(kb, 2, [64, 128], F32)
            for rg in (lwl, lal):
                for t_, b_ in zip(rg.t, rg.b):
                    kb.op("pool", lambda e, t_=t_: e.memset(t_[:], 1.0), writes=[b_])
            r3 = Ring(kb, 2, [128, 3, 768], F32)
            lo = Ring(kb, 2, [128, 192], F32)
            rkvr = Ring(kb, 2, [128, 768], F32)
            po = Ring(kb, 2, [128, 2564], F32)
            av = Ring(kb, 2, [128, 512], F32)
            tw = Ring(kb, 2, [128, 512], F32)
            krr = Ring(kb, 2, [128, 256], F32)
            t1r = Ring(kb, 2, [128, 256], F32)
            t2r = Ring(kb, 2, [128, 256], F32)
            smr = Ring(kb, 2, [128, 16], F32)
            for i in kb.interleave(range(NT), 2):
                pq = 4 * (i % 2)
                pr0 = prow(i * 128)
                rt, rB = r3.next()
                for j in range(3):
                    kb.dma("sp", rt[:, j, :], RKV[pr0 - 1 + j:pr0 - 1 + j + 128, :], reads=dB["RKV"][max(i - 1, 0):i + 2], writes=[rB])
                lt_, lB = lo.next()
                kb.dma("sp", lt_[:], LORA[i * 128:(i + 1) * 128, :], reads=[dB["LORA"][i]], writes=[lB])
                kv, kvB = rkvr.next()
                kb.op("pool", lambda e, rt=rt: e.tensor_tensor(out=rt[:], in0=rt[:], in1=cw3[:], op=ALU.mult), reads=[rB, cw3B], writes=[rB])
                kb.op("dve", lambda e, rt=rt, kv=kv: e.tensor_tensor(out=kv[:], in0=rt[:, 0, :], in1=rt[:, 1, :], op=ALU.add), reads=[rB], writes=[kvB])
                kb.op("dve", lambda e, rt=rt, kv=kv: e.tensor_tensor(out=kv[:], in0=kv[:], in1=rt[:, 2, :], op=ALU.add), reads=[rB, kvB], writes=[kvB])
                r_ = kv[:, 0:256]
                k_ = kv[:, 256:512]
                v_ = kv[:, 512:768]
                for j in range(4):
                    kb.op("pe", lambda e, pq=pq, j=j, lt_=lt_: e.transpose(out=PS[pq + 0][0:32, j * 128:(j + 1) * 128], in_=lt_[:, j * 32:(j + 1) * 32], identity=ident), reads=[lB, cstB], writes=[PB[pq + 0]])
                kb.op("pe", lambda e, pq=pq, lt_=lt_: e.transpose(out=PS[pq + 1][0:64, 0:128], in_=lt_[:, 128:192], identity=ident), reads=[lB, cstB], writes=[PB[pq + 1]])
                wl_, wlB = lwl.next()
                al_, alB = lal.next()
                gl_, glB = lgl.next()
                kb.op("act", lambda e, pq=pq, wl_=wl_: e.activation(out=wl_[0:32, :, :], in_=PS[pq + 0][0:32, 0:256].rearrange("p (d n) -> p d n", d=2), func=AF.Tanh), reads=[PB[pq + 0]], writes=[wlB])
                kb.op("act", lambda e, pq=pq, al_=al_: e.activation(out=al_[0:32, :, :], in_=PS[pq + 0][0:32, 256:512].rearrange("p (d n) -> p d n", d=2), func=AF.Identity), reads=[PB[pq + 0]], writes=[alB])
                kb.op("act", lambda e, pq=pq, gl_=gl_: e.activation(out=gl_[:], in_=PS[pq + 1][0:64, 0:128], func=AF.Sigmoid), reads=[PB[pq + 1]], writes=[glB])
                for d in range(2):
                    kb.op("pe", lambda e, pq=pq, d=d, wl_=wl_: e.matmul(PS[pq + 2][:, d * 256:(d + 1) * 256], lhsT=wl_[0:33, d, :], rhs=wup[0:33, d, :], start=True, stop=True), reads=[wlB, wB], writes=[PB[pq + 2]])
                    kb.op("pe", lambda e, pq=pq, d=d, al_=al_: e.matmul(PS[pq + 3][:, d * 256:(d + 1) * 256], lhsT=al_[0:33, d, :], rhs=aup[0:33, d, :], start=True, stop=True), reads=[alB, wB], writes=[PB[pq + 3]])
                kb.op("pe", lambda e, pq=pq, gl_=gl_: e.matmul(PS[pq + 1][:, 256:512], lhsT=gl_[:], rhs=gup[:], start=True, stop=True), reads=[glB, wB], writes=[PB[pq + 1]])
                pt, pB = po.next()
                a_, aB = av.next()
                w_, wwB = tw.next()
                kb.op("act", lambda e, pq=pq, w_=w_: e.activation(out=w_[:], in_=PS[pq + 2][:, :], func=AF.Sigmoid), reads=[PB[pq + 2]], writes=[wwB])
                kb.op("act", lambda e, pq=pq, a_=a_: e.activation(out=a_[:], in_=PS[pq + 3][:, :], func=AF.Sigmoid), reads=[PB[pq + 3]], writes=[aB])
                kb.op("act", lambda e, pq=pq, pt=pt: e.activation(out=pt[:, 2304:2560], in_=PS[pq + 1][:, 256:512], func=AF.Identity), reads=[PB[pq + 1]], writes=[pB])
                for d in range(2):
                    kb.op("dve", lambda e, d=d, w_=w_, pt=pt: e.tensor_scalar(out=pt[:, d * 1536:d * 1536 + 256], in0=w_[:, d * 256:(d + 1) * 256], scalar1=-0.6065306597126334, scalar2=None, op0=ALU.mult),
                          reads=[wwB], writes=[pB])
                kr, krB = krr.next()
                t1, t1B = t1r.next()
                t2, t2B = t2r.next()
                sm_, smB = smr.next()
                kb.op("dve", lambda e, kr=kr, k_=k_: e.tensor_tensor(out=kr[:], in0=k_, in1=vecs[:, 0, :], op=ALU.mult), reads=[kvB, vecsB], writes=[krB])
                kb.op("pool", lambda e, kr=kr, t1=t1: e.tensor_tensor(out=t1[:], in0=kr[:], in1=kr[:], op=ALU.mult), reads=[krB], writes=[t1B])
                kb.op("dve", lambda e, t1=t1, sm_=sm_: e.tensor_reduce(out=sm_[:, 0:4], in_=t1[:].rearrange("p (h n) -> p h n", h=4), axis=AX.X, op=ALU.add), reads=[t1B], writes=[smB])
                kb.op("dve", lambda e, sm_=sm_: e.tensor_scalar(out=sm_[:, 0:4], in0=sm_[:, 0:4], scalar1=1e-24, scalar2=None, op0=ALU.max), reads=[smB], writes=[smB])
                kb.op("act", lambda e, sm_=sm_: e.activation(out=sm_[:, 4:8], in_=sm_[:, 0:4], func=AF.Sqrt), reads=[smB], writes=[smB])
                kb.op("dve", lambda e, sm_=sm_: e.reciprocal(out=sm_[:, 8:12], in_=sm_[:, 4:8]), reads=[smB], writes=[smB])
                kb.op("dve", lambda e, sm_=sm_, kr=kr, pt=pt: e.tensor_tensor(out=pt[:, 768:1024].rearrange("p (h n) -> p h n", h=4), in0=kr[:].rearrange("p (h n) -> p h n", h=4),
                                                                       in1=sm_[:, 8:12].unsqueeze(2).to_broadcast([128, 4, 64]), op=ALU.mult), reads=[smB, krB], writes=[pB])
                kb.op("pool", lambda e, pt=pt, r_=r_: e.tensor_copy(out=pt[:, 1024:1280], in_=r_), reads=[kvB], writes=[pB])
                kb.op("pool", lambda e, pt=pt, v_=v_: e.tensor_copy(out=pt[:, 1280:1536], in_=v_), reads=[kvB], writes=[pB])
                for d in range(2):
                    ob = d * 1536
                    kb.op("dve", lambda e, d=d, a_=a_, t1=t1: e.scalar_tensor_tensor(out=t1[:], in0=a_[:, d * 256:(d + 1) * 256], scalar=-1.0, in1=vecs[:, 1, :], op0=ALU.add, op1=ALU.mult),
                          reads=[aB, vecsB, t1B], writes=[t1B])
                    kb.op("dve", lambda e, ob=ob, t1=t1, pt=pt, k_=k_: e.scalar_tensor_tensor(out=pt[:, ob + 512:ob + 768], in0=t1[:], scalar=1.0, in1=k_, op0=ALU.add, op1=ALU.mult),
                          reads=[t1B, kvB], writes=[pB])
                    kb.op("pool", lambda e, ob=ob, d=d, a_=a_, pt=pt: e.tensor_tensor(out=pt[:, ob + 256:ob + 512], in0=pt[:, 768:1024], in1=a_[:, d * 256:(d + 1) * 256], op=ALU.mult),
                          reads=[aB, pB], writes=[pB])
                kb.op("pool", lambda e, pt=pt, t2=t2: e.tensor_tensor(out=t2[:], in0=pt[:, 512:768], in1=pt[:, 2048:2304], op=ALU.add), reads=[pB], writes=[t2B])
                kb.op("pool", lambda e, t2=t2, r_=r_: e.tensor_tensor(out=t2[:], in0=t2[:], in1=r_, op=ALU.mult), reads=[kvB, t2B], writes=[t2B])
                kb.op("pool", lambda e, t2=t2: e.tensor_tensor(out=t2[:], in0=t2[:], in1=vecs[:, 2, :], op=ALU.mult), reads=[vecsB, t2B], writes=[t2B])
                kb.op("dve", lambda e, t2=t2, pt=pt: e.tensor_reduce(out=pt[:, 2560:2564], in_=t2[:].rearrange("p (h n) -> p h n", h=4), axis=AX.X, op=ALU.add), reads=[t2B], writes=[pB])
                kb.dma("sp", PREP[i * 128:(i + 1) * 128, :], pt[:], reads=[pB], writes=[dB["PREP"][i]])
        if upto == "PREP":
            break

        with kb.phase():
            idR = kb.sb([64, 64], F32R)
            idRB = Buf()
            kb.op("act", lambda e: e.activation(out=idR[:], in_=cst[0:64, 0:64], func=AF.Identity), reads=[cstB], writes=[idRB])
            id64 = cst[0:64, 0:64]

            def dir_gen(d):
                B0, B1, B2, B3 = 4 * d, 4 * d + 1, 4 * d + 2, 4 * d + 3
                ST = kb.sb([64, 4, 64], F32R)
                STB = Buf()
                chk = Ring(kb, 3, [64, 1536], F32)
                Er = Ring(kb, 2, [64, 3, 256], F32)
                HTr = Ring(kb, 2, [64, 4, 256], F32R)
                FMr = Ring(kb, 2, [64, 4, 4, 64], F32R)
                Gr = Ring(kb, 2, [64, 4, 2, 128], F32R)
                NTr = Ring(kb, 2, [64, 4, 64], F32R)
                NRr = Ring(kb, 3, [64, 4, 128], F32R)
                NTar = Ring(kb, 3, [64, 4, 64], F32R)
                Xr = Ring(kb, 2, [64, 4, 64], F32R)
                Ur = Ring(kb, 2, [64, 4, 64], F32R)
                Yr = Ring(kb, 2, [64, 256], F32)
                PCr = Ring(kb, 2, [64, 4], F32)
                PPr = Ring(kb, 2, [64, 256], F32)
                vRr = Ring(kb, 2, [64, 256], F32R)
                for h in range(4):
                    kb.op("act", lambda e, h=h: e.activation(out=ST[:, h, :], in_=cst[0:64, 0:64], func=AF.Identity, scale=0.0), reads=[cstB], writes=[STB])
                if d == 0:
                    o_lw, o_b, o_kd, o_kk, o_r, o_v, c0 = 0, 256, 512, 768, 1024, 1280, 0
                else:
                    o_kk, o_r, o_v, o_lw, o_b, o_kd, c0 = 0, 256, 512, 768, 1024, 1280, 768
                mask = cst[0:64, 128 + d * 128:256 + d * 128]
                maskT = cst[0:64, 384 + d * 64:448 + d * 64]
                tri = cst[0:64, 512 + d * 64:576 + d * 64]
                order = range(NCH) if d == 0 else ([3, 2, 1, 0] + list(range(NCH - 1, 3, -1)))
                for c in order:
                    ck, ckB = chk.next()
                    kb.dma("sp", ck[:], PREP[c * 64:(c + 1) * 64, c0:c0 + 1536], reads=[dB["PREP"][c // 2]], writes=[ckB])
                    lw = ck[:, o_lw:o_lw + 256]
                    kb.op("pe", lambda e: e.matmul(PS[B0][0:64, 0:256], lhsT=tri, rhs=lw, start=True, stop=True), reads=[ckB, cstB], writes=[PB[B0]])
                    for hf in range(4):
                        kb.op("pe", lambda e, hf=hf: e.matmul(PS[B0][0:64, 256 + 2 * hf:258 + 2 * hf], lhsT=lw[:, hf * 64:(hf + 1) * 64], rhs=cst[0:64, 640:642], start=True, stop=True),
                              reads=[ckB, cstB], writes=[PB[B0]])
                    yield
                    E, EB = Er.next()
                    PC, PCB = PCr.next()
                    kb.op("act", lambda e: e.activation(out=E[:, 0, :], in_=PS[B0][0:64, 0:256], func=AF.Exp), reads=[PB[B0]], writes=[EB])
                    kb.op("act", lambda e: e.activation(out=E[:, 1, :], in_=PS[B0][0:64, 0:256], func=AF.Exp, scale=-1.0), reads=[PB[B0]], writes=[EB])
                    kb.op("act", lambda e: e.activation(out=E[:, 2, :], in_=lw, func=AF.Exp, scale=-1.0), reads=[ckB], writes=[EB])
                    kb.op("act", lambda e: e.activation(out=PC[:], in_=PS[B0][0:64, 256:264:2], func=AF.Exp), reads=[PB[B0]], writes=[PCB])
                    vR, vRB = vRr.next()
                    kb.op("pool", lambda e: e.tensor_copy(out=vR[:], in_=ck[:, o_v:o_v + 256]), reads=[ckB], writes=[vRB])
                    yield
                    HT, HTB = HTr.next()
                    kb.op("dve", lambda e: e.tensor_tensor(out=HT[:, 0, :], in0=ck[:, o_b:o_b + 256], in1=E[:, 1, :], op=ALU.mult), reads=[ckB, EB], writes=[HTB])
                    kb.op("pool", lambda e: e.tensor_tensor(out=HT[:, 1, :], in0=ck[:, o_kd:o_kd + 256], in1=E[:, 1, :], op=ALU.mult), reads=[ckB, EB], writes=[HTB])
                    kb.op("dve", lambda e: e.scalar_tensor_tensor(out=HT[:, 2, :], in0=ck[:, o_kk:o_kk + 256], scalar=-1.0, in1=E[:, 2, :], op0=ALU.mult, op1=ALU.mult),
                          reads=[ckB, EB], writes=[HTB])
                    kb.op("pool", lambda e: e.tensor_tensor(out=HT[:, 3, :], in0=ck[:, o_r:o_r + 256], in1=E[:, 0, :], op=ALU.mult), reads=[ckB, EB], writes=[HTB])
                    yield
                    kb.op("dve", lambda e: e.tensor_tensor(out=HT[:, 2, :], in0=HT[:, 2, :].bitcast(F32), in1=E[:, 0, :], op=ALU.mult), reads=[EB, HTB], writes=[HTB])
                    yield
                    FM, FMB = FMr.next()
                    for hh in range(2):
                        for h in (2 * hh, 2 * hh + 1):
                            for q in range(4):
                                o = ((h % 2) * 4 + q) * 64
                                kb.op("pe", lambda e, q=q, h=h, o=o: e.transpose(out=PS[B1][0:64, o:o + 64], in_=HT[:, q, h * 64:(h + 1) * 64].bitcast(F32), identity=id64), reads=[HTB, cstB], writes=[PB[B1]])
                        yield
                        kb.op("act", lambda e, hh=hh: e.activation(out=FM[:, 2 * hh:2 * hh + 2, :, :].rearrange("p a q n -> p (a q n)"), in_=PS[B1][0:64, :], func=AF.Identity), reads=[PB[B1]], writes=[FMB])
                        yield
                    G, GB = Gr.next()
                    for hh in range(2):
                        for h in (2 * hh, 2 * hh + 1):
                            for g in range(2):
                                o = ((h % 2) * 2 + g) * 128
                                kb.op("pe", lambda e, h=h, g=g, o=o: e.matmul(PS[B2][0:64, o:o + 128], lhsT=FM[:, h, g, :], rhs=FM[:, h, 2:4, :].rearrange("p a n -> p (a n)"), start=True, stop=True),
                                      reads=[FMB], writes=[PB[B2]])
                        if hh == 0:
                            for h in range(4):
                                kb.op("pe", lambda e, h=h: e.matmul(PS[B3][0:64, h * 64:(h + 1) * 64], lhsT=FM[:, h, 2, :], rhs=FM[:, h, 0, :], start=True, stop=True), reads=[FMB], writes=[PB[B3]])
                        yield
                        kb.op("act", lambda e, hh=hh: e.activation(out=G[:, 2 * hh:2 * hh + 2, :, :].rearrange("p h g n -> p (h g n)"), in_=PS[B2][0:64, :], func=AF.Identity), reads=[PB[B2]], writes=[GB])
                        yield
                    NTt, NTB = NTr.next()
                    kb.op("act", lambda e: e.activation(out=NTt[:].rearrange("p h n -> p (h n)"), in_=PS[B3][0:64, 0:256], func=AF.Identity), reads=[PB[B3]], writes=[NTB])
                    kb.op("pool", lambda e: e.tensor_tensor(out=G[:].rearrange("p h g n -> p (h g) n"), in0=G[:].bitcast(F32).rearrange("p h g n -> p (h g) n"),
                                                           in1=mask.unsqueeze(1).to_broadcast([64, 8, 128]), op=ALU.mult), reads=[GB, cstB], writes=[GB])
                    yield
                    kb.op("dve", lambda e: e.tensor_tensor(out=NTt[:], in0=NTt[:].bitcast(F32), in1=maskT.unsqueeze(1).to_broadcast([64, 4, 64]), op=ALU.mult), reads=[NTB, cstB], writes=[NTB])
                    NR, NRB = NRr.next()
                    NTa, NTaB = NTar.next()
                    kb.op("pool", lambda e, NR=NR: e.tensor_tensor(out=NR[:, :, 64:128], in0=G[:, :, 0, 0:64].bitcast(F32), in1=id64.unsqueeze(1).to_broadcast([64, 4, 64]), op=ALU.add), reads=[GB, cstB], writes=[NRB])
                    for h in range(4):
                        kb.op("pe", lambda e, h=h: e.matmul(PS[B1][0:64, h * 64:(h + 1) * 64], lhsT=NTt[:, h, :], rhs=G[:, h, 0, 0:64], start=True, stop=True), reads=[GB, NTB], writes=[PB[B1]])
                    for h in range(4):
                        kb.op("pe", lambda e, h=h: e.matmul(PS[B2][0:64, h * 64:(h + 1) * 64], lhsT=G[:, h, 0, 0:64], rhs=NTt[:, h, :], start=True, stop=True), reads=[GB, NTB], writes=[PB[B2]])
                    yield
                    kb.op("act", lambda e, NR=NR: e.activation(out=NR[:, :, 0:64], in_=PS[B1][0:64, 0:256].rearrange("p (h n) -> p h n", h=4), func=AF.Identity), reads=[PB[B1]], writes=[NRB])
                    kb.op("act", lambda e, NTa=NTa: e.activation(out=NTa[:].rearrange("p h n -> p (h n)"), in_=PS[B2][0:64, 0:256], func=AF.Identity), reads=[PB[B2]], writes=[NTaB])
                    yield
                    for s_ in range(1, 6):
                        lastS = (s_ == 5)
                        NR2, NR2B = NRr.next()
                        NTa2, NTa2B = NTar.next()
                        wN = 64 if lastS else 128
                        for h in range(4):
                            kb.op("pe", lambda e, h=h, NR=NR, NTa=NTa, wN=wN: e.matmul(PS[B1][0:64, h * 128:h * 128 + wN], lhsT=NTa[:, h, :], rhs=NR[:, h, 128 - wN:128], start=True, stop=True),
                                  reads=[NRB, NTaB], writes=[PB[B1]])
                        if not lastS:
                            for h in range(4):
                                kb.op("pe", lambda e, h=h, NR=NR, NTa=NTa: e.matmul(PS[B2][0:64, h * 64:(h + 1) * 64], lhsT=NR[:, h, 0:64], rhs=NTa[:, h, :], start=True, stop=True),
                                      reads=[NRB, NTaB], writes=[PB[B2]])
                        yield
                        PPt, PPB = PPr.next()
                        pv = PS[B1][0:64, :].rearrange("p (h n) -> p h n", h=4)
                        if lastS:
                            kb.op("act", lambda e, PPt=PPt, pv=pv: e.activation(out=PPt[:].rearrange("p (h n) -> p h n", h=4), in_=pv[:, :, 0:64], func=AF.Identity), reads=[PB[B1]], writes=[PPB])
                        else:
                            kb.op("act", lambda e, PPt=PPt, pv=pv: e.activation(out=PPt[:].rearrange("p (h n) -> p h n", h=4), in_=pv[:, :, 64:128], func=AF.Identity), reads=[PB[B1]], writes=[PPB])
                            kb.op("act", lambda e, NR2=NR2, pv=pv: e.activation(out=NR2[:, :, 0:64], in_=pv[:, :, 0:64], func=AF.Identity), reads=[PB[B1]], writes=[NR2B])
                            kb.op("act", lambda e, NTa2=NTa2: e.activation(out=NTa2[:].rearrange("p h n -> p (h n)"), in_=PS[B2][0:64, 0:256], func=AF.Identity), reads=[PB[B2]], writes=[NTa2B])
                        yield
                        kb.op("dve", lambda e, NR=NR, NR2=NR2, PPt=PPt: e.tensor_tensor(out=NR2[:, :, 64:128], in0=NR[:, :, 64:128].bitcast(F32), in1=PPt[:].rearrange("p (h n) -> p h n", h=4), op=ALU.add),
                              reads=[PPB, NRB], writes=[NR2B])
                        yield
                        NR, NRB, NTa, NTaB = NR2, NR2B, NTa2, NTa2B
                    Tm = lambda h, NR=NR: NR[:, h, 64:128]
                    PmB = NRB
                    Xs, XB = Xr.next()
                    Us, UB = Ur.next()
                    Ys, YB = Yr.next()
                    vh = lambda h: vR[:, h * 64:(h + 1) * 64]
                    for h in range(4):
                        kb.op("pe", lambda e, h=h: e.matmul(PS[B1][0:64, h * 64:(h + 1) * 64], lhsT=FM[:, h, 2, :], rhs=ST[:, h, :], start=True, stop=False), reads=[FMB, STB], writes=[PB[B1]])
                        kb.op("pe", lambda e, h=h: e.matmul(PS[B1][0:64, h * 64:(h + 1) * 64], lhsT=G[:, h, 1, 0:64], rhs=vh(h), start=False, stop=True), reads=[GB, vRB], writes=[PB[B1]])
                    yield
                    kb.op("act", lambda e: e.activation(out=Xs[:].rearrange("p h n -> p (h n)"), in_=PS[B1][0:64, 0:256], func=AF.Identity), reads=[PB[B1]], writes=[XB])
                    yield
                    for h in range(4):
                        kb.op("pe", lambda e, h=h: e.matmul(PS[B2][0:64, h * 64:(h + 1) * 64], lhsT=Tm(h), rhs=Xs[:, h, :], start=True, stop=True), reads=[PmB, XB], writes=[PB[B2]])
                    yield
                    kb.op("act", lambda e: e.activation(out=Us[:].rearrange("p h n -> p (h n)"), in_=PS[B2][0:64, 0:256], func=AF.Identity), reads=[PB[B2]], writes=[UB])
                    yield
                    for h in range(4):
                        kb.op("pe", lambda e, h=h: e.matmul(PS[B3][0:64, h * 64:(h + 1) * 64], lhsT=FM[:, h, 3, :], rhs=ST[:, h, :], start=True, stop=False), reads=[FMB, STB], writes=[PB[B3]])
                        kb.op("pe", lambda e, h=h: e.matmul(PS[B3][0:64, h * 64:(h + 1) * 64], lhsT=G[:, h, 0, 64:128], rhs=Us[:, h, :], start=False, stop=False), reads=[GB, UB], writes=[PB[B3]])
                        kb.op("pe", lambda e, h=h: e.matmul(PS[B3][0:64, h * 64:(h + 1) * 64], lhsT=G[:, h, 1, 64:128], rhs=vh(h), start=False, stop=True), reads=[GB, vRB], writes=[PB[B3]])
                    for h in range(4):
                        kb.op("pe", lambda e, h=h: e.matmul(PS[B0][0:64, h * 64:(h + 1) * 64], lhsT=idR[:], rhs=ST[:, h, :], start=True, stop=False), reads=[STB, idRB], writes=[PB[B0]])
                        kb.op("pe", lambda e, h=h: e.matmul(PS[B0][0:64, h * 64:(h + 1) * 64], lhsT=HT[:, 0, h * 64:(h + 1) * 64], rhs=Us[:, h, :], start=False, stop=False), reads=[HTB, UB], writes=[PB[B0]])
                        kb.op("pe", lambda e, h=h: e.matmul(PS[B0][0:64, h * 64:(h + 1) * 64], lhsT=HT[:, 1, h * 64:(h + 1) * 64], rhs=vh(h), start=False, stop=True), reads=[HTB, vRB], writes=[PB[B0]])
                    yield
                    kb.op("act", lambda e: e.activation(out=Ys[:], in_=PS[B3][0:64, 0:256], func=AF.Identity), reads=[PB[B3]], writes=[YB])
                    for h in range(4):
                        kb.op("act", lambda e, h=h: e.activation(out=ST[:, h, :], in_=PS[B0][0:64, h * 64:(h + 1) * 64], func=AF.Identity, scale=PC[:, h:h + 1]), reads=[PB[B0], PCB], writes=[STB])
                    kb.dma("sp", YD[d, c * 64:(c + 1) * 64, :], Ys[:], reads=[YB], writes=[dB["YD%d" % d][c // 2]])
                    yield

            gens = [dir_gen(0), dir_gen(1)]
            while gens:
                for g_ in list(gens):
                    try:
                        next(g_)
                    except StopIteration:
                        gens.remove(g_)
        if upto == "YD":
            break

        with kb.phase():
            gnv = kb.sb([128, 2, 256], F32)
            gnB = Buf()
            kb.dma("sp", gnv[:, 0, :], I["gn_g"][l, :].partition_broadcast(128), writes=[gnB])
            kb.dma("sp", gnv[:, 1, :], I["gn_b"][l, :].partition_broadcast(128), writes=[gnB])
            yr = Ring(kb, 4, [128, 2, 256], F32)
            vr = Ring(kb, 4, [128, 256], F32)
            gr = Ring(kb, 4, [128, 260], F32)
            ycr = Ring(kb, 4, [128, 256], F32)
            sqr = Ring(kb, 4, [128, 256], F32)
            smr = Ring(kb, 4, [128, 16], F32)
            for i in kb.interleave(range(2 if last else 0, NT), 4):
                yt, yB = yr.next()
                vt_, vB = vr.next()
                gt, gB = gr.next()
                for d in range(2):
                    kb.dma("sp", yt[:, d, :], YD[d, i * 128:(i + 1) * 128, :], reads=[dB["YD%d" % d][i]], writes=[yB])
                kb.dma("sp", vt_[:], PREP[i * 128:(i + 1) * 128, 1280:1536], reads=[dB["PREP"][i]], writes=[vB])
                kb.dma("sp", gt[:], PREP[i * 128:(i + 1) * 128, 2304:2564], reads=[dB["PREP"][i]], writes=[gB])
                yc, ycB = ycr.next()
                sq_, sqB = sqr.next()
                sm_, smB = smr.next()
                v4 = lambda t_: t_.rearrange("p (h n) -> p h n", h=4)
                bc = lambda a_: a_.unsqueeze(2).to_broadcast([128, 4, 64])
                kb.op("dve", lambda e, yt=yt, yc=yc: e.tensor_tensor(out=yc[:], in0=yt[:, 0, :], in1=yt[:, 1, :], op=ALU.add), reads=[yB], writes=[ycB])
                kb.op("dve", lambda e, yc=yc, sm_=sm_: e.tensor_reduce(out=sm_[:, 0:4], in_=v4(yc[:]), axis=AX.X, op=ALU.add), reads=[ycB], writes=[smB])
                kb.op("dve", lambda e, sm_=sm_: e.tensor_scalar(out=sm_[:, 0:4], in0=sm_[:, 0:4], scalar1=-1.0 / 64, scalar2=None, op0=ALU.mult), reads=[smB], writes=[smB])
                kb.op("dve", lambda e, yc=yc, sm_=sm_: e.tensor_tensor(out=v4(yc[:]), in0=v4(yc[:]), in1=bc(sm_[:, 0:4]), op=ALU.add), reads=[smB, ycB], writes=[ycB])
                kb.op("pool", lambda e, yc=yc, sq_=sq_: e.tensor_tensor(out=sq_[:], in0=yc[:], in1=yc[:], op=ALU.mult), reads=[ycB], writes=[sqB])
                kb.op("dve", lambda e, sq_=sq_, sm_=sm_: e.tensor_reduce(out=sm_[:, 4:8], in_=v4(sq_[:]), axis=AX.X, op=ALU.add), reads=[sqB], writes=[smB])
                kb.op("dve", lambda e, sm_=sm_: e.tensor_scalar(out=sm_[:, 4:8], in0=sm_[:, 4:8], scalar1=1.0 / 64, scalar2=GN_EPS, op0=ALU.mult, op1=ALU.add), reads=[smB], writes=[smB])
                kb.op("act", lambda e, sm_=sm_: e.activation(out=sm_[:, 8:12], in_=sm_[:, 4:8], func=AF.Sqrt), reads=[smB], writes=[smB])
                kb.op("dve", lambda e, sm_=sm_: e.reciprocal(out=sm_[:, 12:16], in_=sm_[:, 8:12]), reads=[smB], writes=[smB])
                kb.op("dve", lambda e, yc=yc, sm_=sm_: e.tensor_tensor(out=v4(yc[:]), in0=v4(yc[:]), in1=bc(sm_[:, 12:16]), op=ALU.mult), reads=[smB, ycB], writes=[ycB])
                kb.op("dve", lambda e, yc=yc: e.tensor_tensor(out=yc[:], in0=yc[:], in1=gnv[:, 0, :], op=ALU.mult), reads=[gnB, ycB], writes=[ycB])
                kb.op("dve", lambda e, yc=yc: e.tensor_tensor(out=yc[:], in0=yc[:], in1=gnv[:, 1, :], op=ALU.add), reads=[gnB, ycB], writes=[ycB])
                kb.op("pool", lambda e, vt_=vt_, gt=gt, sq_=sq_: e.tensor_tensor(out=v4(sq_[:]), in0=v4(vt_[:]), in1=bc(gt[:, 256:260]), op=ALU.mult), reads=[vB, gB, sqB], writes=[sqB])
                kb.op("dve", lambda e, yc=yc, sq_=sq_: e.tensor_tensor(out=yc[:], in0=yc[:], in1=sq_[:], op=ALU.add), reads=[sqB, ycB], writes=[ycB])
                kb.op("dve", lambda e, yc=yc, gt=gt: e.tensor_tensor(out=yc[:], in0=yc[:], in1=gt[:, 0:256], op=ALU.mult), reads=[gB, ycB], writes=[ycB])
                kb.dma("sp", MIX[i * 128:(i + 1) * 128, 512:768], yc[:], reads=[ycB], writes=[dB["MIX"][i]])
        if upto == "MIX":
            break

        def layer_norm_tile(t_, tB, sq_, sqB, sm_, smB, lnp, lnpB):
            gi = 0
            kb.op("dve", lambda e: e.tensor_reduce(out=sm_[:, 0:1], in_=t_[:], axis=AX.X, op=ALU.add), reads=[tB], writes=[smB])
            kb.op("dve", lambda e: e.tensor_scalar(out=sm_[:, 1:2], in0=sm_[:, 0:1], scalar1=-1.0 / D, scalar2=None, op0=ALU.mult), reads=[smB], writes=[smB])
            kb.op("dve", lambda e: e.tensor_scalar(out=t_[:], in0=t_[:], scalar1=sm_[:, 1:2], scalar2=None, op0=ALU.add), reads=[smB, tB], writes=[tB])
            kb.op("pool", lambda e: e.tensor_tensor(out=sq_[:], in0=t_[:], in1=t_[:], op=ALU.mult), reads=[tB], writes=[sqB])
            kb.op("dve", lambda e: e.tensor_reduce(out=sm_[:, 2:3], in_=sq_[:], axis=AX.X, op=ALU.add), reads=[sqB], writes=[smB])
            kb.op("dve", lambda e: e.tensor_scalar(out=sm_[:, 3:4], in0=sm_[:, 2:3], scalar1=1.0 / D, scalar2=LN_EPS, op0=ALU.mult, op1=ALU.add), reads=[smB], writes=[smB])
            kb.op("act", lambda e: e.activation(out=sm_[:, 4:5], in_=sm_[:, 3:4], func=AF.Sqrt), reads=[smB], writes=[smB])
            kb.op("dve", lambda e: e.reciprocal(out=sm_[:, 5:6], in_=sm_[:, 4:5]), reads=[smB], writes=[smB])
            kb.op("dve", lambda e: e.scalar_tensor_tensor(out=t_[:], in0=t_[:], scalar=sm_[:, 5:6], in1=lnp[:, gi, :], op0=ALU.mult, op1=ALU.mult), reads=[smB, tB, lnpB], writes=[tB])
            kb.op("dve", lambda e: e.tensor_tensor(out=t_[:], in0=t_[:], in1=lnp[:, gi + 1, :], op=ALU.add), reads=[tB, lnpB], writes=[tB])

        with kb.phase():
            gate1, gate1B, ln1t, ln1B = load_gate_ln(0)
            wo = kb.sb([128, 8, D], BF16)
            woB = Buf()
            for c in range(8):
                kb.dma("pool", wo[:, c, :], I["w_out"][l, c * 128:(c + 1) * 128, :], writes=[woB])
            mr = Ring(kb, 2, [128, D], F32)
            mTr = Ring(kb, 2, [128, 8, 128], BF16)
            xr = Ring(kb, 2, [128, D], F32)
            tr_ = Ring(kb, 2, [128, D], F32)
            sqr = Ring(kb, 2, [128, D], F32)
            smr = Ring(kb, 2, [128, 8], F32)
            for i in kb.interleave(range(2 if last else 0, NT), 2):
                pq = 4 * (i % 2)
                w = 1 if i < 2 else 0
                mt, mB = mr.next()
                xt, xB = xr.next()
                kb.dma("sp", mt[:], MIX[i * 128:(i + 1) * 128, :], reads=[dB["MIX"][i]], writes=[mB])
                kb.dma("sp", xt[:], xsrc(l, i), reads=([dB["X"][i]] if l > 0 else []), writes=[xB])
                for c in range(8):
                    kb.op("pe", lambda e, pq=pq, c=c, mt=mt: e.transpose(out=PS[pq + c // 4][:, (c % 4) * 128:(c % 4 + 1) * 128], in_=mt[:, c * 128:(c + 1) * 128], identity=ident), reads=[mB, cstB], writes=[PB[pq + c // 4]])
                mT, mTB = mTr.next()
                kb.op("act", lambda e, pq=pq, mT=mT: e.activation(out=mT[:, 0:4, :].rearrange("p c n -> p (c n)"), in_=PS[pq][:, :], func=AF.Identity), reads=[PB[pq]], writes=[mTB])
                kb.op("dve", lambda e, pq=pq, mT=mT: e.tensor_copy(out=mT[:, 4:8, :].rearrange("p c n -> p (c n)"), in_=PS[pq + 1][:, :]), reads=[PB[pq + 1]], writes=[mTB])
                t_, tB = tr_.next()
                for hf in range(2):
                    for c in range(8):
                        kb.op("pe", lambda e, pq=pq, c=c, hf=hf, mT=mT: e.matmul(PS[pq + 2 + hf][:, :], lhsT=mT[:, c, :], rhs=wo[:, c, hf * 512:(hf + 1) * 512], start=(c == 0), stop=(c == 7)), reads=[mTB, woB], writes=[PB[pq + 2 + hf]])
                    kb.op("dve", lambda e, pq=pq, hf=hf, t_=t_, w=w: e.tensor_tensor(out=t_[:, hf * 512:(hf + 1) * 512], in0=PS[pq + 2 + hf][:, :], in1=gate1[:, w, hf * 512:(hf + 1) * 512], op=ALU.mult), reads=[PB[pq + 2 + hf], gate1B], writes=[tB])
                kb.op("dve", lambda e, t_=t_, xt=xt: e.scalar_tensor_tensor(out=t_[:], in0=xt[:], scalar=DN_ALPHA, in1=t_[:], op0=ALU.mult, op1=ALU.add), reads=[xB, tB], writes=[tB])
                sq_, sqB = sqr.next()
                sm_, smB = smr.next()
                layer_norm_tile(t_, tB, sq_, sqB, sm_, smB, ln1t, ln1B)
                kb.dma("sp", X1[i * 128:(i + 1) * 128, :], t_[:], reads=[tB], writes=[dB["X1"][i]])
        if upto == "X1":
            break

        with kb.phase():
            jj = l // 2
            moe = (l % 2 == 1)
            gate2, gate2B, ln2t, ln2B = load_gate_ln(1)
            if moe:
                experts = [(I["moe_w1"][jj, e], I["moe_w3"][jj, e], I["moe_w2"][jj, e]) for e in range(NE)]
            else:
                experts = [(I["ffn_w1"][jj, :, e * DFE:(e + 1) * DFE], I["ffn_w3"][jj, :, e * DFE:(e + 1) * DFE], I["ffn_w2"][jj, e * DFE:(e + 1) * DFE, :]) for e in range(2)]
            NFC = DFE // 128
            x1g = kb.sb([128, 4, D], F32)
            x1B = Buf()
            hT = kb.sb([128, 8, 512], BF16)
            hTB = Buf()
            hTfr = Ring(kb, 2, [128, 8, 128], F32)
            gTs = [kb.sb([128, NFC, 512], BF16) for _ in range(2)]
            gTBs = [Buf(), Buf()]
            acc = kb.sb([128, 4, D], F32)
            accB = Buf()
            w2e = Ring(kb, 2, [128, NFC, D], BF16)
            w1r = Ring(kb, 4, [128, 8, 128], BF16)
            w3r = Ring(kb, 4, [128, 8, 128], BF16)
            sar = Ring(kb, 2, [128, 512], F32)
            gates = kb.sb([128, 4, NE], F32)
            gatesB = Buf()
            rt = kb.sb([128, 4, 24], F32)
            rtB = Buf()
            sqr = Ring(kb, 2, [128, D], F32)
            smr = Ring(kb, 2, [128, 8], F32)
            if moe:
                rw = kb.sb([128, 8, NE], F32)
                rb = kb.sb([128, NE], F32)
                rwB = Buf()
                kb.dma("sp", rw[:], I["router_w"][jj].rearrange("(c p) e -> p c e", p=128), writes=[rwB])
                kb.dma("sp", rb[:], I["router_b"][jj, :].partition_broadcast(128), writes=[rwB])
            groups = ([] if last else [(0, 2)]) + [(2 + 4 * g_, 4) for g_ in range(8)]
            for (i0, nt_) in groups:
                w = 1 if i0 < 2 else 0
                ng = nt_ * 128
                for ti in range(nt_):
                    i = i0 + ti
                    kb.dma("sp", x1g[:, ti, :], X1[i * 128:(i + 1) * 128, :], reads=[dB["X1"][i]], writes=[x1B])
                    for c in range(8):
                        kb.op("pe", lambda e, c=c, ti=ti: e.transpose(out=PS[c // 4][:, (c % 4) * 128:(c % 4 + 1) * 128], in_=x1g[:, ti, c * 128:(c + 1) * 128], identity=ident), reads=[x1B, cstB], writes=[PB[c // 4]])
                    hf_, hfB = hTfr.next()
                    for c in range(8):
                        kb.op("act", lambda e, c=c, hf_=hf_, w=w: e.activation(out=hf_[:, c, :], in_=PS[c // 4][:, (c % 4) * 128:(c % 4 + 1) * 128], func=AF.Identity,
                                                                          scale=modS[:, w, 3, c:c + 1], bias=modS[:, w, 2, c:c + 1]), reads=[PB[c // 4], modSB], writes=[hfB])
                    kb.op("pool", lambda e, hf_=hf_, ti=ti: e.tensor_copy(out=hT[:, :, ti * 128:(ti + 1) * 128], in_=hf_[:]), reads=[hfB], writes=[hTB])
                    if moe:
                        for c in range(8):
                            kb.op("pe", lambda e, c=c, hf_=hf_: e.matmul(PS[2][:, 0:NE], lhsT=hf_[:, c, :], rhs=rw[:, c, :], start=(c == 0), stop=(c == 7)), reads=[hfB, rwB], writes=[PB[2]])
                        r_ = rt[:, ti, :]
                        kb.op("dve", lambda e, r_=r_: e.tensor_tensor(out=r_[:, 0:8], in0=PS[2][:, 0:NE], in1=rb[:], op=ALU.add), reads=[PB[2], rwB], writes=[rtB])
                        kb.op("dve", lambda e, r_=r_: e.tensor_reduce(out=r_[:, 16:17], in_=r_[:, 0:8], axis=AX.X, op=ALU.max), reads=[rtB], writes=[rtB])
                        kb.op("dve", lambda e, r_=r_: e.tensor_scalar(out=r_[:, 8:16], in0=r_[:, 0:8], scalar1=r_[:, 16:17], scalar2=None, op0=ALU.is_equal), reads=[rtB], writes=[rtB])
                        kb.op("dve", lambda e, r_=r_: e.scalar_tensor_tensor(out=r_[:, 0:8], in0=r_[:, 8:16], scalar=-1e30, in1=r_[:, 0:8], op0=ALU.mult, op1=ALU.add), reads=[rtB], writes=[rtB])
                        kb.op("dve", lambda e, r_=r_: e.tensor_reduce(out=r_[:, 17:18], in_=r_[:, 0:8], axis=AX.X, op=ALU.max), reads=[rtB], writes=[rtB])
                        kb.op("dve", lambda e, r_=r_: e.tensor_scalar(out=r_[:, 0:8], in0=r_[:, 0:8], scalar1=r_[:, 17:18], scalar2=None, op0=ALU.is_equal), reads=[rtB], writes=[rtB])
                        kb.op("dve", lambda e, r_=r_: e.tensor_tensor(out=r_[:, 18:19], in0=r_[:, 16:17], in1=r_[:, 17:18], op=ALU.subtract), reads=[rtB], writes=[rtB])
                        kb.op("act", lambda e, r_=r_: e.activation(out=r_[:, 19:20], in_=r_[:, 18:19], func=AF.Sigmoid), reads=[rtB], writes=[rtB])
                        kb.op("act", lambda e, r_=r_: e.activation(out=r_[:, 20:21], in_=r_[:, 18:19], func=AF.Sigmoid, scale=-1.0), reads=[rtB], writes=[rtB])
                        kb.op("dve", lambda e, r_=r_: e.tensor_scalar(out=r_[:, 8:16], in0=r_[:, 8:16], scalar1=r_[:, 19:20], scalar2=None, op0=ALU.mult), reads=[rtB], writes=[rtB])
                        kb.op("dve", lambda e, r_=r_, ti=ti: e.scalar_tensor_tensor(out=gates[:, ti, :], in0=r_[:, 0:8], scalar=r_[:, 20:21], in1=r_[:, 8:16], op0=ALU.mult, op1=ALU.add), reads=[rtB], writes=[gatesB])
                for ei, (W1, W3, W2) in enumerate(experts):
                    gT, gTB = gTs[ei % 2], gTBs[ei % 2]
                    w2t, w2B = w2e.next()
                    kb.dma("pool", w2t[:], W2.rearrange("(c p) n -> p c n", p=128), writes=[w2B])
                    for fc in range(NFC):
                        w1t, w1B = w1r.next()
                        w3t, w3B = w3r.next()
                        kb.dma("pool", w1t[:], W1[:, fc * 128:(fc + 1) * 128].rearrange("(c p) f -> p c f", p=128), writes=[w1B])
                        kb.dma("pool", w3t[:], W3[:, fc * 128:(fc + 1) * 128].rearrange("(c p) f -> p c f", p=128), writes=[w3B])
                        pa = 2 + (fc % 2)
                        pbb = 4 + (fc % 2)
                        for c in range(8):
                            kb.op("pe", lambda e, c=c, w1t=w1t, ng=ng, pa=pa: e.matmul(PS[pa][:, 0:ng], lhsT=w1t[:, c, :], rhs=hT[:, c, 0:ng], start=(c == 0), stop=(c == 7)), reads=[w1B, hTB], writes=[PB[pa]])
                        for c in range(8):
                            kb.op("pe", lambda e, c=c, w3t=w3t, ng=ng, pbb=pbb: e.matmul(PS[pbb][:, 0:ng], lhsT=w3t[:, c, :], rhs=hT[:, c, 0:ng], start=(c == 0), stop=(c == 7)), reads=[w3B, hTB], writes=[PB[pbb]])
                        sa, saB = sar.next()
                        kb.op("act", lambda e, sa=sa, ng=ng, pa=pa: e.activation(out=sa[:, 0:ng], in_=PS[pa][:, 0:ng], func=AF.Silu), reads=[PB[pa]], writes=[saB])
                        kb.op("dve", lambda e, sa=sa, fc=fc, ng=ng, pbb=pbb: e.tensor_tensor(out=gT[:, fc, 0:ng], in0=PS[pbb][:, 0:ng], in1=sa[:, 0:ng], op=ALU.mult), reads=[PB[pbb], saB], writes=[gTB])
                    for ti in range(nt_):
                        for hf in range(2):
                            for fc in range(NFC):
                                kb.op("pe", lambda e, fc=fc, ti=ti, hf=hf, w2t=w2t: e.matmul(PS[6 + hf][:, :], lhsT=gT[:, fc, ti * 128:(ti + 1) * 128], rhs=w2t[:, fc, hf * 512:(hf + 1) * 512],
                                                                                     start=(fc == 0), stop=(fc == NFC - 1)), reads=[gTB, w2B], writes=[PB[6 + hf]])
                            a_ = acc[:, ti, hf * 512:(hf + 1) * 512]
                            if moe:
                                gsc = gates[:, ti, ei:ei + 1]
                                if ei == 0:
                                    kb.op("dve", lambda e, a_=a_, hf=hf, gsc=gsc: e.tensor_scalar(out=a_, in0=PS[6 + hf][:, :], scalar1=gsc, scalar2=None, op0=ALU.mult), reads=[PB[6 + hf], gatesB], writes=[accB])
                                else:
                                    kb.op("dve", lambda e, a_=a_, hf=hf, gsc=gsc: e.scalar_tensor_tensor(out=a_, in0=PS[6 + hf][:, :], scalar=gsc, in1=a_, op0=ALU.mult, op1=ALU.add),
                                          reads=[PB[6 + hf], gatesB, accB], writes=[accB])
                            else:
                                if ei == 0:
                                    kb.op("act", lambda e, a_=a_, hf=hf: e.activation(out=a_, in_=PS[6 + hf][:, :], func=AF.Identity), reads=[PB[6 + hf]], writes=[accB])
                                else:
                                    kb.op("dve", lambda e, a_=a_, hf=hf: e.tensor_tensor(out=a_, in0=PS[6 + hf][:, :], in1=a_, op=ALU.add), reads=[PB[6 + hf], accB], writes=[accB])
                for ti in range(nt_):
                    i = i0 + ti
                    if last and i < 2:
                        continue
                    a_ = acc[:, ti, :]
                    kb.op("dve", lambda e, a_=a_, w=w: e.tensor_tensor(out=a_, in0=a_, in1=gate2[:, w, :], op=ALU.mult), reads=[accB, gate2B], writes=[accB])
                    kb.op("dve", lambda e, a_=a_, ti=ti: e.scalar_tensor_tensor(out=a_, in0=x1g[:, ti, :], scalar=DN_ALPHA, in1=a_, op0=ALU.mult, op1=ALU.add), reads=[x1B, accB], writes=[accB])
                    sq_, sqB = sqr.next()
                    sm_, smB = smr.next()
                    layer_norm_tile(a_, accB, sq_, sqB, sm_, smB, ln2t, ln2B)
                    if last:
                        kb.dma("sp", OUT[(i - 2) * 128:(i - 1) * 128, :], a_, reads=[accB])
                    else:
                        kb.dma("sp", X[i * 128:(i + 1) * 128, :], a_, reads=[accB], writes=[dB["X"][i]])
        if upto == "X":
            break
        lstack.close()
        kb.stacks.pop()
    kb.barrier()
    return nc, dumps


def host_inputs(inputs, b):
    m = {n: np.ascontiguousarray(np.asarray(inputs[n], dtype=np.float32).reshape(s)) for n, s in PARAMS}
    m["x"] = np.ascontiguousarray(inputs["x"][b], dtype=np.float32)
    m["ctx"] = np.ascontiguousarray(inputs["ctx"][b], dtype=np.float32)
    cv = np.zeros((16, 128), np.float32)
    cv[0:8] = np.asarray(inputs["c"][b], np.float32).reshape(8, 128)
    cv[8:16] = np.asarray(inputs["c_ctx"], np.float32).reshape(8, 128)
    m["cvec"] = cv
    m["consts"] = make_consts()
    m["rope"] = make_rope()
    return m


def kernel(**inputs):
    nc, _ = build(n_layers=DEPTH, dump=None)
    base = {n: np.ascontiguousarray(np.asarray(inputs[n], dtype=np.float32).reshape(s)) for n, s in PARAMS}
    consts = make_consts()
    rope = make_rope()
    in_maps = []
    for b in range(8):
        m = dict(base)
        m["x"] = np.ascontiguousarray(np.asarray(inputs["x"][b], dtype=np.float32))
        m["ctx"] = np.ascontiguousarray(np.asarray(inputs["ctx"][b], dtype=np.float32))
        cv = np.zeros((16, 128), np.float32)
        cv[0:8] = np.asarray(inputs["c"][b], np.float32).reshape(8, 128)
        cv[8:16] = np.asarray(inputs["c_ctx"], np.float32).reshape(8, 128)
        m["cvec"] = cv
        m["consts"] = consts
        m["rope"] = rope
        in_maps.append(m)
    res = run_bass_kernel_spmd(nc, in_maps, core_ids=list(range(8)))
    out = np.stack([np.asarray(res.results[b]["out"], dtype=np.float32) for b in range(8)], 0)
    return out
```

```python
import math
import contextlib
import bisect
import numpy as np
import concourse.bass as bass
import concourse.mybir as mybir
from concourse.bass_utils import run_bass_kernel_spmd

F32 = mybir.dt.float32
BF16 = mybir.dt.bfloat16
F32R = mybir.dt.float32r
ALU = mybir.AluOpType
AF = mybir.ActivationFunctionType
AX = mybir.AxisListType

D = 1024
TC = 256
TL = 4096
T = TC + TL
NT = T // 128
DEPTH = 4
INW = 3264
DFF = 2816
DFE = 1408
NE = 8
DN_ALPHA = (2 * DEPTH) ** 0.25
LN_EPS = 1e-5
GN_EPS = 64e-5
SUBLN_EPS = 1e-5
CH = 64
NCH = T // CH
PR = T + 3
import os
SCAN_LIMIT = int(os.environ.get('SCAN_LIMIT', '0'))
STAGE = int(os.environ.get('STAGE', '99'))
SUB = int(os.environ.get('SUB', '0'))


def prow(tok):
    return 1 + tok if tok < TC else 2 + tok


class Buf:
    __slots__ = ("w", "r", "excl")

    def __init__(self, excl=False):
        self.w = None
        self.r = {}
        self.excl = excl


class KB:
    KD = 8

    def __init__(self, nc):
        self.nc = nc
        self.eng = {"pe": nc.tensor, "act": nc.scalar, "dve": nc.vector, "pool": nc.gpsimd, "sp": nc.sync}
        self.sem = {e: nc.alloc_semaphore("s_" + e) for e in self.eng}
        self.cnt = {e: 0 for e in self.eng}
        self.ins = {e: [] for e in self.eng}
        self.sigi = {e: [] for e in self.eng}
        self.seen = {e: {} for e in self.eng}
        self.dsem = {q: [nc.alloc_semaphore("d_%s%d" % (q, i)) for i in range(self.KD)] for q in ("sp", "pool", "act")}
        self.duse = {q: [0] * self.KD for q in self.dsem}
        self.di = {q: 0 for q in self.dsem}
        self.nsb = 0
        self.rec = None
        self.stacks = [contextlib.ExitStack()]

    def sb(self, shape, dt=F32, name=None):
        self.nsb += 1
        return self.stacks[-1].enter_context(self.nc.sbuf_tensor("t%d" % self.nsb, list(shape), dt))

    @contextlib.contextmanager
    def phase(self):
        self.stacks.append(contextlib.ExitStack())
        try:
            yield
        finally:
            self.barrier()
            self.stacks.pop().close()

    def wait(self, e, ev):
        if ev is None:
            return
        sem, val, key = ev
        if key == "pe" and e == "pe":
            return
        if sem is None:
            sl = self.sigi[key]
            j = bisect.bisect_left(sl, val)
            if j == len(sl):
                self.ins[key][val - 1].then_inc(self.sem[key], 1)
                sl.append(val)
            val = j + 1
            sem = self.sem[key]
        if self.seen[e].get(key, 0) >= val:
            return
        self.eng[e].wait_ge(sem, val)
        self.seen[e][key] = val

    def _deps(self, e, reads, writes):
        for b in reads:
            self.wait(e, b.w)
            if b.excl:
                for k_, ev in b.r.items():
                    if k_ != e:
                        self.wait(e, ev)
        for b in writes:
            self.wait(e, b.w)
            for ev in b.r.values():
                self.wait(e, ev)

    def _post(self, ev, reads, writes):
        for b in reads:
            b.r[ev[2]] = ev
        for b in writes:
            b.w = ev
            b.r = {}

    def interleave(self, it, k=2):
        items = list(it)
        for j in range(0, len(items), k):
            recs = []
            for x in items[j:j + k]:
                self.rec = []
                yield x
                recs.append(self.rec)
            self.rec = None
            for t in range(max(len(r) for r in recs)):
                for r in recs:
                    if t < len(r):
                        kind, a = r[t]
                        if kind == "op":
                            self.op(*a)
                        else:
                            self.dma(*a)

    def op(self, e, fn, reads=(), writes=()):
        if self.rec is not None:
            self.rec.append(("op", (e, fn, tuple(reads), tuple(writes))))
            return None
        self._deps(e, reads, writes)
        ins = fn(self.eng[e])
        self.cnt[e] += 1
        self.ins[e].append(ins)
        ev = (None, self.cnt[e], e)
        self._post(ev, reads, writes)
        return ev

    def dma(self, q, out, in_, reads=(), writes=()):
        if self.rec is not None:
            self.rec.append(("dma", (q, out, in_, tuple(reads), tuple(writes))))
            return None
        self._deps(q, reads, writes)
        k = self.di[q] % self.KD
        self.di[q] += 1
        key = "d_%s%d" % (q, k)
        sem = self.dsem[q][k]
        if self.duse[q][k] > 0:
            self.wait(q, (sem, 16 * self.duse[q][k], key))
        ins = self.eng[q].dma_start(out=out, in_=in_)
        self.duse[q][k] += 1
        ins.then_inc(sem, 16)
        ev = (sem, 16 * self.duse[q][k], key)
        self._post(ev, reads, writes)
        return ev

    def barrier(self):
        evs = [(None, self.cnt[e], e) for e in self.eng if self.cnt[e] > 0]
        for q in self.dsem:
            for k in range(self.KD):
                if self.duse[q][k] > 0:
                    evs.append((self.dsem[q][k], 16 * self.duse[q][k], "d_%s%d" % (q, k)))
        for e in self.eng:
            for ev in evs:
                self.wait(e, ev)


class Ring:
    def __init__(self, kb, n, shape, dt=F32):
        self.t = [kb.sb(shape, dt) for _ in range(n)]
        self.b = [Buf() for _ in range(n)]
        self.i = 0

    def next(self):
        j = self.i % len(self.t)
        self.i += 1
        return self.t[j], self.b[j]


def make_consts():
    c = np.zeros((128, 1024), np.float32)
    c[:, 0:128] = np.eye(128, dtype=np.float32)
    ii = np.arange(CH)
    for d in range(2):
        before = (ii[:, None] > ii[None, :]) if d == 1 else (ii[:, None] < ii[None, :])
        beq = before | np.eye(CH, dtype=bool)
        c[0:CH, 128 + d * 128:128 + d * 128 + 64] = before
        c[0:CH, 128 + d * 128 + 64:128 + d * 128 + 128] = beq
        c[0:CH, 384 + d * 64:384 + d * 64 + 64] = before.T
        c[0:CH, 512 + d * 64:512 + d * 64 + 64] = beq
    c[:, 640:768] = 1.0
    c[0, 768:896] = 1.0
    c[1, 896:1024] = 1.0
    return c


def make_rope():
    rows = TL // 64
    row = np.repeat(np.arange(rows), 64).astype(np.float64)
    col = np.tile(np.arange(64), rows).astype(np.float64)
    inv = 10000.0 ** (-np.arange(0, 32, 2, dtype=np.float64) / 32)
    ar = row[:, None] * inv
    ac = col[:, None] * inv
    cosT = np.concatenate([np.cos(ar), np.cos(ar), np.cos(ac), np.cos(ac)], 1)
    sinT = np.concatenate([-np.sin(ar), np.sin(ar), -np.sin(ac), np.sin(ac)], 1)
    return np.concatenate([cosT, sinT], 1).astype(np.float32)


PARAMS = [("w_ada", [DEPTH, D, 6 * D]), ("b_ada", [DEPTH, 6 * D]), ("w_in", [DEPTH, D, INW]), ("w_out", [DEPTH, D, D]),
          ("ln1_g", [DEPTH, D]), ("ln1_b", [DEPTH, D]), ("ln2_g", [DEPTH, D]), ("ln2_b", [DEPTH, D]),
          ("lam_q1", [DEPTH, 64]), ("lam_k1", [DEPTH, 64]), ("lam_q2", [DEPTH, 64]), ("lam_k2", [DEPTH, 64]),
          ("subln_g", [DEPTH, 128]), ("rkv_conv", [DEPTH, 3, 768]), ("decay_w0", [DEPTH, 2, 256]),
          ("decay_up", [DEPTH, 2, 32, 256]), ("iclr_a0", [DEPTH, 2, 256]), ("iclr_up", [DEPTH, 2, 32, 256]),
          ("gate_up", [DEPTH, 64, 256]), ("k_k", [DEPTH, 256]), ("k_a", [DEPTH, 256]), ("r_k", [DEPTH, 256]),
          ("gn_g", [DEPTH, 256]), ("gn_b", [DEPTH, 256]), ("conv_w", [DEPTH, 3, 256]),
          ("ffn_w1", [2, D, DFF]), ("ffn_w3", [2, D, DFF]), ("ffn_w2", [2, DFF, D]),
          ("router_w", [2, D, NE]), ("router_b", [2, NE]),
          ("moe_w1", [2, NE, D, DFE]), ("moe_w3", [2, NE, D, DFE]), ("moe_w2", [2, NE, DFE, D])]


def build(n_layers=DEPTH, dump=None):
    nc = bass.Bass("TRN2", target_bir_lowering=False)
    kb = KB(nc)
    I = {}
    I["x"] = nc.dram_tensor("x", [TL, D], F32, kind="ExternalInput").ap()
    I["ctx"] = nc.dram_tensor("ctx", [TC, D], F32, kind="ExternalInput").ap()
    I["cvec"] = nc.dram_tensor("cvec", [16, 128], F32, kind="ExternalInput").ap()
    I["consts"] = nc.dram_tensor("consts", [128, 1024], F32, kind="ExternalInput").ap()
    I["rope"] = nc.dram_tensor("rope", [TL, 128], F32, kind="ExternalInput").ap()
    for n, s in PARAMS:
        I[n] = nc.dram_tensor(n, s, F32, kind="ExternalInput").ap()
    OUT = nc.dram_tensor("out", [TL, D], F32, kind="ExternalOutput").ap()
    dumps = {}

    def scratch(name, shape, dt=F32):
        if dump and name in dump:
            dumps[name] = nc.dram_tensor("dump_" + name, shape, dt, kind="ExternalOutput").ap()
            return dumps[name]
        return nc.dram_tensor("scr_" + name, shape, dt).ap()

    X = scratch("X", [T, D])
    X1 = scratch("X1", [T, D])
    QT = scratch("QT", [4, 128, T], BF16)
    KT = scratch("KT", [4, 128, T], BF16)
    VV = scratch("VV", [T, 512], BF16)
    RKV = scratch("RKV", [PR, 768])
    CIN = scratch("CIN", [PR, 768])
    LORA = scratch("LORA", [T, 192])
    MIX = scratch("MIX", [T, D])
    PREP = scratch("PREP", [T, 2564])
    YD = scratch("YD", [2, T, 256])
    MODD = scratch("MODD", [2, 6 * D])
    dB = {n: [Buf() for _ in range(NT + 2)] for n in ("X", "X1", "QK", "VV", "RKV", "CIN", "LORA", "MIX", "PREP", "YD0", "YD1")}

    cst = kb.sb([128, 1024], F32, "cst")
    cstB = Buf()
    kb.dma("sp", cst[:], I["consts"][:, :], writes=[cstB])
    ident = cst[:, 0:128]
    zrow = kb.sb([1, 768], F32, "zrow")
    zB = Buf()
    kb.op("pool", lambda e: e.memset(zrow[:], 0.0), writes=[zB])
    for r_ in (0, TC + 1, PR - 1):
        kb.dma("sp", RKV[r_:r_ + 1, :], zrow[:], reads=[zB])
        kb.dma("sp", CIN[r_:r_ + 1, :], zrow[:], reads=[zB])
    PS = [nc.alloc_psum_tensor("ps%d" % i, [128, 512], F32) for i in range(8)]
    PB = [Buf(excl=True) for _ in range(8)]
    kb.barrier()

    def xsrc(l, i):
        if l == 0:
            return I["ctx"][i * 128:(i + 1) * 128, :] if i < 2 else I["x"][(i - 2) * 128:(i - 1) * 128, :]
        return X[i * 128:(i + 1) * 128, :]


    upto = dump[0] if dump else None
    stop = [False]

    for l in range(n_layers):
        last = (l == DEPTH - 1)
        lam_init = 0.8 - 0.6 * math.exp(-0.3 * l)
        lstack = contextlib.ExitStack()
        kb.stacks.append(lstack)
        modS = kb.sb([128, 2, 4, 8], F32)
        modSB = Buf()
        mrowDB = Buf()

        def load_gate_ln(which):
            gB_ = kb.sb([128, 2, D], F32)
            gBB_ = Buf()
            ln_ = kb.sb([128, 2, D], F32)
            lnB_ = Buf()
            v = 2 if which == 0 else 5
            for w_ in range(2):
                kb.dma("sp", gB_[:, w_, :], MODD[w_, v * D:(v + 1) * D].partition_broadcast(128), reads=[mrowDB], writes=[gBB_])
            for j, n in enumerate((("ln1_g", "ln1_b") if which == 0 else ("ln2_g", "ln2_b"))):
                kb.dma("sp", ln_[:, j, :], I[n][l, :].partition_broadcast(128), writes=[lnB_])
            return gB_, gBB_, ln_, lnB_

        with kb.phase():
            cv = kb.sb([16, 128], F32)
            cvB = Buf()
            kb.dma("sp", cv[:], I["cvec"][:, :], writes=[cvB])
            s2 = kb.sb([128, 8, 2], F32)
            s2B = Buf()
            kb.op("pe", lambda e: e.transpose(out=PS[0][:, 0:16], in_=cv[:], identity=cst[0:16, 0:16]), reads=[cvB, cstB], writes=[PB[0]])
            kb.op("act", lambda e: e.activation(out=s2[:].rearrange("p c w -> p w c"), in_=PS[0][:, 0:16].rearrange("p (w c) -> p w c", w=2), func=AF.Silu),
                  reads=[PB[0]], writes=[s2B])
            wr = Ring(kb, 2, [128, 8, 512], F32)
            ba = kb.sb([2, 6 * D], F32)
            baB = Buf()
            kb.dma("sp", ba[0:1, :], I["b_ada"][l:l + 1, :], writes=[baB])
            kb.dma("sp", ba[1:2, :], I["b_ada"][l:l + 1, :], writes=[baB])
            mrow = kb.sb([2, 6 * D], F32)
            mrowB = Buf()
            for n in range(12):
                wt, wb = wr.next()
                kb.dma("sp", wt[:], I["w_ada"][l, :, n * 512:(n + 1) * 512].rearrange("(c p) n -> p c n", p=128), writes=[wb])
                pb = 1 + (n % 2)
                for c in range(8):
                    kb.op("pe", lambda e, c=c, wt=wt, pb=pb: e.matmul(PS[pb][0:2, :], lhsT=s2[:, c, :], rhs=wt[:, c, :], start=(c == 0), stop=(c == 7)),
                          reads=[s2B, wb], writes=[PB[pb]])
                kb.op("dve", lambda e, n=n, pb=pb: e.tensor_tensor(out=mrow[:, n * 512:(n + 1) * 512], in0=PS[pb][0:2, :], in1=ba[:, n * 512:(n + 1) * 512], op=ALU.add),
                      reads=[PB[pb], baB], writes=[mrowB])
            for v in (1, 4):
                kb.op("dve", lambda e, v=v: e.tensor_scalar(out=mrow[:, v * D:(v + 1) * D], in0=mrow[:, v * D:(v + 1) * D], scalar1=1.0, scalar2=None, op0=ALU.add),
                      reads=[mrowB], writes=[mrowB])
            kb.dma("sp", MODD[:, :], mrow[:], reads=[mrowB], writes=[mrowDB])
            for j, v in enumerate((0, 1, 3, 4)):
                for c in range(8):
                    o = (j * 8 + c) * 2
                    kb.op("pe", lambda e, o=o, v=v, c=c: e.matmul(PS[3][:, o:o + 2], lhsT=mrow[0:2, v * D + c * 128:v * D + (c + 1) * 128],
                                                              rhs=cst[0:2, 0:2], start=True, stop=True), reads=[mrowB, cstB], writes=[PB[3]])
            kb.op("act", lambda e: e.activation(out=modS[:].rearrange("p w j c -> p j c w"), in_=PS[3][:, 0:64].rearrange("p (j c w) -> p j c w", j=4, c=8), func=AF.Identity),
                  reads=[PB[3]], writes=[modSB])
        if upto == "MODD":
            break

        with kb.phase():
            win = kb.sb([128, 8, INW], BF16)
            winB = Buf()
            for c in range(8):
                kb.dma("pool", win[:, c, :], I["w_in"][l, c * 128:(c + 1) * 128, :], writes=[winB])
            xr = Ring(kb, 2, [128, D], F32)
            hT = Ring(kb, 2, [128, 8, 128], BF16)
            qk = Ring(kb, 2, [128, 1024], F32)
            qkr = Ring(kb, 2, [128, 1024], F32)
            qkt = Ring(kb, 2, [128, 1024], F32)
            rp = Ring(kb, 2, [128, 128], F32)
            qkT = Ring(kb, 2, [128, 8, 128], BF16)
            vb = Ring(kb, 2, [128, 512], BF16)
            pj = Ring(kb, 2, [128, 1728], F32)
            chunks = [(0, 512), (512, 1024), (1024, 1536), (1536, 2048), (2048, 2496), (2496, 3008), (3008, 3264)]
            pending = []

            def flush_pending():
                for r_ in pending:
                    for kind, a_ in r_:
                        (kb.op if kind == "op" else kb.dma)(*a_)
                pending.clear()
            for i in range(NT):
                w = 1 if i < 2 else 0
                pr0 = prow(i * 128)
                xt, xb = xr.next()
                kb.dma("sp", xt[:], xsrc(l, i), reads=([dB["X"][i]] if l > 0 else []), writes=[xb])
                for c in range(8):
                    kb.op("pe", lambda e, c=c, xt=xt: e.transpose(out=PS[c // 4][:, (c % 4) * 128:(c % 4 + 1) * 128], in_=xt[:, c * 128:(c + 1) * 128], identity=ident),
                          reads=[xb, cstB], writes=[PB[c // 4]])
                ht, hb = hT.next()
                for c in range(8):
                    kb.op("act", lambda e, c=c, ht=ht, w=w: e.activation(out=ht[:, c, :], in_=PS[c // 4][:, (c % 4) * 128:(c % 4 + 1) * 128], func=AF.Identity,
                                                                      scale=modS[:, w, 1, c:c + 1], bias=modS[:, w, 0, c:c + 1]),
                          reads=[PB[c // 4], modSB], writes=[hb])
                qt, qb = qk.next()
                vt, vbb = vb.next()
                pt, pjb = pj.next()
                for n, (a, b) in enumerate(chunks):
                    pb = 2 + (n % 6)
                    for c in range(8):
                        kb.op("pe", lambda e, c=c, ht=ht, pb=pb, a=a, b=b: e.matmul(PS[pb][:, 0:b - a], lhsT=ht[:, c, :], rhs=win[:, c, a:b], start=(c == 0), stop=(c == 7)),
                              reads=[hb, winB], writes=[PB[pb]])
                    if n < 2:
                        kb.op("act", lambda e, n=n, pb=pb, qt=qt: e.activation(out=qt[:, n * 512:(n + 1) * 512], in_=PS[pb][:, :], func=AF.Identity), reads=[PB[pb]], writes=[qb])
                    elif n == 2:
                        kb.op("dve", lambda e, pb=pb, vt=vt: e.tensor_copy(out=vt[:], in_=PS[pb][:, :]), reads=[PB[pb]], writes=[vbb])
                    else:
                        eng = "dve" if n % 2 == 0 else "act"
                        if eng == "dve":
                            kb.op("dve", lambda e, pb=pb, pt=pt, a=a, b=b: e.tensor_copy(out=pt[:, a - 1536:b - 1536], in_=PS[pb][:, 0:b - a]), reads=[PB[pb]], writes=[pjb])
                        else:
                            kb.op("act", lambda e, pb=pb, pt=pt, a=a, b=b: e.activation(out=pt[:, a - 1536:b - 1536], in_=PS[pb][:, 0:b - a], func=AF.Identity), reads=[PB[pb]], writes=[pjb])
                flush_pending()
                kb.dma("sp", VV[i * 128:(i + 1) * 128, :], vt[:], reads=[vbb], writes=[dB["VV"][i]])
                kb.dma("sp", RKV[pr0:pr0 + 128, :], pt[:, 0:768], reads=[pjb], writes=[dB["RKV"][i]])
                kb.dma("sp", LORA[i * 128:(i + 1) * 128, :], pt[:, 768:960], reads=[pjb], writes=[dB["LORA"][i]])
                kb.dma("sp", CIN[pr0:pr0 + 128, :], pt[:, 960:1728], reads=[pjb], writes=[dB["CIN"][i]])
                src, srcb = qt, qb
                if i >= 2:
                    rt, rb = rp.next()
                    kb.dma("sp", rt[:], I["rope"][(i - 2) * 128:(i - 1) * 128, :], writes=[rb])
                    q1, q1b = qkr.next()
                    q2, q2b = qkt.next()
                    qv = qt[:].rearrange("p (g a h n) -> p g a h n", g=16, a=2, h=2)
                    kb.op("dve", lambda e, qt=qt, q1=q1, rt=rt: e.tensor_tensor(out=q1[:].rearrange("p (g n) -> p g n", g=16), in0=qt[:].rearrange("p (g n) -> p g n", g=16),
                                                                       in1=rt[:, 0:64].unsqueeze(1).to_broadcast([128, 16, 64]), op=ALU.mult), reads=[qb, rb], writes=[q1b])
                    q2v = q2[:].rearrange("p (g a h n) -> p g a h n", g=16, a=2, h=2)
                    sv = rt[:, 64:128].rearrange("p (a h n) -> p a h n", a=2, h=2)
                    for hh in range(2):
                        kb.op("pool", lambda e, hh=hh, qv=qv, q2v=q2v, sv=sv: e.tensor_tensor(out=q2v[:, :, :, hh, :], in0=qv[:, :, :, 1 - hh, :],
                                                                                     in1=sv[:, :, hh, :].unsqueeze(1).to_broadcast([128, 16, 2, 16]), op=ALU.mult),
                              reads=[qb, rb], writes=[q2b])
                    kb.op("dve", lambda e, q1=q1, q2=q2: e.tensor_tensor(out=q1[:], in0=q1[:], in1=q2[:], op=ALU.add), reads=[q1b, q2b], writes=[q1b])
                    src, srcb = q1, q1b
                kb.rec = []
                for c in range(8):
                    kb.op("pe", lambda e, c=c, src=src: e.transpose(out=PS[c // 4][:, (c % 4) * 128:(c % 4 + 1) * 128], in_=src[:, c * 128:(c + 1) * 128], identity=ident),
                          reads=[srcb, cstB], writes=[PB[c // 4]])
                tt, tb = qkT.next()
                for hf in range(2):
                    kb.op("dve" if hf == 0 else "act",
                          (lambda e, tt=tt: e.tensor_copy(out=tt[:, 0:4, :], in_=PS[0][:, :].rearrange("p (c n) -> p c n", c=4))) if hf == 0 else
                          (lambda e, tt=tt: e.activation(out=tt[:, 4:8, :], in_=PS[1][:, :].rearrange("p (c n) -> p c n", c=4), func=AF.Identity)),
                          reads=[PB[hf]], writes=[tb])
                kb.dma("sp", QT[:, :, i * 128:(i + 1) * 128].rearrange("h p t -> p h t"), tt[:, 0:4, :], reads=[tb], writes=[dB["QK"][i]])
                kb.dma("sp", KT[:, :, i * 128:(i + 1) * 128].rearrange("h p t -> p h t"), tt[:, 4:8, :], reads=[tb], writes=[dB["QK"][i]])
                pending.append(kb.rec)
                kb.rec = None
            flush_pending()
        if upto in ("QT", "RKV", "VV"):
            break

        with kb.phase():
            lq = kb.sb([128, 4, 64], F32)
            lqB = Buf()
            for j, n in enumerate(("lam_q1", "lam_k1", "lam_q2", "lam_k2")):
                kb.dma("sp", lq[:, j, :], I[n][l, :].partition_broadcast(128), writes=[lqB])
            lt = kb.sb([128, 2, 64], F32)
            lv = kb.sb([128, 4], F32)
            lvB = Buf()
            kb.op("dve", lambda e: e.tensor_tensor(out=lt[:], in0=lq[:, 0:4:2, :], in1=lq[:, 1:4:2, :], op=ALU.mult), reads=[lqB], writes=[lvB])
            kb.op("dve", lambda e: e.tensor_reduce(out=lv[:, 0:2], in_=lt[:], axis=AX.X, op=ALU.add), reads=[lvB], writes=[lvB])
            kb.op("act", lambda e: e.activation(out=lv[:, 0:2], in_=lv[:, 0:2], func=AF.Exp), reads=[lvB], writes=[lvB])
            kb.op("dve", lambda e: e.tensor_tensor(out=lv[:, 2:3], in0=lv[:, 1:2], in1=lv[:, 0:1], op=ALU.subtract), reads=[lvB], writes=[lvB])
            kb.op("dve", lambda e: e.tensor_scalar(out=lv[:, 3:4], in0=lv[:, 2:3], scalar1=-lam_init, scalar2=None, op0=ALU.add), reads=[lvB], writes=[lvB])
            nlam = lv[:, 3:4]
            sg = kb.sb([128, 128], F32)
            sgB = Buf()
            kb.dma("sp", sg[:], I["subln_g"][l, :].partition_broadcast(128), writes=[sgB])
            kb.op("dve", lambda e: e.tensor_scalar(out=sg[:], in0=sg[:], scalar1=(1.0 - lam_init), scalar2=None, op0=ALU.mult), reads=[sgB], writes=[sgB])
            qT0 = kb.sb([128, T], BF16)
            qT1 = kb.sb([128, T], BF16)
            qTm = [qT0, qT1]
            kTt = kb.sb([128, T], BF16)
            vaug = kb.sb([128, NT, 129], BF16)
            qkvB = Buf()
            kb.op("pool", lambda e: e.memset(vaug[:], 1.0), writes=[qkvB])
            kb.op("pool", lambda e: e.memset(qT0[:], 0.0), writes=[qkvB])
            kb.op("pool", lambda e: e.memset(qT1[:], 0.0), writes=[qkvB])
            PTring = Ring(kb, 4, [128, NT, 512], BF16)
            att = Ring(kb, 2, [128, 128], F32)
            sm = Ring(kb, 2, [128, 8], F32)
            sqt = Ring(kb, 2, [128, 128], F32)
            sbank = [0]
            for h in range(4):
                kb.dma("sp", qT0[0:64, :], QT[h, 0:64, :], reads=dB["QK"][0:NT], writes=[qkvB])
                kb.dma("sp", qT1[64:128, :], QT[h, 64:128, :], reads=dB["QK"][0:NT], writes=[qkvB])
                kb.dma("sp", kTt[:], KT[h, :, :], reads=dB["QK"][0:NT], writes=[qkvB])
                kb.dma("sp", vaug[:, :, 0:128], VV[:, h * 128:(h + 1) * 128].rearrange("(n p) d -> p n d", p=128), reads=dB["VV"][0:NT], writes=[qkvB])
                blocks = ([] if last else [(0, 256, [0, 1])]) + [(256 + 512 * j, 512, list(range(NT))) for j in range(8)]
                prevB = None

                def merge_emit(A, Bp):
                    kb.rec = None
                    nA = max(len(A), 1)
                    nB = len(Bp) if Bp else 0
                    jb = 0
                    for ia, (kind, a_) in enumerate(A):
                        (kb.op if kind == "op" else kb.dma)(*a_)
                        if ia % 2 == 1 or ia == len(A) - 1:
                            tgt = nB * (ia + 1) // nA
                            while jb < tgt:
                                kind2, b_ = Bp[jb]
                                (kb.op if kind2 == "op" else kb.dma)(*b_)
                                jb += 1
                    while jb < nB:
                        kind2, b_ = Bp[jb]
                        (kb.op if kind2 == "op" else kb.dma)(*b_)
                        jb += 1

                for (q0, nq, kts) in blocks:
                    kb.rec = []
                    PTs, PTB = [None, None], [None, None]
                    for m in range(2):
                        PTs[m], PTB[m] = PTring.next()
                    for ki, kt in enumerate(kts):
                        for m in range(2):
                            pb = sbank[0] % 4
                            sbank[0] += 1
                            kb.op("pe", lambda e, pb=pb, m=m, kt=kt, q0=q0, nq=nq: e.matmul(PS[pb][:, 0:nq], lhsT=kTt[:, kt * 128:(kt + 1) * 128],
                                                                                     rhs=qTm[m][:, q0:q0 + nq], start=True, stop=True),
                                  reads=[qkvB], writes=[PB[pb]])
                            kb.op("act", lambda e, pb=pb, pt_=PTs[m], ki=ki, nq=nq: e.activation(out=pt_[:, ki, 0:nq], in_=PS[pb][:, 0:nq], func=AF.Exp, scale=0.125),
                                  reads=[PB[pb]], writes=[PTB[m]])
                    recA = kb.rec
                    kb.rec = []
                    for qs in range(nq // 128):
                        for m in range(2):
                            ob = 4 + 2 * (qs % 2) + m
                            for ki, kt in enumerate(kts):
                                kb.op("pe", lambda e, ob=ob, pt_=PTs[m], ki=ki, kt=kt, qs=qs, n=len(kts): e.matmul(PS[ob][:, 0:129], lhsT=pt_[:, ki, qs * 128:(qs + 1) * 128], rhs=vaug[:, kt, :],
                                                                                            start=(ki == 0), stop=(ki == n - 1)),
                                      reads=[PTB[m], qkvB], writes=[PB[ob]])
                        o0 = 4 + 2 * (qs % 2)
                        o1 = o0 + 1
                        st_, sB = sm.next()
                        at, aB = att.next()
                        sq_, sqB = sqt.next()
                        kb.op("dve", lambda e, st_=st_, o0=o0: e.reciprocal(out=st_[:, 0:1], in_=PS[o0][:, 128:129]), reads=[PB[o0]], writes=[sB])
                        kb.op("dve", lambda e, st_=st_, o1=o1: e.reciprocal(out=st_[:, 1:2], in_=PS[o1][:, 128:129]), reads=[PB[o1]], writes=[sB])
                        kb.op("dve", lambda e, st_=st_: e.tensor_tensor(out=st_[:, 2:3], in0=st_[:, 1:2], in1=nlam, op=ALU.mult), reads=[sB, lvB], writes=[sB])
                        kb.op("dve", lambda e, st_=st_, at=at, o0=o0: e.tensor_scalar(out=at[:], in0=PS[o0][:, 0:128], scalar1=st_[:, 0:1], scalar2=None, op0=ALU.mult),
                              reads=[PB[o0], sB], writes=[aB])
                        kb.op("dve", lambda e, st_=st_, at=at, o1=o1: e.scalar_tensor_tensor(out=at[:], in0=PS[o1][:, 0:128], scalar=st_[:, 2:3], in1=at[:], op0=ALU.mult, op1=ALU.add),
                              reads=[PB[o1], sB, aB], writes=[aB])
                        kb.op("pool", lambda e, at=at, sq_=sq_: e.tensor_tensor(out=sq_[:], in0=at[:], in1=at[:], op=ALU.mult), reads=[aB], writes=[sqB])
                        kb.op("dve", lambda e, st_=st_, sq_=sq_: e.tensor_reduce(out=st_[:, 3:4], in_=sq_[:], axis=AX.X, op=ALU.add), reads=[sqB], writes=[sB])
                        kb.op("dve", lambda e, st_=st_: e.tensor_scalar(out=st_[:, 4:5], in0=st_[:, 3:4], scalar1=1.0 / 128, scalar2=SUBLN_EPS, op0=ALU.mult, op1=ALU.add), reads=[sB], writes=[sB])
                        kb.op("act", lambda e, st_=st_: e.activation(out=st_[:, 5:6], in_=st_[:, 4:5], func=AF.Sqrt), reads=[sB], writes=[sB])
                        kb.op("dve", lambda e, st_=st_: e.reciprocal(out=st_[:, 6:7], in_=st_[:, 5:6]), reads=[sB], writes=[sB])
                        kb.op("dve", lambda e, st_=st_, at=at: e.scalar_tensor_tensor(out=at[:], in0=at[:], scalar=st_[:, 6:7], in1=sg[:], op0=ALU.mult, op1=ALU.mult),
                              reads=[sB, aB, sgB], writes=[aB])
                        t0 = q0 + qs * 128
                        kb.dma("sp", MIX[t0:t0 + 128, h * 128:(h + 1) * 128], at[:], reads=[aB], writes=[dB["MIX"][t0 // 128]])
                    recB = kb.rec
                    merge_emit(recA, prevB)
                    prevB = recB
                merge_emit([], prevB)

        with kb.phase():
            cw = kb.sb([128, 3, 256], F32)
            cwB = Buf()
            for j in range(3):
                kb.dma("sp", cw[:, j, :], I["conv_w"][l, j, :].partition_broadcast(128), writes=[cwB])
            c3 = Ring(kb, 4, [128, 3, 768], F32)
            u3 = Ring(kb, 4, [128, 3, 256], F32)
            co = Ring(kb, 4, [128, 256], F32)
            for i in kb.interleave(range(2 if last else 0, NT), 4):
                pr0 = prow(i * 128)
                ct, cB = c3.next()
                for j in range(3):
                    kb.dma("sp", ct[:, j, :], CIN[pr0 - 1 + j:pr0 - 1 + j + 128, :], reads=dB["CIN"][max(i - 1, 0):i + 2], writes=[cB])
                ut, uB = u3.next()
                ot, oB = co.next()
                kb.op("pool", lambda e, ct=ct, ut=ut: e.tensor_tensor(out=ut[:], in0=ct[:, :, 512:768], in1=ct[:, :, 0:256], op=ALU.mult), reads=[cB], writes=[uB])
                kb.op("dve", lambda e, ut=ut: e.tensor_tensor(out=ut[:], in0=ut[:], in1=cw[:], op=ALU.mult), reads=[uB, cwB], writes=[uB])
                kb.op("dve", lambda e, ut=ut, ot=ot: e.tensor_tensor(out=ot[:], in0=ut[:, 0, :], in1=ut[:, 1, :], op=ALU.add), reads=[uB], writes=[oB])
                kb.op("dve", lambda e, ut=ut, ot=ot: e.tensor_tensor(out=ot[:], in0=ot[:], in1=ut[:, 2, :], op=ALU.add), reads=[uB, oB], writes=[oB])
                kb.op("dve", lambda e, ct=ct, ot=ot: e.tensor_tensor(out=ot[:], in0=ot[:], in1=ct[:, 1, 256:512], op=ALU.mult), reads=[cB, oB], writes=[oB])
                kb.dma("sp", MIX[i * 128:(i + 1) * 128, 768:1024], ot[:], reads=[oB], writes=[dB["MIX"][i]])

        with kb.phase():
            cw3 = kb.sb([128, 3, 768], F32)
            cw3B = Buf()
            for j in range(3):
                kb.dma("sp", cw3[:, j, :], I["rkv_conv"][l, j, :].partition_broadcast(128), writes=[cw3B])
            vecs = kb.sb([128, 3, 256], F32)
            vecsB = Buf()
            for j, n in enumerate(("k_k", "k_a", "r_k")):
                kb.dma("sp", vecs[:, j, :], I[n][l, :].partition_broadcast(128), writes=[vecsB])
            wup = kb.sb([33, 2, 256], F32)
            aup = kb.sb([33, 2, 256], F32)
            gup = kb.sb([64, 256], F32)
            wB = Buf()
            for d in range(2):
                kb.dma("sp", wup[0:32, d, :], I["decay_up"][l, d, :, :], writes=[wB])
                kb.dma("sp", wup[32:33, d, :], I["decay_w0"][l, d:d + 1, :], writes=[wB])
                kb.dma("sp", aup[0:32, d, :], I["iclr_up"][l, d, :, :], writes=[wB])
                kb.dma("sp", aup[32:33, d, :], I["iclr_a0"][l, d:d + 1, :], writes=[wB])
            kb.dma("sp", gup[:], I["gate_up"][l, :, :], writes=[wB])
            lwl = Ring(kb, 2, [33, 2, 128], F32)
            lal = Ring(kb, 2, [33, 2, 128], F32)
            lgl = Ring(kb, 2, [64, 128], F32)
            for rg in (lwl, lal):
                for t_, b_ in zip(rg.t, rg.b):
                    kb.op("pool", lambda e, t_=t_: e.memset(t_[:], 1.0), writes=[b_])
            r3 = Ring(kb, 2, [128, 3, 768], F32)
            lo = Ring(kb, 2, [128, 192], F32)
            rkvr = Ring(kb, 2, [128, 768], F32)
            po = Ring(kb, 2, [128, 2564], F32)
            av = Ring(kb, 2, [128, 512], F32)
            tw = Ring(kb, 2, [128, 512], F32)
            krr = Ring(kb, 2, [128, 256], F32)
            t1r = Ring(kb, 2, [128, 256], F32)
            t2r = Ring(kb, 2, [128, 256], F32)
            smr = Ring(kb, 2, [128, 16], F32)
            for i in kb.interleave(range(NT), 2):
                pq = 4 * (i % 2)
                pr0 = prow(i * 128)
                rt, rB = r3.next()
                for j in range(3):
                    kb.dma("sp", rt[:, j, :], RKV[pr0 - 1 + j:pr0 - 1 + j + 128, :], reads=dB["RKV"][max(i - 1, 0):i + 2], writes=[rB])
                lt_, lB = lo.next()
                kb.dma("sp", lt_[:], LORA[i * 128:(i + 1) * 128, :], reads=[dB["LORA"][i]], writes=[lB])
                kv, kvB = rkvr.next()
                kb.op("pool", lambda e, rt=rt: e.tensor_tensor(out=rt[:], in0=rt[:], in1=cw3[:], op=ALU.mult), reads=[rB, cw3B], writes=[rB])
                kb.op("dve", lambda e, rt=rt, kv=kv: e.tensor_tensor(out=kv[:], in0=rt[:, 0, :], in1=rt[:, 1, :], op=ALU.add), reads=[rB], writes=[kvB])
                kb.op("dve", lambda e, rt=rt, kv=kv: e.tensor_tensor(out=kv[:], in0=kv[:], in1=rt[:, 2, :], op=ALU.add), reads=[rB, kvB], writes=[kvB])
                r_ = kv[:, 0:256]
                k_ = kv[:, 256:512]
                v_ = kv[:, 512:768]
                for j in range(4):
                    kb.op("pe", lambda e, pq=pq, j=j, lt_=lt_: e.transpose(out=PS[pq + 0][0:32, j * 128:(j + 1) * 128], in_=lt_[:, j * 32:(j + 1) * 32], identity=ident), reads=[lB, cstB], writes=[PB[pq + 0]])
                kb.op("pe", lambda e, pq=pq, lt_=lt_: e.transpose(out=PS[pq + 1][0:64, 0:128], in_=lt_[:, 128:192], identity=ident), reads=[lB, cstB], writes=[PB[pq + 1]])
                wl_, wlB = lwl.next()
                al_, alB = lal.next()
                gl_, glB = lgl.next()
                kb.op("act", lambda e, pq=pq, wl_=wl_: e.activation(out=wl_[0:32, :, :], in_=PS[pq + 0][0:32, 0:256].rearrange("p (d n) -> p d n", d=2), func=AF.Tanh), reads=[PB[pq + 0]], writes=[wlB])
                kb.op("act", lambda e, pq=pq, al_=al_: e.activation(out=al_[0:32, :, :], in_=PS[pq + 0][0:32, 256:512].rearrange("p (d n) -> p d n", d=2), func=AF.Identity), reads=[PB[pq + 0]], writes=[alB])
                kb.op("act", lambda e, pq=pq, gl_=gl_: e.activation(out=gl_[:], in_=PS[pq + 1][0:64, 0:128], func=AF.Sigmoid), reads=[PB[pq + 1]], writes=[glB])
                for d in range(2):
                    kb.op("pe", lambda e, pq=pq, d=d, wl_=wl_: e.matmul(PS[pq + 2][:, d * 256:(d + 1) * 256], lhsT=wl_[0:33, d, :], rhs=wup[0:33, d, :], start=True, stop=True), reads=[wlB, wB], writes=[PB[pq + 2]])
                    kb.op("pe", lambda e, pq=pq, d=d, al_=al_: e.matmul(PS[pq + 3][:, d * 256:(d + 1) * 256], lhsT=al_[0:33, d, :], rhs=aup[0:33, d, :], start=True, stop=True), reads=[alB, wB], writes=[PB[pq + 3]])
                kb.op("pe", lambda e, pq=pq, gl_=gl_: e.matmul(PS[pq + 1][:, 256:512], lhsT=gl_[:], rhs=gup[:], start=True, stop=True), reads=[glB, wB], writes=[PB[pq + 1]])
                pt, pB = po.next()
                a_, aB = av.next()
                w_, wwB = tw.next()
                kb.op("act", lambda e, pq=pq, w_=w_: e.activation(out=w_[:], in_=PS[pq + 2][:, :], func=AF.Sigmoid), reads=[PB[pq + 2]], writes=[wwB])
                kb.op("act", lambda e, pq=pq, a_=a_: e.activation(out=a_[:], in_=PS[pq + 3][:, :], func=AF.Sigmoid), reads=[PB[pq + 3]], writes=[aB])
                kb.op("act", lambda e, pq=pq, pt=pt: e.activation(out=pt[:, 2304:2560], in_=PS[pq + 1][:, 256:512], func=AF.Identity), reads=[PB[pq + 1]], writes=[pB])
                for d in range(2):
                    kb.op("dve", lambda e, d=d, w_=w_, pt=pt: e.tensor_scalar(out=pt[:, d * 1536:d * 1536 + 256], in0=w_[:, d * 256:(d + 1) * 256], scalar1=-0.6065306597126334, scalar2=None, op0=ALU.mult),
                          reads=[wwB], writes=[pB])
                kr, krB = krr.next()
                t1, t1B = t1r.next()
                t2, t2B = t2r.next()
                sm_, smB = smr.next()
                kb.op("dve", lambda e, kr=kr, k_=k_: e.tensor_tensor(out=kr[:], in0=k_, in1=vecs[:, 0, :], op=ALU.mult), reads=[kvB, vecsB], writes=[krB])
                kb.op("pool", lambda e, kr=kr, t1=t1: e.tensor_tensor(out=t1[:], in0=kr[:], in1=kr[:], op=ALU.mult), reads=[krB], writes=[t1B])
                kb.op("dve", lambda e, t1=t1, sm_=sm_: e.tensor_reduce(out=sm_[:, 0:4], in_=t1[:].rearrange("p (h n) -> p h n", h=4), axis=AX.X, op=ALU.add), reads=[t1B], writes=[smB])
                kb.op("dve", lambda e, sm_=sm_: e.tensor_scalar(out=sm_[:, 0:4], in0=sm_[:, 0:4], scalar1=1e-24, scalar2=None, op0=ALU.max), reads=[smB], writes=[smB])
                kb.op("act", lambda e, sm_=sm_: e.activation(out=sm_[:, 4:8], in_=sm_[:, 0:4], func=AF.Sqrt), reads=[smB], writes=[smB])
                kb.op("dve", lambda e, sm_=sm_: e.reciprocal(out=sm_[:, 8:12], in_=sm_[:, 4:8]), reads=[smB], writes=[smB])
                kb.op("dve", lambda e, sm_=sm_, kr=kr, pt=pt: e.tensor_tensor(out=pt[:, 768:1024].rearrange("p (h n) -> p h n", h=4), in0=kr[:].rearrange("p (h n) -> p h n", h=4),
                                                                       in1=sm_[:, 8:12].unsqueeze(2).to_broadcast([128, 4, 64]), op=ALU.mult), reads=[smB, krB], writes=[pB])
                kb.op("pool", lambda e, pt=pt, r_=r_: e.tensor_copy(out=pt[:, 1024:1280], in_=r_), reads=[kvB], writes=[pB])
                kb.op("pool", lambda e, pt=pt, v_=v_: e.tensor_copy(out=pt[:, 1280:1536], in_=v_), reads=[kvB], writes=[pB])
                for d in range(2):
                    ob = d * 1536
                    kb.op("dve", lambda e, d=d, a_=a_, t1=t1: e.scalar_tensor_tensor(out=t1[:], in0=a_[:, d * 256:(d + 1) * 256], scalar=-1.0, in1=vecs[:, 1, :], op0=ALU.add, op1=ALU.mult),
                          reads=[aB, vecsB, t1B], writes=[t1B])
                    kb.op("dve", lambda e, ob=ob, t1=t1, pt=pt, k_=k_: e.scalar_tensor_tensor(out=pt[:, ob + 512:ob + 768], in0=t1[:], scalar=1.0, in1=k_, op0=ALU.add, op1=ALU.mult),
                          reads=[t1B, kvB], writes=[pB])
                    kb.op("pool", lambda e, ob=ob, d=d, a_=a_, pt=pt: e.tensor_tensor(out=pt[:, ob + 256:ob + 512], in0=pt[:, 768:1024], in1=a_[:, d * 256:(d + 1) * 256], op=ALU.mult),
                          reads=[aB, pB], writes=[pB])
                kb.op("pool", lambda e, pt=pt, t2=t2: e.tensor_tensor(out=t2[:], in0=pt[:, 512:768], in1=pt[:, 2048:2304], op=ALU.add), reads=[pB], writes=[t2B])
                kb.op("pool", lambda e, t2=t2, r_=r_: e.tensor_tensor(out=t2[:], in0=t2[:], in1=r_, op=ALU.mult), reads=[kvB, t2B], writes=[t2B])
                kb.op("pool", lambda e, t2=t2: e.tensor_tensor(out=t2[:], in0=t2[:], in1=vecs[:, 2, :], op=ALU.mult), reads=[vecsB, t2B], writes=[t2B])
                kb.op("dve", lambda e, t2=t2, pt=pt: e.tensor_reduce(out=pt[:, 2560:2564], in_=t2[:].rearrange("p (h n) -> p h n", h=4), axis=AX.X, op=ALU.add), reads=[t2B], writes=[pB])
                kb.dma("sp", PREP[i * 128:(i + 1) * 128, :], pt[:], reads=[pB], writes=[dB["PREP"][i]])
        if upto == "PREP":
            break

        with kb.phase():
            idR = kb.sb([64, 64], F32R)
            idRB = Buf()
            kb.op("act", lambda e: e.activation(out=idR[:], in_=cst[0:64, 0:64], func=AF.Identity), reads=[cstB], writes=[idRB])
            id64 = cst[0:64, 0:64]

            def dir_gen(d):
                B0, B1, B2, B3 = 4 * d, 4 * d + 1, 4 * d + 2, 4 * d + 3
                ST = kb.sb([64, 4, 64], F32R)
                STB = Buf()
                chk = Ring(kb, 3, [64, 1536], F32)
                Er = Ring(kb, 2, [64, 3, 256], F32)
                HTr = Ring(kb, 2, [64, 4, 256], F32R)
                FMr = Ring(kb, 2, [64, 4, 4, 64], F32R)
                Gr = Ring(kb, 2, [64, 4, 2, 128], F32R)
                NTr = Ring(kb, 2, [64, 4, 64], F32R)
                NRr = Ring(kb, 3, [64, 4, 128], F32R)
                NTar = Ring(kb, 3, [64, 4, 64], F32R)
                Xr = Ring(kb, 2, [64, 4, 64], F32R)
                Ur = Ring(kb, 2, [64, 4, 64], F32R)
                Yr = Ring(kb, 2, [64, 256], F32)
                PCr = Ring(kb, 2, [64, 4], F32)
                PPr = Ring(kb, 2, [64, 256], F32)
                vRr = Ring(kb, 2, [64, 256], F32R)
                for h in range(4):
                    kb.op("act", lambda e, h=h: e.activation(out=ST[:, h, :], in_=cst[0:64, 0:64], func=AF.Identity, scale=0.0), reads=[cstB], writes=[STB])
                if d == 0:
                    o_lw, o_b, o_kd, o_kk, o_r, o_v, c0 = 0, 256, 512, 768, 1024, 1280, 0
                else:
                    o_kk, o_r, o_v, o_lw, o_b, o_kd, c0 = 0, 256, 512, 768, 1024, 1280, 768
                mask = cst[0:64, 128 + d * 128:256 + d * 128]
                maskT = cst[0:64, 384 + d * 64:448 + d * 64]
                tri = cst[0:64, 512 + d * 64:576 + d * 64]
                order = range(NCH) if d == 0 else ([3, 2, 1, 0] + list(range(NCH - 1, 3, -1)))
                for c in order:
                    ck, ckB = chk.next()
                    kb.dma("sp", ck[:], PREP[c * 64:(c + 1) * 64, c0:c0 + 1536], reads=[dB["PREP"][c // 2]], writes=[ckB])
                    lw = ck[:, o_lw:o_lw + 256]
                    kb.op("pe", lambda e: e.matmul(PS[B0][0:64, 0:256], lhsT=tri, rhs=lw, start=True, stop=True), reads=[ckB, cstB], writes=[PB[B0]])
                    for hf in range(4):
                        kb.op("pe", lambda e, hf=hf: e.matmul(PS[B0][0:64, 256 + 2 * hf:258 + 2 * hf], lhsT=lw[:, hf * 64:(hf + 1) * 64], rhs=cst[0:64, 640:642], start=True, stop=True),
                              reads=[ckB, cstB], writes=[PB[B0]])
                    yield
                    E, EB = Er.next()
                    PC, PCB = PCr.next()
                    kb.op("act", lambda e: e.activation(out=E[:, 0, :], in_=PS[B0][0:64, 0:256], func=AF.Exp), reads=[PB[B0]], writes=[EB])
                    kb.op("act", lambda e: e.activation(out=E[:, 1, :], in_=PS[B0][0:64, 0:256], func=AF.Exp, scale=-1.0), reads=[PB[B0]], writes=[EB])
                    kb.op("act", lambda e: e.activation(out=E[:, 2, :], in_=lw, func=AF.Exp, scale=-1.0), reads=[ckB], writes=[EB])
                    kb.op("act", lambda e: e.activation(out=PC[:], in_=PS[B0][0:64, 256:264:2], func=AF.Exp), reads=[PB[B0]], writes=[PCB])
                    vR, vRB = vRr.next()
                    kb.op("pool", lambda e: e.tensor_copy(out=vR[:], in_=ck[:, o_v:o_v + 256]), reads=[ckB], writes=[vRB])
                    yield
                    HT, HTB = HTr.next()
                    kb.op("dve", lambda e: e.tensor_tensor(out=HT[:, 0, :], in0=ck[:, o_b:o_b + 256], in1=E[:, 1, :], op=ALU.mult), reads=[ckB, EB], writes=[HTB])
                    kb.op("pool", lambda e: e.tensor_tensor(out=HT[:, 1, :], in0=ck[:, o_kd:o_kd + 256], in1=E[:, 1, :], op=ALU.mult), reads=[ckB, EB], writes=[HTB])
                    kb.op("dve", lambda e: e.scalar_tensor_tensor(out=HT[:, 2, :], in0=ck[:, o_kk:o_kk + 256], scalar=-1.0, in1=E[:, 2, :], op0=ALU.mult, op1=ALU.mult),
                          reads=[ckB, EB], writes=[HTB])
                    kb.op("pool", lambda e: e.tensor_tensor(out=HT[:, 3, :], in0=ck[:, o_r:o_r + 256], in1=E[:, 0, :], op=ALU.mult), reads=[ckB, EB], writes=[HTB])
                    yield
                    kb.op("dve", lambda e: e.tensor_tensor(out=HT[:, 2, :], in0=HT[:, 2, :].bitcast(F32), in1=E[:, 0, :], op=ALU.mult), reads=[EB, HTB], writes=[HTB])
                    yield
                    FM, FMB = FMr.next()
                    for hh in range(2):
                        for h in (2 * hh, 2 * hh + 1):
                            for q in range(4):
                                o = ((h % 2) * 4 + q) * 64
                                kb.op("pe", lambda e, q=q, h=h, o=o: e.transpose(out=PS[B1][0:64, o:o + 64], in_=HT[:, q, h * 64:(h + 1) * 64].bitcast(F32), identity=id64), reads=[HTB, cstB], writes=[PB[B1]])
                        yield
                        kb.op("act", lambda e, hh=hh: e.activation(out=FM[:, 2 * hh:2 * hh + 2, :, :].rearrange("p a q n -> p (a q n)"), in_=PS[B1][0:64, :], func=AF.Identity), reads=[PB[B1]], writes=[FMB])
                        yield
                    G, GB = Gr.next()
                    for hh in range(2):
                        for h in (2 * hh, 2 * hh + 1):
                            for g in range(2):
                                o = ((h % 2) * 2 + g) * 128
                                kb.op("pe", lambda e, h=h, g=g, o=o: e.matmul(PS[B2][0:64, o:o + 128], lhsT=FM[:, h, g, :], rhs=FM[:, h, 2:4, :].rearrange("p a n -> p (a n)"), start=True, stop=True),
                                      reads=[FMB], writes=[PB[B2]])
                        if hh == 0:
                            for h in range(4):
                                kb.op("pe", lambda e, h=h: e.matmul(PS[B3][0:64, h * 64:(h + 1) * 64], lhsT=FM[:, h, 2, :], rhs=FM[:, h, 0, :], start=True, stop=True), reads=[FMB], writes=[PB[B3]])
                        yield
                        kb.op("act", lambda e, hh=hh: e.activation(out=G[:, 2 * hh:2 * hh + 2, :, :].rearrange("p h g n -> p (h g n)"), in_=PS[B2][0:64, :], func=AF.Identity), reads=[PB[B2]], writes=[GB])
                        yield
                    NTt, NTB = NTr.next()
                    kb.op("act", lambda e: e.activation(out=NTt[:].rearrange("p h n -> p (h n)"), in_=PS[B3][0:64, 0:256], func=AF.Identity), reads=[PB[B3]], writes=[NTB])
                    kb.op("pool", lambda e: e.tensor_tensor(out=G[:].rearrange("p h g n -> p (h g) n"), in0=G[:].bitcast(F32).rearrange("p h g n -> p (h g) n"),
                                                           in1=mask.unsqueeze(1).to_broadcast([64, 8, 128]), op=ALU.mult), reads=[GB, cstB], writes=[GB])
                    yield
                    kb.op("dve", lambda e: e.tensor_tensor(out=NTt[:], in0=NTt[:].bitcast(F32), in1=maskT.unsqueeze(1).to_broadcast([64, 4, 64]), op=ALU.mult), reads=[NTB, cstB], writes=[NTB])
                    NR, NRB = NRr.next()
                    NTa, NTaB = NTar.next()
                    kb.op("pool", lambda e, NR=NR: e.tensor_tensor(out=NR[:, :, 64:128], in0=G[:, :, 0, 0:64].bitcast(F32), in1=id64.unsqueeze(1).to_broadcast([64, 4, 64]), op=ALU.add), reads=[GB, cstB], writes=[NRB])
                    for h in range(4):
                        kb.op("pe", lambda e, h=h: e.matmul(PS[B1][0:64, h * 64:(h + 1) * 64], lhsT=NTt[:, h, :], rhs=G[:, h, 0, 0:64], start=True, stop=True), reads=[GB, NTB], writes=[PB[B1]])
                    for h in range(4):
                        kb.op("pe", lambda e, h=h: e.matmul(PS[B2][0:64, h * 64:(h + 1) * 64], lhsT=G[:, h, 0, 0:64], rhs=NTt[:, h, :], start=True, stop=True), reads=[GB, NTB], writes=[PB[B2]])
                    yield
                    kb.op("act", lambda e, NR=NR: e.activation(out=NR[:, :, 0:64], in_=PS[B1][0:64, 0:256].rearrange("p (h n) -> p h n", h=4), func=AF.Identity), reads=[PB[B1]], writes=[NRB])
                    kb.op("act", lambda e, NTa=NTa: e.activation(out=NTa[:].rearrange("p h n -> p (h n)"), in_=PS[B2][0:64, 0:256], func=AF.Identity), reads=[PB[B2]], writes=[NTaB])
                    yield
                    for s_ in range(1, 6):
                        lastS = (s_ == 5)
                        NR2, NR2B = NRr.next()
                        NTa2, NTa2B = NTar.next()
                        wN = 64 if lastS else 128
                        for h in range(4):
                            kb.op("pe", lambda e, h=h, NR=NR, NTa=NTa, wN=wN: e.matmul(PS[B1][0:64, h * 128:h * 128 + wN], lhsT=NTa[:, h, :], rhs=NR[:, h, 128 - wN:128], start=True, stop=True),
                                  reads=[NRB, NTaB], writes=[PB[B1]])
                        if not lastS:
                            for h in range(4):
                                kb.op("pe", lambda e, h=h, NR=NR, NTa=NTa: e.matmul(PS[B2][0:64, h * 64:(h + 1) * 64], lhsT=NR[:, h, 0:64], rhs=NTa[:, h, :], start=True, stop=True),
                                      reads=[NRB, NTaB], writes=[PB[B2]])
                        yield
                        PPt, PPB = PPr.next()
                        pv = PS[B1][0:64, :].rearrange("p (h n) -> p h n", h=4)
                        if lastS:
                            kb.op("act", lambda e, PPt=PPt, pv=pv: e.activation(out=PPt[:].rearrange("p (h n) -> p h n", h=4), in_=pv[:, :, 0:64], func=AF.Identity), reads=[PB[B1]], writes=[PPB])
                        else:
                            kb.op("act", lambda e, PPt=PPt, pv=pv: e.activation(out=PPt[:].rearrange("p (h n) -> p h n", h=4), in_=pv[:, :, 64:128], func=AF.Identity), reads=[PB[B1]], writes=[PPB])
                            kb.op("act", lambda e, NR2=NR2, pv=pv: e.activation(out=NR2[:, :, 0:64], in_=pv[:, :, 0:64], func=AF.Identity), reads=[PB[B1]], writes=[NR2B])
                            kb.op("act", lambda e, NTa2=NTa2: e.activation(out=NTa2[:].rearrange("p h n -> p (h n)"), in_=PS[B2][0:64, 0:256], func=AF.Identity), reads=[PB[B2]], writes=[NTa2B])
                        yield
                        kb.op("dve", lambda e, NR=NR, NR2=NR2, PPt=PPt: e.tensor_tensor(out=NR2[:, :, 64:128], in0=NR[:, :, 64:128].bitcast(F32), in1=PPt[:].rearrange("p (h n) -> p h n", h=4), op=ALU.add),
                              reads=[PPB, NRB], writes=[NR2B])
                        yield
                        NR, NRB, NTa, NTaB = NR2, NR2B, NTa2, NTa2B
                    Tm = lambda h, NR=NR: NR[:, h, 64:128]
                    PmB = NRB
                    Xs, XB = Xr.next()
                    Us, UB = Ur.next()
                    Ys, YB = Yr.next()
                    vh = lambda h: vR[:, h * 64:(h + 1) * 64]
                    for h in range(4):
                        kb.op("pe", lambda e, h=h: e.matmul(PS[B1][0:64, h * 64:(h + 1) * 64], lhsT=FM[:, h, 2, :], rhs=ST[:, h, :], start=True, stop=False), reads=[FMB, STB], writes=[PB[B1]])
                        kb.op("pe", lambda e, h=h: e.matmul(PS[B1][0:64, h * 64:(h + 1) * 64], lhsT=G[:, h, 1, 0:64], rhs=vh(h), start=False, stop=True), reads=[GB, vRB], writes=[PB[B1]])
                    yield
                    kb.op("act", lambda e: e.activation(out=Xs[:].rearrange("p h n -> p (h n)"), in_=PS[B1][0:64, 0:256], func=AF.Identity), reads=[PB[B1]], writes=[XB])
                    yield
                    for h in range(4):
                        kb.op("pe", lambda e, h=h: e.matmul(PS[B2][0:64, h * 64:(h + 1) * 64], lhsT=Tm(h), rhs=Xs[:, h, :], start=True, stop=True), reads=[PmB, XB], writes=[PB[B2]])
                    yield
                    kb.op("act", lambda e: e.activation(out=Us[:].rearrange("p h n -> p (h n)"), in_=PS[B2][0:64, 0:256], func=AF.Identity), reads=[PB[B2]], writes=[UB])
                    yield
                    for h in range(4):
                        kb.op("pe", lambda e, h=h: e.matmul(PS[B3][0:64, h * 64:(h + 1) * 64], lhsT=FM[:, h, 3, :], rhs=ST[:, h, :], start=True, stop=False), reads=[FMB, STB], writes=[PB[B3]])
                        kb.op("pe", lambda e, h=h: e.matmul(PS[B3][0:64, h * 64:(h + 1) * 64], lhsT=G[:, h, 0, 64:128], rhs=Us[:, h, :], start=False, stop=False), reads=[GB, UB], writes=[PB[B3]])
                        kb.op("pe", lambda e, h=h: e.matmul(PS[B3][0:64, h * 64:(h + 1) * 64], lhsT=G[:, h, 1, 64:128], rhs=vh(h), start=False, stop=True), reads=[GB, vRB], writes=[PB[B3]])
                    for h in range(4):
                        kb.op("pe", lambda e, h=h: e.matmul(PS[B0][0:64, h * 64:(h + 1) * 64], lhsT=idR[:], rhs=ST[:, h, :], start=True, stop=False), reads=[STB, idRB], writes=[PB[B0]])
                        kb.op("pe", lambda e, h=h: e.matmul(PS[B0][0:64, h * 64:(h + 1) * 64], lhsT=HT[:, 0, h * 64:(h + 1) * 64], rhs=Us[:, h, :], start=False, stop=False), reads=[HTB, UB], writes=[PB[B0]])
                        kb.op("pe", lambda e, h=h: e.matmul(PS[B0][0:64, h * 64:(h + 1) * 64], lhsT=HT[:, 1, h * 64:(h + 1) * 64], rhs=vh(h), start=False, stop=True), reads=[HTB, vRB], writes=[PB[B0]])
                    yield
                    kb.op("act", lambda e: e.activation(out=Ys[:], in_=PS[B3][0:64, 0:256], func=AF.Identity), reads=[PB[B3]], writes=[YB])
                    for h in range(4):
                        kb.op("act", lambda e, h=h: e.activation(out=ST[:, h, :], in_=PS[B0][0:64, h * 64:(h + 1) * 64], func=AF.Identity, scale=PC[:, h:h + 1]), reads=[PB[B0], PCB], writes=[STB])
                    kb.dma("sp", YD[d, c * 64:(c + 1) * 64, :], Ys[:], reads=[YB], writes=[dB["YD%d" % d][c // 2]])
                    yield

            gens = [dir_gen(0), dir_gen(1)]
            while gens:
                for g_ in list(gens):
                    try:
                        next(g_)
                    except StopIteration:
                        gens.remove(g_)
        if upto == "YD":
            break

        with kb.phase():
            gnv = kb.sb([128, 2, 256], F32)
            gnB = Buf()
            kb.dma("sp", gnv[:, 0, :], I["gn_g"][l, :].partition_broadcast(128), writes=[gnB])
            kb.dma("sp", gnv[:, 1, :], I["gn_b"][l, :].partition_broadcast(128), writes=[gnB])
            yr = Ring(kb, 4, [128, 2, 256], F32)
            vr = Ring(kb, 4, [128, 256], F32)
            gr = Ring(kb, 4, [128, 260], F32)
            ycr = Ring(kb, 4, [128, 256], F32)
            sqr = Ring(kb, 4, [128, 256], F32)
            smr = Ring(kb, 4, [128, 16], F32)
            for i in kb.interleave(range(2 if last else 0, NT), 4):
                yt, yB = yr.next()
                vt_, vB = vr.next()
                gt, gB = gr.next()
                for d in range(2):
                    kb.dma("sp", yt[:, d, :], YD[d, i * 128:(i + 1) * 128, :], reads=[dB["YD%d" % d][i]], writes=[yB])
                kb.dma("sp", vt_[:], PREP[i * 128:(i + 1) * 128, 1280:1536], reads=[dB["PREP"][i]], writes=[vB])
                kb.dma("sp", gt[:], PREP[i * 128:(i + 1) * 128, 2304:2564], reads=[dB["PREP"][i]], writes=[gB])
                yc, ycB = ycr.next()
                sq_, sqB = sqr.next()
                sm_, smB = smr.next()
                v4 = lambda t_: t_.rearrange("p (h n) -> p h n", h=4)
                bc = lambda a_: a_.unsqueeze(2).to_broadcast([128, 4, 64])
                kb.op("dve", lambda e, yt=yt, yc=yc: e.tensor_tensor(out=yc[:], in0=yt[:, 0, :], in1=yt[:, 1, :], op=ALU.add), reads=[yB], writes=[ycB])
                kb.op("dve", lambda e, yc=yc, sm_=sm_: e.tensor_reduce(out=sm_[:, 0:4], in_=v4(yc[:]), axis=AX.X, op=ALU.add), reads=[ycB], writes=[smB])
                kb.op("dve", lambda e, sm_=sm_: e.tensor_scalar(out=sm_[:, 0:4], in0=sm_[:, 0:4], scalar1=-1.0 / 64, scalar2=None, op0=ALU.mult), reads=[smB], writes=[smB])
                kb.op("dve", lambda e, yc=yc, sm_=sm_: e.tensor_tensor(out=v4(yc[:]), in0=v4(yc[:]), in1=bc(sm_[:, 0:4]), op=ALU.add), reads=[smB, ycB], writes=[ycB])
                kb.op("pool", lambda e, yc=yc, sq_=sq_: e.tensor_tensor(out=sq_[:], in0=yc[:], in1=yc[:], op=ALU.mult), reads=[ycB], writes=[sqB])
                kb.op("dve", lambda e, sq_=sq_, sm_=sm_: e.tensor_reduce(out=sm_[:, 4:8], in_=v4(sq_[:]), axis=AX.X, op=ALU.add), reads=[sqB], writes=[smB])
                kb.op("dve", lambda e, sm_=sm_: e.tensor_scalar(out=sm_[:, 4:8], in0=sm_[:, 4:8], scalar1=1.0 / 64, scalar2=GN_EPS, op0=ALU.mult, op1=ALU.add), reads=[smB], writes=[smB])
                kb.op("act", lambda e, sm_=sm_: e.activation(out=sm_[:, 8:12], in_=sm_[:, 4:8], func=AF.Sqrt), reads=[smB], writes=[smB])
                kb.op("dve", lambda e, sm_=sm_: e.reciprocal(out=sm_[:, 12:16], in_=sm_[:, 8:12]), reads=[smB], writes=[smB])
                kb.op("dve", lambda e, yc=yc, sm_=sm_: e.tensor_tensor(out=v4(yc[:]), in0=v4(yc[:]), in1=bc(sm_[:, 12:16]), op=ALU.mult), reads=[smB, ycB], writes=[ycB])
                kb.op("dve", lambda e, yc=yc: e.tensor_tensor(out=yc[:], in0=yc[:], in1=gnv[:, 0, :], op=ALU.mult), reads=[gnB, ycB], writes=[ycB])
                kb.op("dve", lambda e, yc=yc: e.tensor_tensor(out=yc[:], in0=yc[:], in1=gnv[:, 1, :], op=ALU.add), reads=[gnB, ycB], writes=[ycB])
                kb.op("pool", lambda e, vt_=vt_, gt=gt, sq_=sq_: e.tensor_tensor(out=v4(sq_[:]), in0=v4(vt_[:]), in1=bc(gt[:, 256:260]), op=ALU.mult), reads=[vB, gB, sqB], writes=[sqB])
                kb.op("dve", lambda e, yc=yc, sq_=sq_: e.tensor_tensor(out=yc[:], in0=yc[:], in1=sq_[:], op=ALU.add), reads=[sqB, ycB], writes=[ycB])
                kb.op("dve", lambda e, yc=yc, gt=gt: e.tensor_tensor(out=yc[:], in0=yc[:], in1=gt[:, 0:256], op=ALU.mult), reads=[gB, ycB], writes=[ycB])
                kb.dma("sp", MIX[i * 128:(i + 1) * 128, 512:768], yc[:], reads=[ycB], writes=[dB["MIX"][i]])
        if upto == "MIX":
            break

        def layer_norm_tile(t_, tB, sq_, sqB, sm_, smB, lnp, lnpB):
            gi = 0
            kb.op("dve", lambda e: e.tensor_reduce(out=sm_[:, 0:1], in_=t_[:], axis=AX.X, op=ALU.add), reads=[tB], writes=[smB])
            kb.op("dve", lambda e: e.tensor_scalar(out=sm_[:, 1:2], in0=sm_[:, 0:1], scalar1=-1.0 / D, scalar2=None, op0=ALU.mult), reads=[smB], writes=[smB])
            kb.op("dve", lambda e: e.tensor_scalar(out=t_[:], in0=t_[:], scalar1=sm_[:, 1:2], scalar2=None, op0=ALU.add), reads=[smB, tB], writes=[tB])
            kb.op("pool", lambda e: e.tensor_tensor(out=sq_[:], in0=t_[:], in1=t_[:], op=ALU.mult), reads=[tB], writes=[sqB])
            kb.op("dve", lambda e: e.tensor_reduce(out=sm_[:, 2:3], in_=sq_[:], axis=AX.X, op=ALU.add), reads=[sqB], writes=[smB])
            kb.op("dve", lambda e: e.tensor_scalar(out=sm_[:, 3:4], in0=sm_[:, 2:3], scalar1=1.0 / D, scalar2=LN_EPS, op0=ALU.mult, op1=ALU.add), reads=[smB], writes=[smB])
            kb.op("act", lambda e: e.activation(out=sm_[:, 4:5], in_=sm_[:, 3:4], func=AF.Sqrt), reads=[smB], writes=[smB])
            kb.op("dve", lambda e: e.reciprocal(out=sm_[:, 5:6], in_=sm_[:, 4:5]), reads=[smB], writes=[smB])
            kb.op("dve", lambda e: e.scalar_tensor_tensor(out=t_[:], in0=t_[:], scalar=sm_[:, 5:6], in1=lnp[:, gi, :], op0=ALU.mult, op1=ALU.mult), reads=[smB, tB, lnpB], writes=[tB])
            kb.op("dve", lambda e: e.tensor_tensor(out=t_[:], in0=t_[:], in1=lnp[:, gi + 1, :], op=ALU.add), reads=[tB, lnpB], writes=[tB])

        with kb.phase():
            gate1, gate1B, ln1t, ln1B = load_gate_ln(0)
            wo = kb.sb([128, 8, D], BF16)
            woB = Buf()
            for c in range(8):
                kb.dma("pool", wo[:, c, :], I["w_out"][l, c * 128:(c + 1) * 128, :], writes=[woB])
            mr = Ring(kb, 2, [128, D], F32)
            mTr = Ring(kb, 2, [128, 8, 128], BF16)
            xr = Ring(kb, 2, [128, D], F32)
            tr_ = Ring(kb, 2, [128, D], F32)
            sqr = Ring(kb, 2, [128, D], F32)
            smr = Ring(kb, 2, [128, 8], F32)
            for i in kb.interleave(range(2 if last else 0, NT), 2):
                pq = 4 * (i % 2)
                w = 1 if i < 2 else 0
                mt, mB = mr.next()
                xt, xB = xr.next()
                kb.dma("sp", mt[:], MIX[i * 128:(i + 1) * 128, :], reads=[dB["MIX"][i]], writes=[mB])
                kb.dma("sp", xt[:], xsrc(l, i), reads=([dB["X"][i]] if l > 0 else []), writes=[xB])
                for c in range(8):
                    kb.op("pe", lambda e, pq=pq, c=c, mt=mt: e.transpose(out=PS[pq + c // 4][:, (c % 4) * 128:(c % 4 + 1) * 128], in_=mt[:, c * 128:(c + 1) * 128], identity=ident), reads=[mB, cstB], writes=[PB[pq + c // 4]])
                mT, mTB = mTr.next()
                kb.op("act", lambda e, pq=pq, mT=mT: e.activation(out=mT[:, 0:4, :].rearrange("p c n -> p (c n)"), in_=PS[pq][:, :], func=AF.Identity), reads=[PB[pq]], writes=[mTB])
                kb.op("dve", lambda e, pq=pq, mT=mT: e.tensor_copy(out=mT[:, 4:8, :].rearrange("p c n -> p (c n)"), in_=PS[pq + 1][:, :]), reads=[PB[pq + 1]], writes=[mTB])
                t_, tB = tr_.next()
                for hf in range(2):
                    for c in range(8):
                        kb.op("pe", lambda e, pq=pq, c=c, hf=hf, mT=mT: e.matmul(PS[pq + 2 + hf][:, :], lhsT=mT[:, c, :], rhs=wo[:, c, hf * 512:(hf + 1) * 512], start=(c == 0), stop=(c == 7)), reads=[mTB, woB], writes=[PB[pq + 2 + hf]])
                    kb.op("dve", lambda e, pq=pq, hf=hf, t_=t_, w=w: e.tensor_tensor(out=t_[:, hf * 512:(hf + 1) * 512], in0=PS[pq + 2 + hf][:, :], in1=gate1[:, w, hf * 512:(hf + 1) * 512], op=ALU.mult), reads=[PB[pq + 2 + hf], gate1B], writes=[tB])
                kb.op("dve", lambda e, t_=t_, xt=xt: e.scalar_tensor_tensor(out=t_[:], in0=xt[:], scalar=DN_ALPHA, in1=t_[:], op0=ALU.mult, op1=ALU.add), reads=[xB, tB], writes=[tB])
                sq_, sqB = sqr.next()
                sm_, smB = smr.next()
                layer_norm_tile(t_, tB, sq_, sqB, sm_, smB, ln1t, ln1B)
                kb.dma("sp", X1[i * 128:(i + 1) * 128, :], t_[:], reads=[tB], writes=[dB["X1"][i]])
        if upto == "X1":
            break

        with kb.phase():
            jj = l // 2
            moe = (l % 2 == 1)
            gate2, gate2B, ln2t, ln2B = load_gate_ln(1)
            if moe:
                experts = [(I["moe_w1"][jj, e], I["moe_w3"][jj, e], I["moe_w2"][jj, e]) for e in range(NE)]
            else:
                experts = [(I["ffn_w1"][jj, :, e * DFE:(e + 1) * DFE], I["ffn_w3"][jj, :, e * DFE:(e + 1) * DFE], I["ffn_w2"][jj, e * DFE:(e + 1) * DFE, :]) for e in range(2)]
            NFC = DFE // 128
            x1g = kb.sb([128, 4, D], F32)
            x1B = Buf()
            hT = kb.sb([128, 8, 512], BF16)
            hTB = Buf()
            hTfr = Ring(kb, 2, [128, 8, 128], F32)
            gTs = [kb.sb([128, NFC, 512], BF16) for _ in range(2)]
            gTBs = [Buf(), Buf()]
            acc = kb.sb([128, 4, D], F32)
            accB = Buf()
            w2e = Ring(kb, 2, [128, NFC, D], BF16)
            w1r = Ring(kb, 4, [128, 8, 128], BF16)
            w3r = Ring(kb, 4, [128, 8, 128], BF16)
            sar = Ring(kb, 2, [128, 512], F32)
            gates = kb.sb([128, 4, NE], F32)
            gatesB = Buf()
            rt = kb.sb([128, 4, 24], F32)
            rtB = Buf()
            sqr = Ring(kb, 2, [128, D], F32)
            smr = Ring(kb, 2, [128, 8], F32)
            if moe:
                rw = kb.sb([128, 8, NE], F32)
                rb = kb.sb([128, NE], F32)
                rwB = Buf()
                kb.dma("sp", rw[:], I["router_w"][jj].rearrange("(c p) e -> p c e", p=128), writes=[rwB])
                kb.dma("sp", rb[:], I["router_b"][jj, :].partition_broadcast(128), writes=[rwB])
            groups = ([] if last else [(0, 2)]) + [(2 + 4 * g_, 4) for g_ in range(8)]
            for (i0, nt_) in groups:
                w = 1 if i0 < 2 else 0
                ng = nt_ * 128
                for ti in range(nt_):
                    i = i0 + ti
                    kb.dma("sp", x1g[:, ti, :], X1[i * 128:(i + 1) * 128, :], reads=[dB["X1"][i]], writes=[x1B])
                    for c in range(8):
                        kb.op("pe", lambda e, c=c, ti=ti: e.transpose(out=PS[c // 4][:, (c % 4) * 128:(c % 4 + 1) * 128], in_=x1g[:, ti, c * 128:(c + 1) * 128], identity=ident), reads=[x1B, cstB], writes=[PB[c // 4]])
                    hf_, hfB = hTfr.next()
                    for c in range(8):
                        kb.op("act", lambda e, c=c, hf_=hf_, w=w: e.activation(out=hf_[:, c, :], in_=PS[c // 4][:, (c % 4) * 128:(c % 4 + 1) * 128], func=AF.Identity,
                                                                          scale=modS[:, w, 3, c:c + 1], bias=modS[:, w, 2, c:c + 1]), reads=[PB[c // 4], modSB], writes=[hfB])
                    kb.op("pool", lambda e, hf_=hf_, ti=ti: e.tensor_copy(out=hT[:, :, ti * 128:(ti + 1) * 128], in_=hf_[:]), reads=[hfB], writes=[hTB])
                    if moe:
                        for c in range(8):
                            kb.op("pe", lambda e, c=c, hf_=hf_: e.matmul(PS[2][:, 0:NE], lhsT=hf_[:, c, :], rhs=rw[:, c, :], start=(c == 0), stop=(c == 7)), reads=[hfB, rwB], writes=[PB[2]])
                        r_ = rt[:, ti, :]
                        kb.op("dve", lambda e, r_=r_: e.tensor_tensor(out=r_[:, 0:8], in0=PS[2][:, 0:NE], in1=rb[:], op=ALU.add), reads=[PB[2], rwB], writes=[rtB])
                        kb.op("dve", lambda e, r_=r_: e.tensor_reduce(out=r_[:, 16:17], in_=r_[:, 0:8], axis=AX.X, op=ALU.max), reads=[rtB], writes=[rtB])
                        kb.op("dve", lambda e, r_=r_: e.tensor_scalar(out=r_[:, 8:16], in0=r_[:, 0:8], scalar1=r_[:, 16:17], scalar2=None, op0=ALU.is_equal), reads=[rtB], writes=[rtB])
                        kb.op("dve", lambda e, r_=r_: e.scalar_tensor_tensor(out=r_[:, 0:8], in0=r_[:, 8:16], scalar=-1e30, in1=r_[:, 0:8], op0=ALU.mult, op1=ALU.add), reads=[rtB], writes=[rtB])
                        kb.op("dve", lambda e, r_=r_: e.tensor_reduce(out=r_[:, 17:18], in_=r_[:, 0:8], axis=AX.X, op=ALU.max), reads=[rtB], writes=[rtB])
                        kb.op("dve", lambda e, r_=r_: e.tensor_scalar(out=r_[:, 0:8], in0=r_[:, 0:8], scalar1=r_[:, 17:18], scalar2=None, op0=ALU.is_equal), reads=[rtB], writes=[rtB])
                        kb.op("dve", lambda e, r_=r_: e.tensor_tensor(out=r_[:, 18:19], in0=r_[:, 16:17], in1=r_[:, 17:18], op=ALU.subtract), reads=[rtB], writes=[rtB])
                        kb.op("act", lambda e, r_=r_: e.activation(out=r_[:, 19:20], in_=r_[:, 18:19], func=AF.Sigmoid), reads=[rtB], writes=[rtB])
                        kb.op("act", lambda e, r_=r_: e.activation(out=r_[:, 20:21], in_=r_[:, 18:19], func=AF.Sigmoid, scale=-1.0), reads=[rtB], writes=[rtB])
                        kb.op("dve", lambda e, r_=r_: e.tensor_scalar(out=r_[:, 8:16], in0=r_[:, 8:16], scalar1=r_[:, 19:20], scalar2=None, op0=ALU.mult), reads=[rtB], writes=[rtB])
                        kb.op("dve", lambda e, r_=r_, ti=ti: e.scalar_tensor_tensor(out=gates[:, ti, :], in0=r_[:, 0:8], scalar=r_[:, 20:21], in1=r_[:, 8:16], op0=ALU.mult, op1=ALU.add), reads=[rtB], writes=[gatesB])
                for ei, (W1, W3, W2) in enumerate(experts):
                    gT, gTB = gTs[ei % 2], gTBs[ei % 2]
                    w2t, w2B = w2e.next()
                    kb.dma("pool", w2t[:], W2.rearrange("(c p) n -> p c n", p=128), writes=[w2B])
                    for fc in range(NFC):
                        w1t, w1B = w1r.next()
                        w3t, w3B = w3r.next()
                        kb.dma("pool", w1t[:], W1[:, fc * 128:(fc + 1) * 128].rearrange("(c p) f -> p c f", p=128), writes=[w1B])
                        kb.dma("pool", w3t[:], W3[:, fc * 128:(fc + 1) * 128].rearrange("(c p) f -> p c f", p=128), writes=[w3B])
                        pa = 2 + (fc % 2)
                        pbb = 4 + (fc % 2)
                        for c in range(8):
                            kb.op("pe", lambda e, c=c, w1t=w1t, ng=ng, pa=pa: e.matmul(PS[pa][:, 0:ng], lhsT=w1t[:, c, :], rhs=hT[:, c, 0:ng], start=(c == 0), stop=(c == 7)), reads=[w1B, hTB], writes=[PB[pa]])
                        for c in range(8):
                            kb.op("pe", lambda e, c=c, w3t=w3t, ng=ng, pbb=pbb: e.matmul(PS[pbb][:, 0:ng], lhsT=w3t[:, c, :], rhs=hT[:, c, 0:ng], start=(c == 0), stop=(c == 7)), reads=[w3B, hTB], writes=[PB[pbb]])
                        sa, saB = sar.next()
                        kb.op("act", lambda e, sa=sa, ng=ng, pa=pa: e.activation(out=sa[:, 0:ng], in_=PS[pa][:, 0:ng], func=AF.Silu), reads=[PB[pa]], writes=[saB])
                        kb.op("dve", lambda e, sa=sa, fc=fc, ng=ng, pbb=pbb: e.tensor_tensor(out=gT[:, fc, 0:ng], in0=PS[pbb][:, 0:ng], in1=sa[:, 0:ng], op=ALU.mult), reads=[PB[pbb], saB], writes=[gTB])
                    for ti in range(nt_):
                        for hf in range(2):
                            for fc in range(NFC):
                                kb.op("pe", lambda e, fc=fc, ti=ti, hf=hf, w2t=w2t: e.matmul(PS[6 + hf][:, :], lhsT=gT[:, fc, ti * 128:(ti + 1) * 128], rhs=w2t[:, fc, hf * 512:(hf + 1) * 512],
                                                                                     start=(fc == 0), stop=(fc == NFC - 1)), reads=[gTB, w2B], writes=[PB[6 + hf]])
                            a_ = acc[:, ti, hf * 512:(hf + 1) * 512]
                            if moe:
                                gsc = gates[:, ti, ei:ei + 1]
                                if ei == 0:
                                    kb.op("dve", lambda e, a_=a_, hf=hf, gsc=gsc: e.tensor_scalar(out=a_, in0=PS[6 + hf][:, :], scalar1=gsc, scalar2=None, op0=ALU.mult), reads=[PB[6 + hf], gatesB], writes=[accB])
                                else:
                                    kb.op("dve", lambda e, a_=a_, hf=hf, gsc=gsc: e.scalar_tensor_tensor(out=a_, in0=PS[6 + hf][:, :], scalar=gsc, in1=a_, op0=ALU.mult, op1=ALU.add),
                                          reads=[PB[6 + hf], gatesB, accB], writes=[accB])
                            else:
                                if ei == 0:
                                    kb.op("act", lambda e, a_=a_, hf=hf: e.activation(out=a_, in_=PS[6 + hf][:, :], func=AF.Identity), reads=[PB[6 + hf]], writes=[accB])
                                else:
                                    kb.op("dve", lambda e, a_=a_, hf=hf: e.tensor_tensor(out=a_, in0=PS[6 + hf][:, :], in1=a_, op=ALU.add), reads=[PB[6 + hf], accB], writes=[accB])
                for ti in range(nt_):
                    i = i0 + ti
                    if last and i < 2:
                        continue
                    a_ = acc[:, ti, :]
                    kb.op("dve", lambda e, a_=a_, w=w: e.tensor_tensor(out=a_, in0=a_, in1=gate2[:, w, :], op=ALU.mult), reads=[accB, gate2B], writes=[accB])
                    kb.op("dve", lambda e, a_=a_, ti=ti: e.scalar_tensor_tensor(out=a_, in0=x1g[:, ti, :], scalar=DN_ALPHA, in1=a_, op0=ALU.mult, op1=ALU.add), reads=[x1B, accB], writes=[accB])
                    sq_, sqB = sqr.next()
                    sm_, smB = smr.next()
                    layer_norm_tile(a_, accB, sq_, sqB, sm_, smB, ln2t, ln2B)
                    if last:
                        kb.dma("sp", OUT[(i - 2) * 128:(i - 1) * 128, :], a_, reads=[accB])
                    else:
                        kb.dma("sp", X[i * 128:(i + 1) * 128, :], a_, reads=[accB], writes=[dB["X"][i]])
        if upto == "X":
            break
        lstack.close()
        kb.stacks.pop()
    kb.barrier()
    return nc, dumps


def host_inputs(inputs, b):
    m = {n: np.ascontiguousarray(np.asarray(inputs[n], dtype=np.float32).reshape(s)) for n, s in PARAMS}
    m["x"] = np.ascontiguousarray(inputs["x"][b], dtype=np.float32)
    m["ctx"] = np.ascontiguousarray(inputs["ctx"][b], dtype=np.float32)
    cv = np.zeros((16, 128), np.float32)
    cv[0:8] = np.asarray(inputs["c"][b], np.float32).reshape(8, 128)
    cv[8:16] = np.asarray(inputs["c_ctx"], np.float32).reshape(8, 128)
    m["cvec"] = cv
    m["consts"] = make_consts()
    m["rope"] = make_rope()
    return m


def kernel(**inputs):
    nc, _ = build(n_layers=DEPTH, dump=None)
    base = {n: np.ascontiguousarray(np.asarray(inputs[n], dtype=np.float32).reshape(s)) for n, s in PARAMS}
    consts = make_consts()
    rope = make_rope()
    in_maps = []
    for b in range(8):
        m = dict(base)
        m["x"] = np.ascontiguousarray(np.asarray(inputs["x"][b], dtype=np.float32))
        m["ctx"] = np.ascontiguousarray(np.asarray(inputs["ctx"][b], dtype=np.float32))
        cv = np.zeros((16, 128), np.float32)
        cv[0:8] = np.asarray(inputs["c"][b], np.float32).reshape(8, 128)
        cv[8:16] = np.asarray(inputs["c_ctx"], np.float32).reshape(8, 128)
        m["cvec"] = cv
        m["consts"] = consts
        m["rope"] = rope
        in_maps.append(m)
    res = run_bass_kernel_spmd(nc, in_maps, core_ids=list(range(8)))
    out = np.stack([np.asarray(res.results[b]["out"], dtype=np.float32) for b in range(8)], 0)
    return out
```
